# Optimizing a Trainium2 kernel written in Bass

```python
import jax
import jax.numpy as jnp
from jax import lax
import numpy as np

D_MODEL = 1024
BATCH = 16
SEQ = 4096
DEPTH = 1

D_MIX = D_MODEL
NORM_EPS = 1e-6

RWKV_HEADS = 8
RWKV_HEAD_DIM = 64
D_RWKV = RWKV_HEADS * RWKV_HEAD_DIM
DECAY_LORA = 32
ICLR_LORA = 32
GATE_LORA = 96
GN_EPS = 64e-5
RWKV_IN_SIZES = (D_RWKV, D_RWKV, D_RWKV, DECAY_LORA, ICLR_LORA, GATE_LORA)
D_RWKV_IN = sum(RWKV_IN_SIZES)

MLA_HEADS = 8
QK_NOPE_DIM = 64
QK_ROPE_DIM = 32
QK_HEAD_DIM = QK_NOPE_DIM + QK_ROPE_DIM
V_HEAD_DIM = 64
D_MLA = MLA_HEADS * V_HEAD_DIM
Q_LORA_RANK = 256
KV_LORA_RANK = 128
ROPE_THETA = 10000.0
Q_BLOCK = 128
MAX_POS_OFFSET = 4096
D_MLA_IN = Q_LORA_RANK + KV_LORA_RANK + QK_ROPE_DIM

D_IN = D_RWKV_IN + D_MLA_IN

N_EXPERTS = 256
TOP_K = 8
N_GROUPS = 8
TOPK_GROUPS = 4
D_EXPERT = 256
D_SHARED = 256
ROUTED_SCALE = 2.5
MOE_BLOCK = 256

kernel_name = 'hybrid_rwkv7_mla_moe_adaln'


def rms_norm(x, gain):
    xf = x.astype(jnp.float32)
    y = xf * lax.rsqrt(jnp.mean(xf * xf, axis=-1, keepdims=True) + NORM_EPS)
    return (y * gain.astype(jnp.float32)).astype(x.dtype)


def swiglu(gu):
    g, u = jnp.split(gu, 2, axis=-1)
    return jax.nn.silu(g) * u


def token_shift(u, mu):
    prev = jnp.pad(u, ((0, 0), (1, 0), (0, 0)))[:, :-1]
    return u + (prev - u) * mu


def rope_tables(positions):
    inv_freq = ROPE_THETA ** (-jnp.arange(0, QK_ROPE_DIM, 2, dtype=jnp.float32) / QK_ROPE_DIM)
    ang = positions.astype(jnp.float32)[..., None] * inv_freq
    return jnp.cos(ang)[:, :, None, :], jnp.sin(ang)[:, :, None, :]


def apply_rope_tail(x, cos, sin):
    x_nope, x_pe = jnp.split(x.astype(jnp.float32), [QK_NOPE_DIM], axis=-1)
    x1, x2 = jnp.split(x_pe, 2, axis=-1)
    x_pe = jnp.concatenate([x1 * cos - x2 * sin, x2 * cos + x1 * sin], axis=-1)
    return jnp.concatenate([x_nope, x_pe], axis=-1).astype(x.dtype)


def rwkv7_scan(r, decay, k, v, kk, a):
    B, T, H, N = r.shape

    def step(S, inp):
        r_t, w_t, k_t, v_t, kk_t, a_t = inp
        sa = jnp.einsum('bhvk,bhk->bhv', S, -kk_t)
        S = (S * w_t[:, :, None, :]
             + sa[..., None] * (kk_t * a_t)[:, :, None, :]
             + v_t[..., None] * k_t[:, :, None, :])
        y = jnp.einsum('bhvk,bhk->bhv', S, r_t)
        return S, y

    xs = tuple(jnp.moveaxis(t, 1, 0) for t in (r, decay, k, v, kk, a))
    S0 = jnp.zeros((B, H, N, N), jnp.float32)
    _, ys = lax.scan(step, S0, xs)
    return jnp.moveaxis(ys, 0, 1)


def rwkv7_group(u, mu, decay_w0, decay_up, iclr_a0, iclr_up, gate_up, k_k, k_a, r_k, ln_w, ln_b):
    B, T, _ = u.shape
    H, N = RWKV_HEADS, RWKV_HEAD_DIM
    out_dtype = u.dtype
    u = token_shift(u.astype(jnp.float32), mu.astype(jnp.float32))
    r, k, v, xw, xa, xg = jnp.split(u, np.cumsum(RWKV_IN_SIZES)[:-1].tolist(), axis=-1)
    log_w = -jax.nn.softplus(-(decay_w0 + jnp.tanh(xw) @ decay_up)) - 0.5
    decay = jnp.exp(-jnp.exp(log_w))
    a = jax.nn.sigmoid(iclr_a0 + xa @ iclr_up)
    g = jax.nn.sigmoid(xg) @ gate_up
    heads = lambda t: t.reshape(B, T, H, N)
    kk = heads(k * k_k)
    kk = kk * lax.rsqrt(jnp.maximum(jnp.sum(kk * kk, axis=-1, keepdims=True), 1e-24))
    k = k * (1.0 + (a - 1.0) * k_a)
    r, k, v, decay, a = heads(r), heads(k), heads(v), heads(decay), heads(a)
    y = rwkv7_scan(r, decay, k, v, kk, a)
    mean = jnp.mean(y, axis=-1, keepdims=True)
    var = jnp.mean(jnp.square(y - mean), axis=-1, keepdims=True)
    y = ((y - mean) * lax.rsqrt(var + GN_EPS)).reshape(B, T, D_RWKV) * ln_w + ln_b
    bonus = jnp.sum(r * k * r_k, axis=-1, keepdims=True) * v
    y = (y + bonus.reshape(B, T, D_RWKV)) * g
    return y.astype(out_dtype)


def causal_block_attention(q, k, v):
    T = q.shape[1]
    scale = QK_HEAD_DIM ** -0.5
    outs = []
    for start in range(0, T, Q_BLOCK):
        end = min(start + Q_BLOCK, T)
        s = jnp.einsum('bqhd,bkhd->bhqk', q[:, start:end], k[:, :end]).astype(jnp.float32) * scale
        q_pos = jnp.arange(start, end)[:, None]
        k_pos = jnp.arange(end)[None, :]
        s = jnp.where(k_pos <= q_pos, s, -jnp.inf)
        p = jax.nn.softmax(s, axis=-1).astype(v.dtype)
        outs.append(jnp.einsum('bhqk,bkhd->bqhd', p, v[:, :end]))
    return jnp.concatenate(outs, axis=1)


def mla_group(u, cos, sin, q_a_norm, w_q_b, kv_a_norm, w_kv_b, q_norm, k_norm):
    B, T, _ = u.shape
    H = MLA_HEADS
    out_dtype = u.dtype
    q_lat, kv_lat, k_pe = jnp.split(u, [Q_LORA_RANK, Q_LORA_RANK + KV_LORA_RANK], axis=-1)
    q = (rms_norm(q_lat, q_a_norm) @ w_q_b).reshape(B, T, H, QK_HEAD_DIM)
    kv = (rms_norm(kv_lat, kv_a_norm) @ w_kv_b).reshape(B, T, H, QK_NOPE_DIM + V_HEAD_DIM)
    k_nope, v = jnp.split(kv, [QK_NOPE_DIM], axis=-1)
    k = jnp.concatenate([k_nope, jnp.broadcast_to(k_pe[:, :, None, :], (B, T, H, QK_ROPE_DIM))], axis=-1)
    q = apply_rope_tail(rms_norm(q, q_norm), cos, sin)
    k = apply_rope_tail(rms_norm(k, k_norm), cos, sin)
    o = causal_block_attention(q, k, v)
    return o.reshape(B, T, D_MLA).astype(out_dtype)


def moe_ffn(h, w_router, router_bias, w_e_gate_up, w_e_down, w_sh_gate_up, w_sh_down):
    B, T, D = h.shape
    N = B * T
    NK = N * TOP_K
    xt = h.reshape(N, D)
    scores = jax.nn.sigmoid((xt @ w_router).astype(jnp.float32))
    sel = scores + router_bias.astype(jnp.float32)
    grp = sel.reshape(N, N_GROUPS, N_EXPERTS // N_GROUPS)
    grp_score = jnp.sum(lax.top_k(grp, 2)[0], axis=-1)
    _, top_grp = lax.top_k(grp_score, TOPK_GROUPS)
    grp_mask = jnp.any(top_grp[..., None] == jnp.arange(N_GROUPS), axis=1)
    exp_mask = jnp.repeat(grp_mask, N_EXPERTS // N_GROUPS, axis=-1)
    _, top_e = lax.top_k(jnp.where(exp_mask, sel, -jnp.inf), TOP_K)
    w = jnp.take_along_axis(scores, top_e, axis=-1)
    w = w / jnp.sum(w, axis=-1, keepdims=True) * ROUTED_SCALE

    e_flat = top_e.reshape(NK).astype(jnp.int32)
    tok_flat = jnp.repeat(jnp.arange(N, dtype=jnp.int32), TOP_K)
    w_flat = w.reshape(NK)
    order = jnp.argsort(e_flat, stable=True)
    e_sorted, tok_sorted, w_sorted = e_flat[order], tok_flat[order], w_flat[order]
    counts = jnp.bincount(e_flat, length=N_EXPERTS).astype(jnp.int32)
    starts = jnp.cumsum(counts) - counts
    padded = (counts + MOE_BLOCK - 1) // MOE_BLOCK * MOE_BLOCK
    pad_ends = jnp.cumsum(padded)
    pad_starts = pad_ends - padded
    dest = pad_starts[e_sorted] + (jnp.arange(NK, dtype=jnp.int32) - starts[e_sorted])
    n_blocks = (NK + N_EXPERTS * (MOE_BLOCK - 1)) // MOE_BLOCK
    P = n_blocks * MOE_BLOCK
    buf_tok = jnp.full((P,), N, jnp.int32).at[dest].set(tok_sorted)
    buf_w = jnp.zeros((P,), jnp.float32).at[dest].set(w_sorted)
    block_expert = jnp.minimum(
        jnp.searchsorted(pad_ends, jnp.arange(n_blocks, dtype=jnp.int32) * MOE_BLOCK, side='right'),
        N_EXPERTS - 1).astype(jnp.int32)

    x_pad = jnp.concatenate([xt, jnp.zeros((1, D), xt.dtype)], axis=0)

    def body(acc, blk):
        idx, wts, e = blk
        xb = x_pad[idx]
        yb = swiglu(xb @ w_e_gate_up[e]) @ w_e_down[e]
        return acc.at[idx].add(yb.astype(jnp.float32) * wts[:, None]), None

    acc0 = jnp.zeros((N + 1, D), jnp.float32)
    acc, _ = lax.scan(body, acc0, (buf_tok.reshape(n_blocks, MOE_BLOCK),
                                   buf_w.reshape(n_blocks, MOE_BLOCK), block_expert))
    shared = swiglu(xt @ w_sh_gate_up) @ w_sh_down
    return (acc[:N].astype(h.dtype) + shared).reshape(B, T, D)


def hybrid_layer(x, c_act, cos, sin, ada_w, ada_b, norm_mix, w_in, rwkv_mu, decay_w0, decay_up,
                 iclr_a0, iclr_up, gate_up, rwkv_k_k, rwkv_k_a, rwkv_r_k, ln_x_w, ln_x_b,
                 q_a_norm, w_q_b, kv_a_norm, w_kv_b, q_norm, k_norm, w_out, norm_ffn,
                 w_router, router_bias, w_e_gate_up, w_e_down, w_sh_gate_up, w_sh_down):
    mod = c_act @ ada_w + ada_b
    sh_a, sc_a, g_a, sh_f, sc_f, g_f = [m[:, None, :] for m in jnp.split(mod, 6, axis=-1)]
    h = rms_norm(x, norm_mix) * (1.0 + sc_a) + sh_a
    u = h @ w_in
    y_r = rwkv7_group(u[..., :D_RWKV_IN], rwkv_mu, decay_w0, decay_up, iclr_a0, iclr_up, gate_up,
                      rwkv_k_k, rwkv_k_a, rwkv_r_k, ln_x_w, ln_x_b)
    y_m = mla_group(u[..., D_RWKV_IN:], cos, sin, q_a_norm, w_q_b, kv_a_norm, w_kv_b,
                    q_norm, k_norm)
    x = x + g_a * (jnp.concatenate([y_r, y_m], axis=-1) @ w_out)
    h = rms_norm(x, norm_ffn) * (1.0 + sc_f) + sh_f
    x = x + g_f * moe_ffn(h, w_router, router_bias, w_e_gate_up, w_e_down, w_sh_gate_up, w_sh_down)
    return x


def setup_inputs(seed: int = 0) -> dict:
    key = jax.random.key(seed)
    ks = iter(jax.random.split(key, 40))
    f32 = jnp.float32
    L, D = DEPTH, D_MODEL

    def nrm(shape, scale):
        return jax.random.normal(next(ks), shape, f32) * scale

    def gain(shape):
        return 1.0 + nrm(shape, 0.02)

    x = nrm((BATCH, SEQ, D), 1.0)
    c = nrm((BATCH, D), 1.0)
    start = jax.random.randint(next(ks), (BATCH, 1), 0, MAX_POS_OFFSET, jnp.int32)
    positions = start + jnp.arange(SEQ, dtype=jnp.int32)[None, :]
    return {
        'x': x,
        'c': c,
        'positions': positions,
        'ada_w': nrm((L, D, 6 * D), 0.5 * D ** -0.5),
        'ada_b': nrm((L, 6 * D), 0.02),
        'norm_mix': gain((L, D)),
        'w_in': nrm((L, D, D_IN), D ** -0.5),
        'rwkv_mu': jax.random.uniform(next(ks), (L, D_RWKV_IN), f32),
        'decay_w0': jax.random.uniform(next(ks), (L, D_RWKV), f32, minval=-5.0, maxval=1.0),
        'decay_up': nrm((L, DECAY_LORA, D_RWKV), DECAY_LORA ** -0.5),
        'iclr_a0': nrm((L, D_RWKV), 0.1),
        'iclr_up': nrm((L, ICLR_LORA, D_RWKV), ICLR_LORA ** -0.5),
        'gate_up': nrm((L, GATE_LORA, D_RWKV), GATE_LORA ** -0.5),
        'rwkv_k_k': 0.85 + nrm((L, D_RWKV), 0.05),
        'rwkv_k_a': 1.0 + nrm((L, D_RWKV), 0.05),
        'rwkv_r_k': nrm((L, RWKV_HEADS, RWKV_HEAD_DIM), 0.1),
        'ln_x_w': gain((L, D_RWKV)),
        'ln_x_b': nrm((L, D_RWKV), 0.02),
        'q_a_norm': gain((L, Q_LORA_RANK)),
        'w_q_b': nrm((L, Q_LORA_RANK, MLA_HEADS * QK_HEAD_DIM), Q_LORA_RANK ** -0.5),
        'kv_a_norm': gain((L, KV_LORA_RANK)),
        'w_kv_b': nrm((L, KV_LORA_RANK, MLA_HEADS * (QK_NOPE_DIM + V_HEAD_DIM)), KV_LORA_RANK ** -0.5),
        'q_norm': gain((L, QK_HEAD_DIM)),
        'k_norm': gain((L, QK_HEAD_DIM)),
        'w_out': nrm((L, D_MIX, D), D_MIX ** -0.5),
        'norm_ffn': gain((L, D)),
        'w_router': nrm((L, D, N_EXPERTS), D ** -0.5),
        'router_bias': nrm((L, N_EXPERTS), 0.01),
        'w_e_gate_up': nrm((L, N_EXPERTS, D, 2 * D_EXPERT), D ** -0.5),
        'w_e_down': nrm((L, N_EXPERTS, D_EXPERT, D), D_EXPERT ** -0.5),
        'w_sh_gate_up': nrm((L, D, 2 * D_SHARED), D ** -0.5),
        'w_sh_down': nrm((L, D_SHARED, D), D_SHARED ** -0.5),
    }


def reference(x, c, positions, ada_w, ada_b, norm_mix, w_in, rwkv_mu, decay_w0, decay_up,
              iclr_a0, iclr_up, gate_up, rwkv_k_k, rwkv_k_a, rwkv_r_k, ln_x_w, ln_x_b,
              q_a_norm, w_q_b, kv_a_norm, w_kv_b, q_norm, k_norm, w_out, norm_ffn,
              w_router, router_bias, w_e_gate_up, w_e_down, w_sh_gate_up, w_sh_down):
    cos, sin = rope_tables(positions)
    c_act = jax.nn.silu(c)
    for l in range(DEPTH):
        x = hybrid_layer(x, c_act, cos, sin, ada_w[l], ada_b[l], norm_mix[l], w_in[l], rwkv_mu[l],
                         decay_w0[l], decay_up[l], iclr_a0[l], iclr_up[l], gate_up[l],
                         rwkv_k_k[l], rwkv_k_a[l], rwkv_r_k[l], ln_x_w[l], ln_x_b[l],
                         q_a_norm[l], w_q_b[l], kv_a_norm[l], w_kv_b[l], q_norm[l], k_norm[l],
                         w_out[l], norm_ffn[l], w_router[l], router_bias[l], w_e_gate_up[l],
                         w_e_down[l], w_sh_gate_up[l], w_sh_down[l])
    return x
```

```python
import os
import numpy as np
from contextlib import ExitStack
import concourse.bass as bass
import concourse.mybir as mybir
from concourse.bass_utils import run_bass_kernel_spmd


F32 = mybir.dt.float32
F32R = mybir.dt.float32r
BF16 = mybir.dt.bfloat16
I32 = mybir.dt.int32
U32 = mybir.dt.uint32
ACT = mybir.ActivationFunctionType
ALU = mybir.AluOpType
AX = mybir.AxisListType


class Buf:
    __slots__ = ("t", "lw", "rd", "name")

    def __init__(self, t, name=""):
        self.t = t
        self.lw = None
        self.rd = {}
        self.name = name

    def __getitem__(self, k):
        return self.t[k]


class K:
    def __init__(self, nc, es, ndma=24):
        self.nc = nc
        self.es = es
        self.eng = {"pe": nc.tensor, "act": nc.scalar, "dve": nc.vector, "pool": nc.gpsimd, "sp": nc.sync}
        self.sem = {}
        self.cnt = {}
        for n in ("pe", "act", "dve", "pool"):
            self.sem[n] = es.enter_context(nc.semaphore("s_" + n))
            self.cnt[n] = 0
        self.ndma = ndma
        self.dsem = [es.enter_context(nc.semaphore("s_dma%d" % i)) for i in range(ndma)]
        self.dcnt = [0] * ndma
        self.dnext = 0
        self.waited = {n: {} for n in ("pe", "act", "dve", "pool", "sp")}
        self.nins = 0
        self.imax = 6
        self.gen = 0
        self.top_es = es

    def sb(self, name, shape, dt=F32):
        return Buf(self.es.enter_context(self.nc.sbuf_tensor(name, list(shape), dt)), name)

    def ps(self, name, shape, dt=F32):
        return Buf(self.es.enter_context(self.nc.psum_tensor(name, list(shape), dt)), name)

    def dram(self, name, shape, dt=F32, kind="Internal"):
        return Buf(self.nc.dram_tensor(name, list(shape), dt, kind=kind).ap(), name)

    def _semof(self, key):
        return self.dsem[key[1]] if isinstance(key, tuple) else self.sem[key]

    def _wait(self, e, key, val, gen=None):
        if key == e and e == "pe":
            return
        if gen is not None and gen < self.gen:
            return
        w = self.waited[e]
        if w.get(key, 0) >= val:
            return
        self.eng[e].wait_ge(self._semof(key), val)
        w[key] = val
        self.nins += 1

    def _deps(self, e, R, W):
        for b in R:
            if b.lw is not None:
                self._wait(e, *b.lw)
        for b in W:
            if b.lw is not None:
                self._wait(e, *b.lw)
            for k, (v, g) in b.rd.items():
                self._wait(e, k, v, g)

    def _done(self, key, val, R, W):
        g = None if isinstance(key, tuple) else self.gen
        for b in R:
            o = b.rd.get(key)
            if o is None or o[1] != g or o[0] < val:
                b.rd[key] = (val, g)
        for b in W:
            b.lw = (key, val, g)
            b.rd = {}

    def op(self, e, fn, R=(), W=()):
        self._deps(e, R, W)
        ins = fn(self.eng[e])
        self.cnt[e] += 1
        ins.then_inc(self.sem[e], 1)
        self._done(e, self.cnt[e], R, W)
        self.nins += 1
        return ins

    def dma(self, q, out, in_, R=(), W=(), **kw):
        i = self.dnext
        self.dnext = (self.dnext + 1) % self.ndma
        key = ("dma", i)
        if self.dcnt[i] > 0:
            self._wait(q, key, self.dcnt[i])
        self._deps(q, R, W)
        ins = self.eng[q].dma_start(out=out, in_=in_, **kw)
        self.dcnt[i] += 16
        ins.then_inc(self.dsem[i], 16)
        self._done(key, self.dcnt[i], R, W)
        self.nins += 1
        return ins

    def idma(self, R=(), W=(), **kw):
        q = "pool"
        if not hasattr(self, "ipend"):
            self.ipend = []
        while len(self.ipend) >= self.imax:
            kk, vv = self.ipend.pop(0)
            self._wait("pool", kk, vv)
        i = self.dnext
        self.dnext = (self.dnext + 1) % self.ndma
        key = ("dma", i)
        if self.dcnt[i] > 0:
            self._wait(q, key, self.dcnt[i])
        self._deps(q, R, W)
        ins = self.nc.gpsimd.indirect_dma_start(**kw)
        self.dcnt[i] += 16
        ins.then_inc(self.dsem[i], 16)
        self.ipend.append((key, self.dcnt[i]))
        self._done(key, self.dcnt[i], R, W)
        self.nins += 1
        return ins

    def barrier(self):
        for e in ("pe", "act", "dve", "pool", "sp"):
            for o in ("pe", "act", "dve", "pool"):
                if o != e and self.cnt[o] > 0:
                    self._wait(e, o, self.cnt[o])
            for i in range(self.ndma):
                if self.dcnt[i] > 0:
                    self._wait(e, ("dma", i), self.dcnt[i])

    def regen(self):
        self.barrier()
        self.gen += 1
        for n in ("pe", "act", "dve", "pool"):
            self.sem[n] = self.top_es.enter_context(self.nc.semaphore("s_%s_g%d" % (n, self.gen)))
            self.cnt[n] = 0
        for e in self.waited:
            for n in ("pe", "act", "dve", "pool"):
                self.waited[e].pop(n, None)

    def finish(self, outs):
        for b in outs:
            if b.lw is not None:
                self._wait("sp", *b.lw)


import os
SKIP = os.environ.get('SKIP', '').split(',')
D = 1024
DIN = 2112
NB = 2
EPS = 1e-6


def build(T, stop_after=99, dbg=()):
    nc = bass.Bass("TRN2", target_bir_lowering=False)
    es = ExitStack()
    k = K(nc, es)
    NT = T // 128
    NTOK = NB * T

    def inp(name, shape, dt=F32):
        return Buf(nc.dram_tensor(name, list(shape), dt, kind="ExternalInput").ap(), name)

    x_d = inp("x", [NB * T, D])
    c_d = inp("c", [NB, D])
    ada_w_d = inp("ada_w", [D, 6 * D])
    ada_b_d = inp("ada_b", [1, 6 * D])
    norm_mix_d = inp("norm_mix", [1, D])
    w_in_d = inp("w_in", [D, DIN])
    outs = {}

    def outp(name, shape, dt=F32):
        b = Buf(nc.dram_tensor(name, list(shape), dt, kind="ExternalOutput").ap(), name)
        outs[name] = b
        return b

    ident_f = k.sb("ident_f", [128, 128], F32)
    ident_b = k.sb("ident_b", [128, 128], BF16)
    iot = k.sb("iot", [128, 128], I32)
    k.op("pool", lambda e: e.iota(iot[:], pattern=[[1, 128]], base=0, channel_multiplier=-1), W=[iot])
    k.op("dve", lambda e: e.tensor_scalar(ident_f[:], iot[:], 0.0, None, op0=ALU.is_equal), R=[iot], W=[ident_f])
    k.op("dve", lambda e: e.tensor_copy(ident_b[:], ident_f[:]), R=[ident_f], W=[ident_b])

    MOD = [[k.sb("mod%d_%d" % (b, w), [128, D]) for w in range(6)] for b in range(NB)]
    with ExitStack() as es0:
        k.es = es0
        cT = k.sb("cT", [128, NB, 8])
        cS = k.sb("cS", [128, NB, 8])
        with nc.allow_non_contiguous_dma(reason="tiny c transpose load"):
            for b in range(NB):
                k.dma("sp", cT[:, b, :], c_d[b, :].rearrange("(j p) -> p j", p=128), W=[cT])
        k.op("act", lambda e: e.activation(cS[:], cT[:], ACT.Silu), R=[cT], W=[cS])
        cB = [[k.sb("cB%d_%d" % (b, j), [128, 128]) for j in range(8)] for b in range(NB)]
        for b in range(NB):
            for j in range(8):
                k.op("dve", lambda e: e.tensor_copy(cB[b][j][:], cS[:, b, j:j + 1].to_broadcast([128, 128])),
                     R=[cS], W=[cB[b][j]])
        awb = [k.sb("awb%d" % i, [128, 8, 512]) for i in range(2)]
        abb = [k.sb("abb%d" % i, [128, 512]) for i in range(2)]
        pm = [k.ps("pm%d" % i, [128, 512]) for i in range(2)]
        for cb in range(12):
            aw = awb[cb % 2]
            ab = abb[cb % 2]
            k.dma("sp", aw[:], ada_w_d[:, cb * 512:(cb + 1) * 512].rearrange("(j p) n -> p j n", p=128), W=[aw])
            k.dma("sp", ab[:], ada_b_d[0:1, cb * 512:(cb + 1) * 512].partition_broadcast(128), W=[ab])
            for b in range(NB):
                p = pm[b]
                for j in range(8):
                    k.op("pe", lambda e: e.matmul(p[:], cB[b][j][:], aw[:, j, :], start=(j == 0), stop=(j == 7)),
                         R=[cB[b][j], aw], W=[p])
                dst = MOD[b][cb // 2]
                k.op("dve", lambda e: e.tensor_tensor(dst[:, (cb % 2) * 512:(cb % 2 + 1) * 512], p[:], ab[:], op=ALU.add),
                     R=[p, ab], W=[dst])
        nmb = k.sb("nmb", [128, D])
        k.dma("sp", nmb[:], norm_mix_d[0:1, :].partition_broadcast(128), W=[nmb])
        for b in range(NB):
            m = MOD[b][1]
            k.op("dve", lambda e: e.scalar_tensor_tensor(m[:], m[:], 1.0, nmb[:], op0=ALU.add, op1=ALU.mult),
                 R=[m, nmb], W=[m])
    k.es = es
    k.regen()
    if "mod" in dbg:
        o = outp("dbg_mod", [NB, 6, D])
        for b in range(NB):
            for w in range(6):
                k.dma("sp", o[b, w:w + 1, :], MOD[b][w][0:1, :], R=[MOD[b][w]], W=[o])
    if stop_after <= 0:
        k.finish(list(outs.values()))
        return nc, es

    U_d = outp("U", [NTOK, DIN]) if "U" in dbg else k.dram("U", [NTOK, DIN])
    with ExitStack() as es1:
        k.es = es1
        win = k.sb("win", [128, 8, DIN], BF16)
        wst = [k.sb("wst%d" % i, [128, DIN]) for i in range(2)]
        for j in range(8):
            s = wst[j % 2]
            k.dma("sp", s[:], w_in_d[j * 128:(j + 1) * 128, :], W=[s])
            k.op("pool" if j % 2 else "dve", lambda e: e.tensor_copy(win[:, j, :], s[:]), R=[s], W=[win])
        xt = [k.sb("xt%d" % i, [128, D]) for i in range(2)]
        junk = k.sb("junk", [128, D])
        ssq = [k.sb("ssq%d" % i, [128, 1]) for i in range(2)]
        rstd = [k.sb("rstd%d" % i, [128, 1]) for i in range(2)]
        hn = k.sb("hn", [128, D])
        hb = [k.sb("hb%d" % i, [128, D], BF16) for i in range(2)]
        hT = [k.sb("hT%d" % i, [128, 8, 128], BF16) for i in range(2)]
        ut = [k.sb("ut%d" % i, [128, DIN]) for i in range(2)]
        pT = [k.ps("pT%d" % i, [128, 8, 128], BF16) for i in range(2)]
        pu = [k.ps("pu%d" % i, [128, 512]) for i in range(3)]
        npu = 0
        for i in range(NB * NT):
            b = i // NT
            x_ = xt[i % 2]; sq = ssq[i % 2]; rs = rstd[i % 2]; h_ = hb[i % 2]; hT_ = hT[i % 2]; u_ = ut[i % 2]; pT_ = pT[i % 2]
            k.dma("sp", x_[:], x_d[i * 128:(i + 1) * 128, :], W=[x_])
            k.op("act", lambda e: e.activation(junk[:], x_[:], ACT.Square, accum_out=sq[:]), R=[x_], W=[junk, sq])
            k.op("dve", lambda e: e.tensor_scalar(rs[:], sq[:], 1.0 / D, EPS, op0=ALU.mult, op1=ALU.add), R=[sq], W=[rs])
            k.op("act", lambda e: e.sqrt(rs[:], rs[:]), R=[rs], W=[rs])
            k.op("dve", lambda e: e.reciprocal(rs[:], rs[:]), R=[rs], W=[rs])
            k.op("dve", lambda e: e.scalar_tensor_tensor(hn[:], x_[:], rs[:], MOD[b][1][:], op0=ALU.mult, op1=ALU.mult),
                 R=[x_, rs, MOD[b][1]], W=[hn])
            k.op("dve", lambda e: e.tensor_tensor(h_[:], hn[:], MOD[b][0][:], op=ALU.add), R=[hn, MOD[b][0]], W=[h_])
            for j in range(8):
                k.op("pe", lambda e: e.transpose(pT_[:, j, :], h_[:, j * 128:(j + 1) * 128], ident_b[:]),
                     R=[h_, ident_b], W=[pT_])
            k.op("act", lambda e: e.copy(hT_[:], pT_[:]), R=[pT_], W=[hT_])
            for cbi, (c0, c1) in enumerate([(0, 512), (512, 1024), (1024, 1536), (1536, 2048), (2048, 2112)]):
                p = pu[npu % 3]; npu += 1
                for j in range(8):
                    k.op("pe", lambda e: e.matmul(p[:, 0:c1 - c0], hT_[:, j, :], win[:, j, c0:c1], start=(j == 0), stop=(j == 7)),
                         R=[hT_, win], W=[p])
                k.op("dve" if cbi % 2 else "act",
                     (lambda e: e.tensor_copy(u_[:, c0:c1], p[:, 0:c1 - c0])) if cbi % 2 else
                     (lambda e: e.copy(u_[:, c0:c1], p[:, 0:c1 - c0])), R=[p], W=[u_])
            k.dma("sp", U_d[i * 128:(i + 1) * 128, :], u_[:], R=[u_], W=[U_d])
    k.es = es
    k.regen()
    if stop_after <= 1:
        k.finish(list(outs.values()))
        return nc, es


    rwkv_mu_d = inp("rwkv_mu", [1, 1696]); decay_w0_d = inp("decay_w0", [1, 512]); decay_up_d = inp("decay_up", [32, 512])
    iclr_a0_d = inp("iclr_a0", [1, 512]); iclr_up_d = inp("iclr_up", [32, 512]); gate_up_d = inp("gate_up", [96, 512])
    k_k_d = inp("rwkv_k_k", [1, 512]); k_a_d = inp("rwkv_k_a", [1, 512]); r_k_d = inp("rwkv_r_k", [1, 512])
    ROWS_d = outp("ROWS", [NB, T, 5, 512]) if "ROWS" in dbg else k.dram("ROWS", [NB, T, 5, 512])
    VT_d = outp("VT", [128, NT, 8, 128]) if "VT" in dbg else k.dram("VT", [128, NT, 8, 128])
    BON_d = outp("BON", [NTOK, 512]) if "BON" in dbg else k.dram("BON", [NTOK, 512])
    G_d = outp("G", [NTOK, 512]) if "G" in dbg else k.dram("G", [NTOK, 512])
    with ExitStack() as es2:
        k.es = es2
        def bc(name, src, n):
            t_ = k.sb(name, [128, n])
            k.dma("sp", t_[:], src[0:1, :].partition_broadcast(128), W=[t_])
            return t_
        mu_b = bc("mu_b", rwkv_mu_d, 1696); w0_b = bc("w0_b", decay_w0_d, 512); a0_b = bc("a0_b", iclr_a0_d, 512)
        kk_b = bc("kk_b", k_k_d, 512); ka_b = bc("ka_b", k_a_d, 512); rk_b = bc("rk_b", r_k_d, 512)
        dup = k.sb("dup", [32, 512]); iup = k.sb("iup", [32, 512]); gup = k.sb("gup", [96, 512])
        k.dma("sp", dup[:], decay_up_d[:], W=[dup]); k.dma("sp", iup[:], iclr_up_d[:], W=[iup]); k.dma("sp", gup[:], gate_up_d[:], W=[gup])
        uu = [k.sb("uu%d" % i, [128, 1696]) for i in range(2)]
        up_ = [k.sb("up%d" % i, [128, 1696]) for i in range(2)]
        us = k.sb("us", [128, 1696])
        Z = k.sb("Z", [128, 160]); ZT = k.sb("ZT", [96, 3, 128])
        rows = [k.sb("rows%d" % i, [128, 5, 512]) for i in range(2)]
        gt = [k.sb("gt%d" % i, [128, 512]) for i in range(2)]
        bon = [k.sb("bon%d" % i, [128, 512]) for i in range(2)]
        at = k.sb("at", [128, 512]); t5 = k.sb("t5", [128, 512]); t6 = k.sb("t6", [128, 512])
        s8 = k.sb("s8", [128, 8]); b8 = k.sb("b8", [128, 8])
        vts = [k.sb("vts%d" % i, [64, 8, 128]) for i in range(2)]
        pZ = k.ps("pZ", [128, 3, 128]); pl = k.ps("pl", [128, 3, 512]); pv = k.ps("pv", [64, 8, 128])
        for i in range(NB * NT):
            b = i // NT; it = i % NT
            u_ = uu[i % 2]; p_ = up_[i % 2]; rw = rows[i % 2]; g_ = gt[i % 2]; bo = bon[i % 2]; vt_ = vts[i % 2]
            k.dma("sp", u_[:], U_d[i * 128:(i + 1) * 128, 0:1696], R=[U_d], W=[u_])
            if it == 0:
                k.op("pool", lambda e: e.memset(p_[0:1, :], 0.0), W=[p_])
                k.dma("sp", p_[1:128, :], U_d[i * 128:i * 128 + 127, 0:1696], R=[U_d], W=[p_])
            else:
                k.dma("sp", p_[:], U_d[i * 128 - 1:i * 128 + 127, 0:1696], R=[U_d], W=[p_])
            k.op("pool", lambda e: e.tensor_tensor(p_[:], p_[:], u_[:], op=ALU.subtract), R=[p_, u_], W=[p_])
            k.op("pool", lambda e: e.tensor_tensor(p_[:], p_[:], mu_b[:], op=ALU.mult), R=[p_, mu_b], W=[p_])
            k.op("dve", lambda e: e.tensor_tensor(us[:], p_[:], u_[:], op=ALU.add), R=[p_, u_], W=[us])
            r_ = us[:, 0:512]; kx = us[:, 512:1024]; v_ = us[:, 1024:1536]
            k.op("act", lambda e: e.activation(Z[:, 0:32], us[:, 1536:1568], ACT.Tanh), R=[us], W=[Z])
            k.op("act", lambda e: e.copy(Z[:, 32:64], us[:, 1568:1600]), R=[us], W=[Z])
            k.op("act", lambda e: e.activation(Z[:, 64:160], us[:, 1600:1696], ACT.Sigmoid), R=[us], W=[Z])
            k.op("pe", lambda e: e.transpose(pZ[0:32, 0, :], Z[:, 0:32], ident_f[:]), R=[Z, ident_f], W=[pZ])
            k.op("pe", lambda e: e.transpose(pZ[0:32, 1, :], Z[:, 32:64], ident_f[:]), R=[Z, ident_f], W=[pZ])
            k.op("pe", lambda e: e.transpose(pZ[0:96, 2, :], Z[:, 64:160], ident_f[:]), R=[Z, ident_f], W=[pZ])
            k.op("dve", lambda e: e.tensor_copy(ZT[0:32, 0:2, :], pZ[0:32, 0:2, :]), R=[pZ], W=[ZT])
            k.op("dve", lambda e: e.tensor_copy(ZT[0:96, 2, :], pZ[0:96, 2, :]), R=[pZ], W=[ZT])
            k.op("pe", lambda e: e.matmul(pl[:, 0, :], ZT[0:32, 0, :], dup[:], start=True, stop=True), R=[ZT, dup], W=[pl])
            k.op("pe", lambda e: e.matmul(pl[:, 1, :], ZT[0:32, 1, :], iup[:], start=True, stop=True), R=[ZT, iup], W=[pl])
            k.op("pe", lambda e: e.matmul(pl[:, 2, :], ZT[0:96, 2, :], gup[:], start=True, stop=True), R=[ZT, gup], W=[pl])
            k.op("dve", lambda e: e.tensor_tensor(t5[:], pl[:, 0, :], w0_b[:], op=ALU.add), R=[pl, w0_b], W=[t5])
            k.op("act", lambda e: e.activation(t5[:], t5[:], ACT.Sigmoid), R=[t5], W=[t5])
            k.op("act", lambda e: e.activation(rw[:, 0, :], t5[:], ACT.Exp, scale=-float(np.exp(-0.5))), R=[t5], W=[rw])
            k.op("dve", lambda e: e.tensor_tensor(at[:], pl[:, 1, :], a0_b[:], op=ALU.add), R=[pl, a0_b], W=[at])
            k.op("act", lambda e: e.activation(at[:], at[:], ACT.Sigmoid), R=[at], W=[at])
            k.op("act", lambda e: e.copy(g_[:], pl[:, 2, :]), R=[pl], W=[g_])
            k.op("dve", lambda e: e.tensor_tensor(rw[:, 1, :], kx, kk_b[:], op=ALU.mult), R=[us, kk_b], W=[rw])
            k.op("pool", lambda e: e.tensor_tensor(t6[:], rw[:, 1, :], rw[:, 1, :], op=ALU.mult), R=[rw], W=[t6])
            k.op("dve", lambda e: e.tensor_reduce(s8[:], t6[:].rearrange("p (h n) -> p h n", h=8), axis=AX.X, op=ALU.add), R=[t6], W=[s8])
            k.op("dve", lambda e: e.tensor_scalar(s8[:], s8[:], 1e-24, None, op0=ALU.max), R=[s8], W=[s8])
            k.op("act", lambda e: e.sqrt(s8[:], s8[:]), R=[s8], W=[s8])
            k.op("dve", lambda e: e.reciprocal(s8[:], s8[:]), R=[s8], W=[s8])
            k.op("dve", lambda e: e.tensor_tensor(rw[:, 1, :].rearrange("p (h n) -> p h n", h=8), rw[:, 1, :].rearrange("p (h n) -> p h n", h=8),
                                                  s8[:].unsqueeze(2).to_broadcast([128, 8, 64]), op=ALU.mult), R=[rw, s8], W=[rw])
            k.op("pool", lambda e: e.tensor_tensor(rw[:, 2, :], rw[:, 1, :], at[:], op=ALU.mult), R=[rw, at], W=[rw])
            k.op("dve", lambda e: e.scalar_tensor_tensor(t5[:], at[:], -1.0, ka_b[:], op0=ALU.add, op1=ALU.mult), R=[at, ka_b], W=[t5])
            k.op("dve", lambda e: e.scalar_tensor_tensor(rw[:, 3, :], t5[:], 1.0, kx, op0=ALU.add, op1=ALU.mult), R=[t5, us], W=[rw])
            k.op("act", lambda e: e.copy(rw[:, 4, :], r_), R=[us], W=[rw])
            k.op("pool", lambda e: e.tensor_tensor(t6[:], rw[:, 3, :], r_, op=ALU.mult), R=[rw, us], W=[t6])
            k.op("pool", lambda e: e.tensor_tensor(t6[:], t6[:], rk_b[:], op=ALU.mult), R=[t6, rk_b], W=[t6])
            k.op("dve", lambda e: e.tensor_reduce(b8[:], t6[:].rearrange("p (h n) -> p h n", h=8), axis=AX.X, op=ALU.add), R=[t6], W=[b8])
            k.op("dve", lambda e: e.tensor_tensor(bo[:].rearrange("p (h n) -> p h n", h=8), v_.rearrange("p (h n) -> p h n", h=8),
                                                  b8[:].unsqueeze(2).to_broadcast([128, 8, 64]), op=ALU.mult), R=[us, b8], W=[bo])
            for h in range(8):
                k.op("pe", lambda e: e.transpose(pv[:, h, :], us[:, 1024 + h * 64:1024 + (h + 1) * 64], ident_f[:]), R=[us, ident_f], W=[pv])
            k.op("act", lambda e: e.copy(vt_[:], pv[:]), R=[pv], W=[vt_])
            k.dma("sp", ROWS_d[b, it * 128:(it + 1) * 128, :, :], rw[:], R=[rw], W=[ROWS_d])
            k.dma("sp", VT_d[b * 64:(b + 1) * 64, it, :, :], vt_[:], R=[vt_], W=[VT_d])
            k.dma("sp", BON_d[i * 128:(i + 1) * 128, :], bo[:], R=[bo], W=[BON_d])
            k.dma("sp", G_d[i * 128:(i + 1) * 128, :], g_[:], R=[g_], W=[G_d])
    k.es = es
    k.regen()
    if stop_after <= 2:
        k.finish(list(outs.values()))
        return nc, es


    TBS = 4
    YT_d = outp("YT", [128, T, 8]) if "YT" in dbg else k.dram("YT", [128, T, 8])
    with ExitStack() as es3:
        k.es = es3
        S = k.sb("S", [128, 512])
        t1 = k.sb("t1", [128, 512]); t2 = k.sb("t2", [128, 512])
        t3 = [k.sb("t3_%d" % i, [128, 512]) for i in range(2)]
        t4 = [k.sb("t4_%d" % i, [128, 512]) for i in range(2)]
        sa = k.sb("sa", [128, 8])
        RBt = [es3.enter_context(nc.sbuf_tensor("RB%d" % i, [128, TBS, 5, 512], F32)) for i in range(2)]
        RB0 = [Buf(t_, "rb0") for t_ in RBt]; RB1 = [Buf(t_, "rb1") for t_ in RBt]
        VTc = [k.sb("VTc%d" % i, [128, 8, 128]) for i in range(2)]
        Yc = [k.sb("Yc%d" % i, [128, 128, 8]) for i in range(2)]
        k.op("dve", lambda e: e.memset(S[:], 0.0), W=[S])
        v3 = lambda ap: ap.rearrange("p (h n) -> p h n", h=8)
        nblk = 0; nst = 0
        for it in range(NT):
            vt_ = VTc[it % 2]; y_ = Yc[it % 2]
            k.dma("sp", vt_[:], VT_d[:, it, :, :], R=[VT_d], W=[vt_])
            for blk in range(128 // TBS):
                t0 = it * 128 + blk * TBS
                rbt = RBt[nblk % 2]; rb0 = RB0[nblk % 2]; rb1 = RB1[nblk % 2]; nblk += 1
                k.dma("sp", rbt[0:64].rearrange("p t f n -> p (t f n)"),
                      ROWS_d[0:1, t0:t0 + TBS].rearrange("o t f n -> o (t f n)").partition_broadcast(64), R=[ROWS_d], W=[rb0])
                k.dma("act", rbt[64:128].rearrange("p t f n -> p (t f n)"),
                      ROWS_d[1:2, t0:t0 + TBS].rearrange("o t f n -> o (t f n)").partition_broadcast(64), R=[ROWS_d], W=[rb1])
                RB = [rb0, rb1]
                for s_ in range(TBS):
                    tl = blk * TBS + s_
                    Wb = rbt[:, s_, 0, :]; KKb = rbt[:, s_, 1, :]; KKAb = rbt[:, s_, 2, :]; Kb = rbt[:, s_, 3, :]; Rb = rbt[:, s_, 4, :]
                    t3_ = t3[nst % 2]; t4_ = t4[nst % 2]; nst += 1
                    k.op("pool", lambda e: e.tensor_tensor(v3(t3_[:]), v3(Kb), vt_[:, :, tl].unsqueeze(2).to_broadcast([128, 8, 64]), op=ALU.mult),
                         R=RB + [vt_], W=[t3_])
                    k.op("dve", lambda e: e.tensor_tensor(t1[:], S[:], KKb, op=ALU.mult), R=[S] + RB, W=[t1])
                    k.op("dve", lambda e: e.tensor_reduce(sa[:], v3(t1[:]), axis=AX.X, op=ALU.add, negate=True), R=[t1], W=[sa])
                    k.op("dve", lambda e: e.tensor_tensor(S[:], S[:], Wb, op=ALU.mult), R=[S] + RB, W=[S])
                    k.op("dve", lambda e: e.tensor_tensor(v3(t2[:]), v3(KKAb), sa[:].unsqueeze(2).to_broadcast([128, 8, 64]), op=ALU.mult),
                         R=RB + [sa], W=[t2])
                    k.op("dve", lambda e: e.tensor_tensor(S[:], S[:], t2[:], op=ALU.add), R=[S, t2], W=[S])
                    k.op("dve", lambda e: e.tensor_tensor(S[:], S[:], t3_[:], op=ALU.add), R=[S, t3_], W=[S])
                    k.op("pool", lambda e: e.tensor_tensor(t4_[:], S[:], Rb, op=ALU.mult), R=[S] + RB, W=[t4_])
                    k.op("dve", lambda e: e.tensor_reduce(y_[:, tl, :], v3(t4_[:]), axis=AX.X, op=ALU.add), R=[t4_], W=[y_])
            k.dma("sp", YT_d[:, it * 128:(it + 1) * 128, :], y_[:], R=[y_], W=[YT_d])
            if it % 12 == 11 and it != NT - 1:
                k.regen()
    k.es = es
    k.regen()
    if stop_after <= 3:
        k.finish(list(outs.values()))
        return nc, es

    ln_w_d = inp("ln_x_w", [1, 512]); ln_b_d = inp("ln_x_b", [1, 512])
    YCAT_d = outp("YCAT", [NTOK, 1024]) if "YCAT" in dbg else k.dram("YCAT", [NTOK, 1024])
    with ExitStack() as es4:
        k.es = es4
        lnw_b = k.sb("lnw_b", [128, 512]); lnb_b = k.sb("lnb_b", [128, 512])
        k.dma("sp", lnw_b[:], ln_w_d[0:1, :].partition_broadcast(128), W=[lnw_b])
        k.dma("sp", lnb_b[:], ln_b_d[0:1, :].partition_broadcast(128), W=[lnb_b])
        yc = [k.sb("ycp%d" % i, [128, 128, 8]) for i in range(2)]
        py = k.ps("py", [128, 8, 128])
        ytm = k.sb("ytm", [128, 8, 128])
        yb = k.sb("yb", [128, 8, 64]); yq = k.sb("yq", [128, 8, 64])
        m8 = k.sb("m8", [128, 8]); v8 = k.sb("v8", [128, 8])
        bo = [k.sb("pbo%d" % i, [128, 512]) for i in range(2)]; g_ = [k.sb("pg%d" % i, [128, 512]) for i in range(2)]
        yo = [k.sb("yo%d" % i, [128, 512]) for i in range(2)]
        n = 0
        for it in range(NT):
            y_ = yc[it % 2]
            k.dma("sp", y_[:], YT_d[:, it * 128:(it + 1) * 128, :], R=[YT_d], W=[y_])
            for h in range(8):
                k.op("pe", lambda e: e.transpose(py[:, h, :], y_[:, :, h], ident_f[:]), R=[y_, ident_f], W=[py])
            k.op("act", lambda e: e.copy(ytm[:], py[:]), R=[py], W=[ytm])
            for b in range(NB):
                i = b * NT + it
                bo_ = bo[n % 2]; gg = g_[n % 2]; yo_ = yo[n % 2]; n += 1
                k.dma("sp", bo_[:], BON_d[i * 128:(i + 1) * 128, :], R=[BON_d], W=[bo_])
                k.dma("sp", gg[:], G_d[i * 128:(i + 1) * 128, :], R=[G_d], W=[gg])
                ysl = ytm[:, :, b * 64:(b + 1) * 64]
                k.op("dve", lambda e: e.tensor_reduce(m8[:], ysl, axis=AX.X, op=ALU.add), R=[ytm], W=[m8])
                k.op("dve", lambda e: e.tensor_scalar(m8[:], m8[:], 1.0 / 64, None, op0=ALU.mult), R=[m8], W=[m8])
                k.op("dve", lambda e: e.tensor_tensor(yb[:], ysl, m8[:].unsqueeze(2).to_broadcast([128, 8, 64]), op=ALU.subtract), R=[ytm, m8], W=[yb])
                k.op("pool", lambda e: e.tensor_tensor(yq[:], yb[:], yb[:], op=ALU.mult), R=[yb], W=[yq])
                k.op("dve", lambda e: e.tensor_reduce(v8[:], yq[:], axis=AX.X, op=ALU.add), R=[yq], W=[v8])
                k.op("dve", lambda e: e.tensor_scalar(v8[:], v8[:], 1.0 / 64, 64e-5, op0=ALU.mult, op1=ALU.add), R=[v8], W=[v8])
                k.op("act", lambda e: e.sqrt(v8[:], v8[:]), R=[v8], W=[v8])
                k.op("dve", lambda e: e.reciprocal(v8[:], v8[:]), R=[v8], W=[v8])
                k.op("dve", lambda e: e.tensor_tensor(yb[:], yb[:], v8[:].unsqueeze(2).to_broadcast([128, 8, 64]), op=ALU.mult), R=[yb, v8], W=[yb])
                ybf = yb[:].rearrange("p h n -> p (h n)")
                k.op("dve", lambda e: e.tensor_tensor(yo_[:], ybf, lnw_b[:], op=ALU.mult), R=[yb, lnw_b], W=[yo_])
                k.op("pool", lambda e: e.tensor_tensor(yo_[:], yo_[:], lnb_b[:], op=ALU.add), R=[yo_, lnb_b], W=[yo_])
                k.op("pool", lambda e: e.tensor_tensor(yo_[:], yo_[:], bo_[:], op=ALU.add), R=[yo_, bo_], W=[yo_])
                k.op("dve", lambda e: e.tensor_tensor(yo_[:], yo_[:], gg[:], op=ALU.mult), R=[yo_, gg], W=[yo_])
                k.dma("sp", YCAT_d[i * 128:(i + 1) * 128, 0:512], yo_[:], R=[yo_], W=[YCAT_d])
    k.es = es
    k.regen()
    if stop_after <= 4:
        k.finish(list(outs.values()))
        return nc, es


    pos_d = inp("positions", [NTOK, 1], I32)
    qan_d = inp("q_a_norm", [1, 256]); wqb_d = inp("w_q_b", [256, 768]); kvan_d = inp("kv_a_norm", [1, 128])
    wkvb_d = inp("w_kv_b", [128, 1024]); qn_d = inp("q_norm", [1, 96]); kn_d = inp("k_norm", [1, 96])
    QT_d = outp("QT", [NB, 96, 8, T], BF16) if "QT" in dbg else k.dram("QT", [NB, 96, 8, T], BF16)
    KT_d = outp("KT", [NB, 96, 8, T], BF16) if "KT" in dbg else k.dram("KT", [NB, 96, 8, T], BF16)
    V_d = outp("V", [NB, T, 8, 66], BF16) if "V" in dbg else k.dram("V", [NB, T, 8, 66], BF16)
    TWO_PI = float(2 * np.pi)
    with ExitStack() as es5:
        k.es = es5
        def bc(name, src, n):
            t_ = k.sb(name, [128, n])
            k.dma("sp", t_[:], src[0:1, :].partition_broadcast(128), W=[t_])
            return t_
        qan_b = bc("qan_b", qan_d, 256); kvan_b = bc("kvan_b", kvan_d, 128); qn96 = bc("qn96", qn_d, 96); kn96 = bc("kn96", kn_d, 96)
        qn_b = k.sb("qn_b", [128, 8, 96]); kn_b = k.sb("kn_b", [128, 8, 96])
        k.op("dve", lambda e: e.tensor_copy(qn_b[:], qn96[:].unsqueeze(1).to_broadcast([128, 8, 96])), R=[qn96], W=[qn_b])
        k.op("dve", lambda e: e.tensor_copy(kn_b[:], kn96[:].unsqueeze(1).to_broadcast([128, 8, 96])), R=[kn96], W=[kn_b])
        wq_st = k.sb("wq_st", [128, 2, 768]); wq = k.sb("wq", [128, 2, 768], BF16)
        k.dma("sp", wq_st[:], wqb_d[:].rearrange("(c p) n -> p c n", p=128), W=[wq_st])
        k.op("dve", lambda e: e.tensor_copy(wq[:], wq_st[:]), R=[wq_st], W=[wq])
        wkv_st = k.sb("wkv_st", [128, 1024]); wkv = k.sb("wkv", [128, 1024], BF16)
        k.dma("sp", wkv_st[:], wkvb_d[:], W=[wkv_st])
        k.op("dve", lambda e: e.tensor_copy(wkv[:], wkv_st[:]), R=[wkv_st], W=[wkv])
        ji = k.sb("ji", [128, 16], I32); invf = k.sb("invf", [128, 16])
        k.op("pool", lambda e: e.iota(ji[:], pattern=[[1, 16]], base=0, channel_multiplier=0), W=[ji])
        k.op("dve", lambda e: e.tensor_copy(invf[:], ji[:]), R=[ji], W=[invf])
        k.op("act", lambda e: e.activation(invf[:], invf[:], ACT.Exp, scale=-float(np.log(10000.0) / 16)), R=[invf], W=[invf])
        um = [k.sb("um%d" % i, [128, 416]) for i in range(2)]
        posi = k.sb("posi", [128, 1], I32); posf = k.sb("posf", [128, 1])
        ang = k.sb("ang", [128, 32]); angi = k.sb("angi", [128, 32], I32); angf = k.sb("angf", [128, 32]); msk = k.sb("msk", [128, 32])
        cs = k.sb("cs", [128, 32])
        junkm = k.sb("junkm", [128, 256]); s1 = k.sb("s1", [128, 1]); s2 = k.sb("s2", [128, 1])
        qlb = k.sb("qlb", [128, 256], BF16); kvb = k.sb("kvb", [128, 128], BF16)
        qlT = k.sb("qlT", [128, 2, 128], BF16); kvT = k.sb("kvT", [128, 128], BF16)
        pq = k.ps("pq", [128, 2, 512]); pkv = k.ps("pkv", [128, 2, 512])
        ptr = k.ps("ptr", [128, 3, 128], BF16)
        ptq = k.ps("ptq", [96, 8, 128], BF16)
        qf = k.sb("qf", [128, 8, 96]); kf = k.sb("kf", [128, 8, 96]); sqt = k.sb("sqt", [128, 8, 96])
        r8 = k.sb("r8", [128, 8])
        ra = k.sb("ra", [128, 8, 16]); rb_ = k.sb("rb_", [128, 8, 16]); rc = k.sb("rc", [128, 8, 16]); rd_ = k.sb("rd_", [128, 8, 16])
        qfb = k.sb("qfb", [128, 8, 96], BF16)
        qTs = [k.sb("qTs%d" % i, [96, 8, 128], BF16) for i in range(2)]
        kTs = [k.sb("kTs%d" % i, [96, 8, 128], BF16) for i in range(2)]
        vs = [k.sb("vs%d" % i, [128, 8, 66], BF16) for i in range(2)]
        for vv_ in vs:
            k.op("pool", lambda e: e.memset(vv_[:, :, 64:66], 1.0), W=[vv_])

        def headnorm_rope(xf, gain_b, outT, dstT, b, it):
            k.op("pool", lambda e: e.tensor_tensor(sqt[:], xf[:], xf[:], op=ALU.mult), R=[xf], W=[sqt])
            k.op("dve", lambda e: e.tensor_reduce(r8[:], sqt[:], axis=AX.X, op=ALU.add), R=[sqt], W=[r8])
            k.op("dve", lambda e: e.tensor_scalar(r8[:], r8[:], 1.0 / 96, EPS, op0=ALU.mult, op1=ALU.add), R=[r8], W=[r8])
            k.op("act", lambda e: e.sqrt(r8[:], r8[:]), R=[r8], W=[r8])
            k.op("dve", lambda e: e.reciprocal(r8[:], r8[:]), R=[r8], W=[r8])
            k.op("dve", lambda e: e.tensor_tensor(xf[:], xf[:], r8[:].unsqueeze(2).to_broadcast([128, 8, 96]), op=ALU.mult), R=[xf, r8], W=[xf])
            k.op("dve", lambda e: e.tensor_tensor(xf[:], xf[:], gain_b[:], op=ALU.mult), R=[xf, gain_b], W=[xf])
            sinb = cs[:, 0:16].unsqueeze(1).to_broadcast([128, 8, 16]); cosb = cs[:, 16:32].unsqueeze(1).to_broadcast([128, 8, 16])
            x1 = xf[:, :, 64:80]; x2 = xf[:, :, 80:96]
            k.op("dve", lambda e: e.tensor_tensor(ra[:], x1, cosb, op=ALU.mult), R=[xf, cs], W=[ra])
            k.op("dve", lambda e: e.tensor_tensor(rb_[:], x2, sinb, op=ALU.mult), R=[xf, cs], W=[rb_])
            k.op("dve", lambda e: e.tensor_tensor(rc[:], x2, cosb, op=ALU.mult), R=[xf, cs], W=[rc])
            k.op("dve", lambda e: e.tensor_tensor(rd_[:], x1, sinb, op=ALU.mult), R=[xf, cs], W=[rd_])
            k.op("dve", lambda e: e.tensor_tensor(x1, ra[:], rb_[:], op=ALU.subtract), R=[ra, rb_], W=[xf])
            k.op("dve", lambda e: e.tensor_tensor(x2, rc[:], rd_[:], op=ALU.add), R=[rc, rd_], W=[xf])
            k.op("act", lambda e: e.copy(qfb[:], xf[:]), R=[xf], W=[qfb])
            for h in range(8):
                k.op("pe", lambda e: e.transpose(ptq[:, h, :], qfb[:, h, :], ident_b[:]), R=[qfb, ident_b], W=[ptq])
            k.op("act", lambda e: e.copy(outT[:], ptq[:]), R=[ptq], W=[outT])
            if "noQT" not in SKIP:
                k.dma("sp", dstT[b, :, :, it * 128:(it + 1) * 128], outT[:], R=[outT], W=[dstT])

        for i in range(NB * NT):
            b = i // NT; it = i % NT
            u_ = um[i % 2]
            k.dma("sp", u_[:], U_d[i * 128:(i + 1) * 128, 1696:2112], R=[U_d], W=[u_])
            k.dma("sp", posi[:], pos_d[i * 128:(i + 1) * 128, :], W=[posi])
            if "noROPE" not in SKIP:
                k.op("dve", lambda e: e.tensor_copy(posf[:], posi[:]), R=[posi], W=[posf])
                k.op("dve", lambda e: e.tensor_scalar(ang[:, 0:16], invf[:], posf[:], None, op0=ALU.mult), R=[invf, posf], W=[ang])
                k.op("dve", lambda e: e.tensor_scalar(ang[:, 16:32], ang[:, 0:16], float(np.pi / 2), None, op0=ALU.add), R=[ang], W=[ang])
                k.op("dve", lambda e: e.tensor_scalar(angf[:], ang[:], 1.0 / TWO_PI, None, op0=ALU.mult), R=[ang], W=[angf])
                k.op("dve", lambda e: e.tensor_copy(angi[:], angf[:]), R=[angf], W=[angi])
                k.op("dve", lambda e: e.tensor_copy(angf[:], angi[:]), R=[angi], W=[angf])
                k.op("dve", lambda e: e.scalar_tensor_tensor(ang[:], angf[:], -TWO_PI, ang[:], op0=ALU.mult, op1=ALU.add), R=[angf, ang], W=[ang])
                k.op("dve", lambda e: e.tensor_scalar(msk[:], ang[:], float(np.pi), -TWO_PI, op0=ALU.is_gt, op1=ALU.mult), R=[ang], W=[msk])
                k.op("dve", lambda e: e.tensor_tensor(ang[:], ang[:], msk[:], op=ALU.add), R=[ang, msk], W=[ang])
                k.op("dve", lambda e: e.tensor_scalar(msk[:], ang[:], -float(np.pi), TWO_PI, op0=ALU.is_lt, op1=ALU.mult), R=[ang], W=[msk])
                k.op("dve", lambda e: e.tensor_tensor(ang[:], ang[:], msk[:], op=ALU.add), R=[ang, msk], W=[ang])
                k.op("act", lambda e: e.activation(cs[:], ang[:], ACT.Sin), R=[ang], W=[cs])
            if "noLAT" not in SKIP:
                k.op("act", lambda e: e.activation(junkm[:, 0:256], u_[:, 0:256], ACT.Square, accum_out=s1[:]), R=[u_], W=[junkm, s1])
                k.op("dve", lambda e: e.tensor_scalar(s1[:], s1[:], 1.0 / 256, EPS, op0=ALU.mult, op1=ALU.add), R=[s1], W=[s1])
                k.op("act", lambda e: e.sqrt(s1[:], s1[:]), R=[s1], W=[s1])
                k.op("dve", lambda e: e.reciprocal(s1[:], s1[:]), R=[s1], W=[s1])
                k.op("dve", lambda e: e.scalar_tensor_tensor(qlb[:], u_[:, 0:256], s1[:], qan_b[:], op0=ALU.mult, op1=ALU.mult), R=[u_, s1, qan_b], W=[qlb])
                k.op("act", lambda e: e.activation(junkm[:, 0:128], u_[:, 256:384], ACT.Square, accum_out=s2[:]), R=[u_], W=[junkm, s2])
                k.op("dve", lambda e: e.tensor_scalar(s2[:], s2[:], 1.0 / 128, EPS, op0=ALU.mult, op1=ALU.add), R=[s2], W=[s2])
                k.op("act", lambda e: e.sqrt(s2[:], s2[:]), R=[s2], W=[s2])
                k.op("dve", lambda e: e.reciprocal(s2[:], s2[:]), R=[s2], W=[s2])
                k.op("dve", lambda e: e.scalar_tensor_tensor(kvb[:], u_[:, 256:384], s2[:], kvan_b[:], op0=ALU.mult, op1=ALU.mult), R=[u_, s2, kvan_b], W=[kvb])
                k.op("pe", lambda e: e.transpose(ptr[:, 0, :], qlb[:, 0:128], ident_b[:]), R=[qlb, ident_b], W=[ptr])
                k.op("pe", lambda e: e.transpose(ptr[:, 1, :], qlb[:, 128:256], ident_b[:]), R=[qlb, ident_b], W=[ptr])
                k.op("pe", lambda e: e.transpose(ptr[:, 2, :], kvb[:], ident_b[:]), R=[kvb, ident_b], W=[ptr])
                k.op("act", lambda e: e.copy(qlT[:], ptr[:, 0:2, :]), R=[ptr], W=[qlT])
                k.op("act", lambda e: e.copy(kvT[:], ptr[:, 2, :]), R=[ptr], W=[kvT])
                if "noA" not in SKIP:
                    for c in range(2):
                        k.op("pe", lambda e: e.matmul(pq[:, 0, :], qlT[:, c, :], wq[:, c, 0:512], start=(c == 0), stop=(c == 1)), R=[qlT, wq], W=[pq])
                    for c in range(2):
                        k.op("pe", lambda e: e.matmul(pq[:, 1, 0:256], qlT[:, c, :], wq[:, c, 512:768], start=(c == 0), stop=(c == 1)), R=[qlT, wq], W=[pq])
                    k.op("pe", lambda e: e.matmul(pkv[:, 0, :], kvT[:], wkv[:, 0:512], start=True, stop=True), R=[kvT, wkv], W=[pkv])
                    k.op("pe", lambda e: e.matmul(pkv[:, 1, :], kvT[:], wkv[:, 512:1024], start=True, stop=True), R=[kvT, wkv], W=[pkv])
                    qflat = qf[:].rearrange("p h d -> p (h d)")
                    k.op("act", lambda e: e.copy(qflat[:, 0:512], pq[:, 0, :]), R=[pq], W=[qf])
                    k.op("act", lambda e: e.copy(qflat[:, 512:768], pq[:, 1, 0:256]), R=[pq], W=[qf])
                    kv3 = pkv[:].rearrange("p c (h e) -> p (c h) e", e=128)
                    k.op("dve", lambda e: e.tensor_copy(kf[:, :, 0:64], kv3[:, :, 0:64]), R=[pkv], W=[kf])
                    k.op("dve", lambda e: e.tensor_copy(kf[:, :, 64:96], u_[:, 384:416].unsqueeze(1).to_broadcast([128, 8, 32])), R=[u_], W=[kf])
            v_ = vs[i % 2]
            if "noLAT" not in SKIP and "noA" not in SKIP and "noVC" not in SKIP:
                k.op("dve", lambda e: e.tensor_copy(v_[:, :, 0:64], kv3[:, :, 64:128]), R=[pkv], W=[v_])
            if "noV" not in SKIP:
                k.dma("sp", V_d[b, it * 128:(it + 1) * 128, :, :], v_[:], R=[v_], W=[V_d])
            if "noHN" not in SKIP:
                headnorm_rope(qf, qn_b, qTs[i % 2], QT_d, b, it)
                headnorm_rope(kf, kn_b, kTs[i % 2], KT_d, b, it)
    k.es = es
    k.regen()
    if stop_after <= 5:
        k.finish(list(outs.values()))
        return nc, es


    QS = min(4, NT)
    NJ = NT // QS
    QW = QS * 128
    with ExitStack() as es6:
        k.es = es6
        MASK = []
        mi = k.sb("mi", [128, QW], I32)
        for r_ in range(QS):
            m_ = k.sb("mask%d" % r_, [128, QW], BF16)
            k.op("pool", lambda e: e.iota(mi[:], pattern=[[1, QW]], base=-r_ * 128, channel_multiplier=-1), R=[], W=[mi])
            k.op("dve", lambda e: e.tensor_scalar(m_[:], mi[:], 0.0, None, op0=ALU.is_ge), R=[mi], W=[m_])
            MASK.append(m_)
        Vall = k.sb("Vall", [128, NT, 8, 66], BF16)
        QTs = [k.sb("QTs%d" % i, [96, T], BF16) for i in range(2)]
        KTs = [k.sb("KTs%d" % i, [96, T], BF16) for i in range(2)]
        pTs = [k.sb("pTs%d" % i, [128, QW], BF16) for i in range(3)]
        ps_ = [k.ps("ps%d" % i, [128, 512]) for i in range(2)]
        po = [k.ps("po%d" % i, [128, 512]) for i in range(QS)]
        rec = k.sb("rec", [128, 1])
        ym = [k.sb("ym%d" % i, [128, 64]) for i in range(4)]
        SC = float(96 ** -0.5)
        nbh = 0; npt = 0; nym = 0
        for b in range(NB):
            for kb in range(NT):
                k.dma("sp", Vall[:, kb, :, :], V_d[b, kb * 128:(kb + 1) * 128, :, :], R=[V_d], W=[Vall])
            for h in range(8):
                Q_ = QTs[nbh % 2]; K_ = KTs[nbh % 2]; nbh += 1
                k.dma("sp", Q_[:], QT_d[b, :, h, :], R=[QT_d], W=[Q_])
                k.dma("sp", K_[:], KT_d[b, :, h, :], R=[KT_d], W=[K_])
                for J in range(NJ):
                    nkb = QS * J + QS
                    for kb in range(nkb):
                        p_ = ps_[npt % 2]; pT = pTs[npt % 3]; npt += 1
                        k.op("pe", lambda e: e.matmul(p_[:, 0:QW], K_[:, kb * 128:(kb + 1) * 128], Q_[:, J * QW:(J + 1) * QW], start=True, stop=True),
                             R=[K_, Q_], W=[p_])
                        k.op("act", lambda e: e.activation(pT[:], p_[:, 0:QW], ACT.Exp, scale=SC), R=[p_], W=[pT])
                        r_ = kb - QS * J
                        if r_ >= 0:
                            k.op("dve", lambda e: e.tensor_tensor(pT[:], pT[:], MASK[r_][:], op=ALU.mult), R=[pT, MASK[r_]], W=[pT])
                        for qb in range(QS):
                            if QS * J + qb >= kb:
                                k.op("pe", lambda e: e.matmul(po[qb][:, 0:65], pT[:, qb * 128:(qb + 1) * 128], Vall[:, kb, h, 0:65],
                                                              start=(kb == 0), stop=(kb == QS * J + qb)), R=[pT, Vall], W=[po[qb]])
                    for qb in range(QS):
                        y_ = ym[nym % 4]; nym += 1
                        k.op("dve", lambda e: e.reciprocal(rec[:], po[qb][:, 64:65]), R=[po[qb]], W=[rec])
                        k.op("dve", lambda e: e.tensor_scalar(y_[:], po[qb][:, 0:64], rec[:], None, op0=ALU.mult), R=[po[qb], rec], W=[y_])
                        i = b * NT + QS * J + qb
                        k.dma("sp", YCAT_d[i * 128:(i + 1) * 128, 512 + h * 64:512 + (h + 1) * 64], y_[:], R=[y_], W=[YCAT_d])
    k.es = es
    k.regen()
    if stop_after <= 6:
        k.finish(list(outs.values()))
        return nc, es


    NE = 256
    BLK = 128
    NBLK = (NTOK * 8) // BLK + NE
    NROWS = NBLK * BLK
    w_out_d = inp("w_out", [D, D]); norm_ffn_d = inp("norm_ffn", [1, D]); w_router_d = inp("w_router", [D, NE]); rbias_d = inp("router_bias", [1, NE])
    X1_d = outp("X1", [NTOK, D]) if "X1" in dbg else k.dram("X1", [NTOK, D])
    H2T_d = outp("H2T", [128, 8, NTOK], BF16) if "H2T" in dbg else k.dram("H2T", [128, 8, NTOK], BF16)
    XBUF_d = k.dram("XBUF", [NROWS, D], BF16)
    YBUF_d = k.dram("YBUF", [NROWS, D], BF16)
    H2B_d = k.dram("H2B", [NTOK, D], BF16)
    dbgH2 = outp("H2", [NTOK, D]) if "H2" in dbg else None
    dbgG = outp("GATE", [NTOK, NE]) if "GATE" in dbg else None
    dbgDW = outp("DW", [NTOK, 16]) if "DW" in dbg else None
    NTT = NB * NT
    BREG = nc.gpsimd.to_reg(NROWS - 1)
    DESTI = k.sb("DESTI", [128, NTT, 8], I32)
    WK = k.sb("WK", [128, NTT, 8])
    IDXG = k.sb("IDXG", [128, NBLK, 8], I32)
    IDXD = k.sb("IDXD", [128, NBLK, 2], I32)
    with ExitStack() as es7:
        k.es = es7
        nfb = k.sb("nfb", [128, D])
        k.dma("sp", nfb[:], norm_ffn_d[0:1, :].partition_broadcast(128), W=[nfb])
        for b in range(NB):
            m = MOD[b][4]
            k.op("dve", lambda e: e.scalar_tensor_tensor(m[:], m[:], 1.0, nfb[:], op0=ALU.add, op1=ALU.mult), R=[m, nfb], W=[m])
        rb_b = k.sb("rb_b", [128, NE])
        k.dma("sp", rb_b[:], rbias_d[0:1, :].partition_broadcast(128), W=[rb_b])
        wo = k.sb("wo", [128, 8, D], BF16); wr = k.sb("wr", [128, 8, NE])
        wst = [k.sb("wost%d" % i, [128, D]) for i in range(2)]
        for j in range(8):
            s_ = wst[j % 2]
            k.dma("sp", s_[:], w_out_d[j * 128:(j + 1) * 128, :], W=[s_])
            k.op("pool" if j % 2 else "dve", lambda e: e.tensor_copy(wo[:, j, :], s_[:]), R=[s_], W=[wo])
        k.dma("sp", wr[:], w_router_d[:].rearrange("(j p) n -> p j n", p=128), W=[wr])
        UT = k.sb("UT", [128, 128], BF16); ONESB = k.sb("ONESB", [128, 128], BF16); ones256 = k.sb("ones256", [128, NE])
        uti = k.sb("uti", [128, 128], I32)
        k.op("pool", lambda e: e.iota(uti[:], pattern=[[1, 128]], base=0, channel_multiplier=-1), W=[uti])
        k.op("dve", lambda e: e.tensor_scalar(UT[:], uti[:], 0.0, None, op0=ALU.is_gt), R=[uti], W=[UT])
        k.op("dve", lambda e: e.memset(ONESB[:], 1.0), W=[ONESB])
        k.op("dve", lambda e: e.memset(ones256[:], 1.0), W=[ones256])
        ecap_i = k.sb("ecap_i", [128, NE], I32); eidx = k.sb("eidx", [128, NE])
        k.op("pool", lambda e: e.iota(ecap_i[:], pattern=[[1, NE]], base=0, channel_multiplier=0), W=[ecap_i])
        k.op("dve", lambda e: e.tensor_copy(eidx[:], ecap_i[:]), R=[ecap_i], W=[eidx])
        EK = k.sb("EK", [128, NTT, 8]); RK = k.sb("RK", [128, NTT, 8])
        carry = k.sb("carry", [128, NE])
        k.op("dve", lambda e: e.memset(carry[:], 0.0), W=[carry])
        yc_ = [k.sb("ycat%d" % i, [128, D]) for i in range(2)]
        ycb = k.sb("ycb", [128, D], BF16); ycT = k.sb("ycT", [128, 8, 128], BF16)
        xt = [k.sb("x3_%d" % i, [128, D]) for i in range(2)]
        x1 = [k.sb("x1_%d" % i, [128, D]) for i in range(2)]
        junk = k.sb("junk3", [128, D]); ssq = k.sb("ssq3", [128, 1])
        h2 = k.sb("h2", [128, D]); h2b = [k.sb("h2b%d" % i, [128, D], BF16) for i in range(2)]
        h2T = k.sb("h2T", [128, 8, 128]); h2Tb = [k.sb("h2Tb%d" % i, [128, 8, 128], BF16) for i in range(2)]
        pT3 = k.ps("pT3", [128, 8, 128], BF16); pTf = k.ps("pTf", [128, 8, 128])
        pp = k.ps("pp", [128, 2, 512]); plg = k.ps("plg", [128, NE]); prk = k.ps("prk", [128, 2, NE])
        sc = k.sb("sc", [128, NE]); sel = k.sb("sel", [128, NE]); selm = k.sb("selm", [128, NE])
        m8g = k.sb("m8g", [128, 8, 8]); gs = k.sb("gs", [128, 8]); gm8 = k.sb("gm8", [128, 8]); gmask = k.sb("gmask", [128, 8]); em8 = k.sb("em8", [128, 8])
        emask = k.sb("emask", [128, NE]); emb = k.sb("emb", [128, NE], BF16); Gm = k.sb("Gm", [128, NE]); wsum = k.sb("wsum", [128, 1])
        pos = k.sb("pos", [128, NE]); dfull = k.sb("dfull", [128, NE]); ovf = k.sb("ovf", [128, NE]); slot = k.sb("slot", [128, NE])
        junk2 = k.sb("junk2", [128, NE]); destf = k.sb("destf", [128, 8])
        for i in range(NTT):
            b = i // NT
            y_ = yc_[i % 2]; x_ = xt[i % 2]; x1_ = x1[i % 2]; hb_ = h2b[i % 2]; hTb_ = h2Tb[i % 2]
            k.dma("sp", y_[:], YCAT_d[i * 128:(i + 1) * 128, :], R=[YCAT_d], W=[y_])
            k.dma("sp", x_[:], x_d[i * 128:(i + 1) * 128, :], W=[x_])
            k.op("act", lambda e: e.copy(ycb[:], y_[:]), R=[y_], W=[ycb])
            for j in range(8):
                k.op("pe", lambda e: e.transpose(pT3[:, j, :], ycb[:, j * 128:(j + 1) * 128], ident_b[:]), R=[ycb, ident_b], W=[pT3])
            k.op("act", lambda e: e.copy(ycT[:], pT3[:]), R=[pT3], W=[ycT])
            for hh in range(2):
                for j in range(8):
                    k.op("pe", lambda e: e.matmul(pp[:, hh, :], ycT[:, j, :], wo[:, j, hh * 512:(hh + 1) * 512], start=(j == 0), stop=(j == 7)), R=[ycT, wo], W=[pp])
            ppf = pp[:].rearrange("p a n -> p (a n)")
            k.op("dve", lambda e: e.tensor_tensor(x1_[:], ppf, MOD[b][2][:], op=ALU.mult), R=[pp, MOD[b][2]], W=[x1_])
            k.op("pool", lambda e: e.tensor_tensor(x1_[:], x1_[:], x_[:], op=ALU.add), R=[x1_, x_], W=[x1_])
            k.dma("sp", X1_d[i * 128:(i + 1) * 128, :], x1_[:], R=[x1_], W=[X1_d])
            k.op("act", lambda e: e.activation(junk[:], x1_[:], ACT.Square, accum_out=ssq[:]), R=[x1_], W=[junk, ssq])
            k.op("dve", lambda e: e.tensor_scalar(ssq[:], ssq[:], 1.0 / D, EPS, op0=ALU.mult, op1=ALU.add), R=[ssq], W=[ssq])
            k.op("act", lambda e: e.sqrt(ssq[:], ssq[:]), R=[ssq], W=[ssq])
            k.op("dve", lambda e: e.reciprocal(ssq[:], ssq[:]), R=[ssq], W=[ssq])
            k.op("dve", lambda e: e.scalar_tensor_tensor(h2[:], x1_[:], ssq[:], MOD[b][4][:], op0=ALU.mult, op1=ALU.mult), R=[x1_, ssq, MOD[b][4]], W=[h2])
            k.op("pool", lambda e: e.tensor_tensor(h2[:], h2[:], MOD[b][3][:], op=ALU.add), R=[h2, MOD[b][3]], W=[h2])
            k.op("act", lambda e: e.copy(hb_[:], h2[:]), R=[h2], W=[hb_])
            if dbgH2 is not None:
                k.dma("sp", dbgH2[i * 128:(i + 1) * 128, :], h2[:], R=[h2], W=[dbgH2])
            for j in range(8):
                k.op("pe", lambda e: e.transpose(pTf[:, j, :], h2[:, j * 128:(j + 1) * 128], ident_f[:]), R=[h2, ident_f], W=[pTf])
            k.op("dve", lambda e: e.tensor_copy(h2T[:], pTf[:]), R=[pTf], W=[h2T])
            k.op("pool", lambda e: e.tensor_copy(hTb_[:], h2T[:]), R=[h2T], W=[hTb_])
            if "noH2T" not in SKIP:
                k.dma("sp", H2T_d[:, :, i * 128:(i + 1) * 128], hTb_[:], R=[hTb_], W=[H2T_d])
            if "noRT" not in SKIP:
                for j in range(8):
                    k.op("pe", lambda e: e.matmul(plg[:], h2T[:, j, :], wr[:, j, :], start=(j == 0), stop=(j == 7)), R=[h2T, wr], W=[plg])
                k.op("act", lambda e: e.activation(sc[:], plg[:], ACT.Sigmoid), R=[plg], W=[sc])
                k.op("dve", lambda e: e.tensor_tensor(sel[:], sc[:], rb_b[:], op=ALU.add), R=[sc, rb_b], W=[sel])
                for g in range(8):
                    k.op("dve", lambda e: e.max(m8g[:, g, :], sel[:, g * 32:(g + 1) * 32]), R=[sel], W=[m8g])
                k.op("dve", lambda e: e.tensor_tensor(gs[:], m8g[:, :, 0], m8g[:, :, 1], op=ALU.add), R=[m8g], W=[gs])
                k.op("dve", lambda e: e.max(gm8[:], gs[:]), R=[gs], W=[gm8])
                k.op("dve", lambda e: e.tensor_scalar(gmask[:], gs[:], gm8[:, 3:4], None, op0=ALU.is_ge), R=[gs, gm8], W=[gmask])
                k.op("dve", lambda e: e.scalar_tensor_tensor(selm[:].rearrange("p (g n) -> p g n", g=8), sel[:].rearrange("p (g n) -> p g n", g=8), 2.0,
                                                             gmask[:].unsqueeze(2).to_broadcast([128, 8, 32]), op0=ALU.add, op1=ALU.mult), R=[sel, gmask], W=[selm])
                k.op("dve", lambda e: e.max(em8[:], selm[:]), R=[selm], W=[em8])
                k.op("dve", lambda e: e.tensor_scalar(emask[:], selm[:], em8[:, 7:8], None, op0=ALU.is_ge), R=[selm, em8], W=[emask])
                k.op("act", lambda e: e.copy(emb[:], emask[:]), R=[emask], W=[emb])
                k.op("dve", lambda e: e.scalar_tensor_tensor(Gm[:], sc[:], 1.0, emask[:], op0=ALU.mult, op1=ALU.mult, accum_out=wsum[:]), R=[sc, emask], W=[Gm, wsum])
                k.op("dve", lambda e: e.reciprocal(wsum[:], wsum[:]), R=[wsum], W=[wsum])
                k.op("dve", lambda e: e.tensor_scalar(Gm[:], Gm[:], wsum[:], 2.5, op0=ALU.mult, op1=ALU.mult), R=[Gm, wsum], W=[Gm])
                if dbgG is not None:
                    k.dma("sp", dbgG[i * 128:(i + 1) * 128, :], Gm[:], R=[Gm], W=[dbgG])
            if "noRT" not in SKIP and "noRK" not in SKIP:
                k.op("pe", lambda e: e.matmul(prk[:, 0, :], UT[:], emb[:], start=True, stop=True), R=[UT, emb], W=[prk])
                k.op("pe", lambda e: e.matmul(prk[:, 1, :], ONESB[:], emb[:], start=True, stop=True), R=[ONESB, emb], W=[prk])
                k.op("dve", lambda e: e.tensor_tensor(pos[:], prk[:, 0, :], carry[:], op=ALU.add), R=[prk, carry], W=[pos])
                k.op("dve", lambda e: e.tensor_tensor(carry[:], prk[:, 1, :], carry[:], op=ALU.add), R=[prk, carry], W=[carry])
                k.op("dve", lambda e: e.tensor_tensor_scan(slot[:], ones256[:], emask[:], 0.0, op0=ALU.mult, op1=ALU.add), R=[ones256, emask], W=[slot])
                k.op("dve", lambda e: e.tensor_tensor(slot[:], slot[:], emask[:], op=ALU.mult), R=[slot, emask], W=[slot])
                for ks in range(8):
                    k.op("dve", lambda e: e.scalar_tensor_tensor(junk2[:], slot[:], float(ks + 1), eidx[:], op0=ALU.is_equal, op1=ALU.mult, accum_out=EK[:, i, ks:ks + 1]),
                         R=[slot, eidx], W=[junk2, EK])
                    k.op("dve", lambda e: e.scalar_tensor_tensor(junk2[:], slot[:], float(ks + 1), pos[:], op0=ALU.is_equal, op1=ALU.mult, accum_out=RK[:, i, ks:ks + 1]),
                         R=[slot, pos], W=[junk2, RK])
                    k.op("dve", lambda e: e.scalar_tensor_tensor(junk2[:], slot[:], float(ks + 1), Gm[:], op0=ALU.is_equal, op1=ALU.mult, accum_out=WK[:, i, ks:ks + 1]),
                         R=[slot, Gm], W=[junk2, WK])
            k.dma("sp", H2B_d[i * 128:(i + 1) * 128, :], hb_[:], R=[hb_], W=[H2B_d])
        cnt_i = k.sb("cnt_i", [128, NE], I32); padc = k.sb("padc", [128, NE]); pend = k.sb("pend", [128, NE]); pstart = k.sb("pstart", [128, NE])
        k.op("dve", lambda e: e.tensor_scalar(padc[:], carry[:], float(BLK - 1), None, op0=ALU.add), R=[carry], W=[padc])
        k.op("dve", lambda e: e.tensor_copy(cnt_i[:], padc[:]), R=[padc], W=[cnt_i])
        k.op("dve", lambda e: e.tensor_scalar(cnt_i[:], cnt_i[:], 7, 7, op0=ALU.arith_shift_right, op1=ALU.logical_shift_left), R=[cnt_i], W=[cnt_i])
        k.op("dve", lambda e: e.tensor_copy(padc[:], cnt_i[:]), R=[cnt_i], W=[padc])
        k.op("dve", lambda e: e.tensor_tensor_scan(pend[:], ones256[:], padc[:], 0.0, op0=ALU.mult, op1=ALU.add), R=[ones256, padc], W=[pend])
        k.op("dve", lambda e: e.tensor_tensor(pstart[:], pend[:], padc[:], op=ALU.subtract), R=[pend, padc], W=[pstart])
        bexp = k.sb("bexp", [128, NBLK])
        for j in range(NBLK):
            k.op("dve", lambda e: e.tensor_scalar(junk2[:], pend[:], float(BLK * j), 0.0, op0=ALU.is_le, op1=ALU.add, accum_out=bexp[:, j:j + 1]), R=[pend], W=[junk2, bexp])
        k.op("dve", lambda e: e.tensor_scalar(bexp[:], bexp[:], float(NE - 1), None, op0=ALU.min), R=[bexp], W=[bexp])
        bgi = k.sb("bgi", [128, 8], I32); bgf = k.sb("bgf", [128, 8])
        k.op("pool", lambda e: e.iota(bgi[:], pattern=[[128, 8]], base=0, channel_multiplier=1), W=[bgi])
        k.op("dve", lambda e: e.tensor_copy(bgf[:], bgi[:]), R=[bgi], W=[bgf])
        JC = 96
        idxf = k.sb("idxf", [128, JC, 8])
        for j0 in range(0, NBLK, JC):
            jn = min(JC, NBLK - j0)
            k.op("dve", lambda e: e.scalar_tensor_tensor(idxf[:, 0:jn, :], bexp[:, j0:j0 + jn].unsqueeze(2).to_broadcast([128, jn, 8]), 1024.0,
                                                         bgf[:].unsqueeze(1).to_broadcast([128, jn, 8]), op0=ALU.mult, op1=ALU.add), R=[bexp, bgf], W=[idxf])
            k.op("dve", lambda e: e.tensor_copy(IDXG[:, j0:j0 + jn, :], idxf[:, 0:jn, :]), R=[idxf], W=[IDXG])
            k.op("dve", lambda e: e.scalar_tensor_tensor(idxf[:, 0:jn, 0:2], bexp[:, j0:j0 + jn].unsqueeze(2).to_broadcast([128, jn, 2]), 256.0,
                                                         bgf[:, 0:2].unsqueeze(1).to_broadcast([128, jn, 2]), op0=ALU.mult, op1=ALU.add), R=[bexp, bgf], W=[idxf])
            k.op("dve", lambda e: e.tensor_copy(IDXD[:, j0:j0 + jn, :], idxf[:, 0:jn, 0:2]), R=[idxf], W=[IDXD])
        if "BEXP" in dbg:
            o = outp("BEXP", [1, NBLK]); k.dma("sp", o[0:1, :], bexp[0:1, :], R=[bexp], W=[o])
            o2 = outp("PSTART", [1, NE]); k.dma("sp", o2[0:1, :], pstart[0:1, :], R=[pstart], W=[o2])
        hbb = [k.sb("hbb%d" % i, [128, D], BF16) for i in range(2)]
        for i in range(NTT):
            hb_ = hbb[i % 2]
            k.dma("sp", hb_[:], H2B_d[i * 128:(i + 1) * 128, :], R=[H2B_d], W=[hb_])
            for ks in range(8):
                k.op("dve", lambda e: e.scalar_tensor_tensor(junk2[:], eidx[:], EK[:, i, ks:ks + 1], pstart[:], op0=ALU.is_equal, op1=ALU.mult, accum_out=destf[:, ks:ks + 1]),
                     R=[eidx, EK, pstart], W=[junk2, destf])
            k.op("dve", lambda e: e.tensor_tensor(destf[:], destf[:], RK[:, i, :], op=ALU.add), R=[destf, RK], W=[destf])
            k.op("dve", lambda e: e.tensor_copy(DESTI[:, i, :], destf[:]), R=[destf], W=[DESTI])
            if dbgDW is not None:
                k.dma("sp", dbgDW[i * 128:(i + 1) * 128, 0:8], destf[:], R=[destf], W=[dbgDW])
                k.dma("sp", dbgDW[i * 128:(i + 1) * 128, 8:16], WK[:, i, :], R=[WK], W=[dbgDW])
            for ks in range(8):
                k.idma(out=XBUF_d[:, :], out_offset=bass.IndirectOffsetOnAxis(ap=DESTI[:, i, ks:ks + 1], axis=0), in_=hb_[:, :], in_offset=None,
                       bounds_check=BREG, oob_is_err=False, R=[hb_, DESTI], W=[XBUF_d])
        if True:
            o = outp("CNT", [1, NE])
            k.dma("sp", o[0:1, :], carry[0:1, :], R=[carry], W=[o])
    k.es = es
    k.regen()
    if stop_after <= 7:
        k.finish(list(outs.values()))
        return nc, es


    wegu_d = inp("w_e_gate_up", [NE * D, 512]); wed_d = inp("w_e_down", [NE * 256, D])
    NBX = int(os.environ.get("NBX", str(NBLK)))
    with ExitStack() as es8:
        k.es = es8
        gst = [k.sb("gst%d" % i, [128, 8, 512]) for i in range(2)]
        dst_ = [k.sb("dst%d" % i, [128, 2, D]) for i in range(2)]
        wgu = [k.sb("wgu%d" % i, [128, 8, 512], BF16) for i in range(2)]
        wd = [k.sb("wd%d" % i, [128, 2, D], BF16) for i in range(2)]
        xin = [k.sb("xin%d" % i, [128, D], BF16) for i in range(3)]
        xT = [k.sb("xT%d" % i, [128, 8, BLK], BF16) for i in range(2)]
        sg = [k.sb("sg%d" % i, [128, BLK]) for i in range(2)]
        actT = [k.sb("actT%d" % i, [128, 2, BLK], BF16) for i in range(2)]
        yo = [k.sb("yo4_%d" % i, [128, D], BF16) for i in range(3)]
        pTx = k.ps("pTx", [128, 8, 128], BF16)
        pu = [k.ps("pu4_%d" % i, [128, 512]) for i in range(4)]
        pd = k.ps("pd", [128, 2, 512])
        for jb in range(NBX):
            g_ = gst[jb % 2]; d_ = dst_[jb % 2]; wg = wgu[jb % 2]; wd_ = wd[jb % 2]; xT_ = xT[jb % 2]; aT = actT[jb % 2]
            xi = xin[jb % 3]; y_ = yo[jb % 3]
            for c in range(8):
                k.idma(out=g_[:, c, :], out_offset=None, in_=wegu_d[:, :], in_offset=bass.IndirectOffsetOnAxis(ap=IDXG[:, jb, c:c + 1], axis=0),
                       R=[wegu_d, IDXG], W=[g_])
            for c in range(2):
                k.idma(out=d_[:, c, :], out_offset=None, in_=wed_d[:, :], in_offset=bass.IndirectOffsetOnAxis(ap=IDXD[:, jb, c:c + 1], axis=0),
                       R=[wed_d, IDXD], W=[d_])
            k.dma("sp", xi[:], XBUF_d[jb * BLK:(jb + 1) * BLK, :], R=[XBUF_d], W=[xi])
            k.op("dve", lambda e: e.tensor_copy(wg[:, 0:5, :], g_[:, 0:5, :]), R=[g_], W=[wg])
            k.op("act", lambda e: e.copy(wg[:, 5:8, :], g_[:, 5:8, :]), R=[g_], W=[wg])
            k.op("act", lambda e: e.copy(wd_[:], d_[:]), R=[d_], W=[wd_])
            for j in range(8):
                k.op("pe", lambda e: e.transpose(pTx[:, j, :], xi[:, j * 128:(j + 1) * 128], ident_b[:]), R=[xi, ident_b], W=[pTx])
            k.op("act", lambda e: e.copy(xT_[:], pTx[:]), R=[pTx], W=[xT_])
            for c in range(4):
                for j in range(8):
                    k.op("pe", lambda e: e.matmul(pu[c][:, 0:BLK], wg[:, j, c * 128:(c + 1) * 128], xT_[:, j, :], start=(j == 0), stop=(j == 7)), R=[wg, xT_], W=[pu[c]])
            for c in range(2):
                s_ = sg[c]
                k.op("act", lambda e: e.activation(s_[:], pu[c][:, 0:BLK], ACT.Silu), R=[pu[c]], W=[s_])
                k.op("dve", lambda e: e.tensor_tensor(aT[:, c, :], s_[:], pu[2 + c][:, 0:BLK], op=ALU.mult), R=[s_, pu[2 + c]], W=[aT])
            for hh in range(2):
                for c in range(2):
                    k.op("pe", lambda e: e.matmul(pd[:, hh, :], aT[:, c, :], wd_[:, c, hh * 512:(hh + 1) * 512], start=(c == 0), stop=(c == 1)), R=[aT, wd_], W=[pd])
            k.op("dve", lambda e: e.tensor_copy(y_[:], pd[:].rearrange("p a n -> p (a n)")), R=[pd], W=[y_])
            k.dma("sp", YBUF_d[jb * BLK:(jb + 1) * BLK, :], y_[:], R=[y_], W=[YBUF_d])
    k.es = es
    k.regen()
    if stop_after <= 8:
        k.finish(list(outs.values()))
        return nc, es

    wsgu_d = inp("w_sh_gate_up", [D, 512]); wsd_d = inp("w_sh_down", [256, D])
    OUT_d = outp("out", [NTOK, D])
    TB5 = min(4, NTT)
    with ExitStack() as es9:
        k.es = es9
        gst = k.sb("sgst", [128, 8, 512]); dst_ = k.sb("sdst", [128, 2, D])
        wg = k.sb("swgu", [128, 8, 512], BF16); wd_ = k.sb("swd", [128, 2, D], BF16)
        k.dma("sp", gst[:], wsgu_d[:].rearrange("(j p) n -> p j n", p=128), W=[gst])
        k.dma("sp", dst_[:], wsd_d[:].rearrange("(c p) n -> p c n", p=128), W=[dst_])
        k.op("dve", lambda e: e.tensor_copy(wg[:], gst[:]), R=[gst], W=[wg])
        k.op("pool", lambda e: e.tensor_copy(wd_[:], dst_[:]), R=[dst_], W=[wd_])
        xTs = [k.sb("xTs%d" % i, [128, 8, TB5 * 128], BF16) for i in range(2)]
        sg = [k.sb("sg5_%d" % i, [128, TB5 * 128]) for i in range(2)]
        aT5 = [k.sb("aT5_%d" % i, [128, 2, TB5 * 128], BF16) for i in range(2)]
        yg = [k.sb("yg%d" % i, [128, D], BF16) for i in range(4)]
        for y_ in yg:
            k.op("pool", lambda e: e.memset(y_[:], 0.0), W=[y_])
        acc = [k.sb("acc%d" % i, [128, D]) for i in range(2)]
        x1t = [k.sb("x1t%d" % i, [128, D]) for i in range(2)]
        pu = [k.ps("pu5_%d" % i, [128, 512]) for i in range(4)]
        pd = [k.ps("pd5_%d" % i, [128, 2, 512]) for i in range(2)]
        nyg = 0
        for blk in range(NTT // TB5):
            xT_ = xTs[blk % 2]; aT = aT5[blk % 2]
            W5 = TB5 * 128
            k.dma("sp", xT_[:], H2T_d[:, :, blk * W5:(blk + 1) * W5], R=[H2T_d], W=[xT_])
            for c in range(4):
                for j in range(8):
                    k.op("pe", lambda e: e.matmul(pu[c][:, 0:W5], wg[:, j, c * 128:(c + 1) * 128], xT_[:, j, :], start=(j == 0), stop=(j == 7)), R=[wg, xT_], W=[pu[c]])
            for c in range(2):
                s_ = sg[c]
                k.op("act", lambda e: e.activation(s_[:], pu[c][:, 0:W5], ACT.Silu), R=[pu[c]], W=[s_])
                k.op("dve", lambda e: e.tensor_tensor(aT[:, c, :], s_[:], pu[2 + c][:, 0:W5], op=ALU.mult), R=[s_, pu[2 + c]], W=[aT])
            for tt in range(TB5):
                i = blk * TB5 + tt; b = i // NT
                pd_ = pd[i % 2]; a_ = acc[i % 2]; x1_ = x1t[i % 2]
                for hh in range(2):
                    for c in range(2):
                        k.op("pe", lambda e: e.matmul(pd_[:, hh, :], aT[:, c, tt * 128:(tt + 1) * 128], wd_[:, c, hh * 512:(hh + 1) * 512], start=(c == 0), stop=(c == 1)), R=[aT, wd_], W=[pd_])
                k.dma("sp", x1_[:], X1_d[i * 128:(i + 1) * 128, :], R=[X1_d], W=[x1_])
                k.op("act", lambda e: e.copy(a_[:], pd_[:].rearrange("p a n -> p (a n)")), R=[pd_], W=[a_])
                for ks in range(8):
                    y_ = yg[nyg % 4]; nyg += 1
                    k.idma(out=y_[:, :], out_offset=None, in_=YBUF_d[:, :], in_offset=bass.IndirectOffsetOnAxis(ap=DESTI[:, i, ks:ks + 1], axis=0),
                           bounds_check=BREG, oob_is_err=False, R=[YBUF_d, DESTI], W=[y_])
                    k.op("dve", lambda e: e.scalar_tensor_tensor(a_[:], y_[:], WK[:, i, ks:ks + 1], a_[:], op0=ALU.mult, op1=ALU.add), R=[y_, WK, a_], W=[a_])
                k.op("dve", lambda e: e.tensor_tensor(a_[:], a_[:], MOD[b][5][:], op=ALU.mult), R=[a_, MOD[b][5]], W=[a_])
                k.op("pool", lambda e: e.tensor_tensor(a_[:], a_[:], x1_[:], op=ALU.add), R=[a_, x1_], W=[a_])
                k.dma("sp", OUT_d[i * 128:(i + 1) * 128, :], a_[:], R=[a_], W=[OUT_d])
    k.es = es
    k.regen()
    k.finish(list(outs.values()))
    return nc, es


_CACHE = {}


def kernel(**inputs):
    T = 4096
    ncores = 8
    if "nc" not in _CACHE:
        _CACHE["nc"] = build(T)
    nc, es = _CACHE["nc"]
    names = [a.memorylocations[0].name for a in nc.allocations
             if hasattr(a, "kind") and a.kind == "ExternalInput" and a.memorylocations[0].name != "partition_id"]
    shared = {}
    for n in names:
        if n in ("x", "c", "positions"):
            continue
        a = np.asarray(inputs[n])[0]
        if n == "rwkv_r_k":
            a = a.reshape(1, 512)
        elif n == "w_e_gate_up":
            a = a.reshape(256 * 1024, 512)
        elif n == "w_e_down":
            a = a.reshape(256 * 256, 1024)
        elif a.ndim == 1:
            a = a.reshape(1, -1)
        shared[n] = np.ascontiguousarray(a)
    x = np.asarray(inputs["x"]); c = np.asarray(inputs["c"]); pos = np.asarray(inputs["positions"])
    in_maps = []
    for cid in range(ncores):
        m = dict(shared)
        m["x"] = np.ascontiguousarray(x[2 * cid:2 * cid + 2].reshape(2 * T, 1024))
        m["c"] = np.ascontiguousarray(c[2 * cid:2 * cid + 2])
        m["positions"] = np.ascontiguousarray(pos[2 * cid:2 * cid + 2].reshape(2 * T, 1).astype(np.int32))
        in_maps.append(m)
    res = run_bass_kernel_spmd(nc, in_maps, core_ids=list(range(ncores)))
    try:
        print("max expert count per core:", [int(r["CNT"].max()) for r in res.results], flush=True)
    except Exception:
        pass
    out = np.stack([r["out"].reshape(2, T, 1024) for r in res.results], 0).reshape(16, T, 1024)
    return out.astype(np.float32)
```

```python
import os
import numpy as np
from contextlib import ExitStack
import concourse.bass as bass
import concourse.mybir as mybir
from concourse.bass_utils import run_bass_kernel_spmd


F32 = mybir.dt.float32
F32R = mybir.dt.float32r
BF16 = mybir.dt.bfloat16
I32 = mybir.dt.int32
U32 = mybir.dt.uint32
ACT = mybir.ActivationFunctionType
ALU = mybir.AluOpType
AX = mybir.AxisListType


class Buf:
    __slots__ = ("t", "lw", "rd", "name")

    def __init__(self, t, name=""):
        self.t = t
        self.lw = None
        self.rd = {}
        self.name = name

    def __getitem__(self, k):
        return self.t[k]


class K:
    def __init__(self, nc, es, ndma=24):
        self.nc = nc
        self.es = es
        self.eng = {"pe": nc.tensor, "act": nc.scalar, "dve": nc.vector, "pool": nc.gpsimd, "sp": nc.sync}
        self.sem = {}
        self.cnt = {}
        for n in ("pe", "act", "dve", "pool"):
            self.sem[n] = es.enter_context(nc.semaphore("s_" + n))
            self.cnt[n] = 0
        self.ndma = ndma
        self.dsem = [es.enter_context(nc.semaphore("s_dma%d" % i)) for i in range(ndma)]
        self.dcnt = [0] * ndma
        self.dnext = 0
        self.waited = {n: {} for n in ("pe", "act", "dve", "pool", "sp")}
        self.nins = 0
        self.imax = 6
        self.gen = 0
        self.top_es = es

    def sb(self, name, shape, dt=F32):
        return Buf(self.es.enter_context(self.nc.sbuf_tensor(name, list(shape), dt)), name)

    def ps(self, name, shape, dt=F32):
        return Buf(self.es.enter_context(self.nc.psum_tensor(name, list(shape), dt)), name)

    def dram(self, name, shape, dt=F32, kind="Internal"):
        return Buf(self.nc.dram_tensor(name, list(shape), dt, kind=kind).ap(), name)

    def _semof(self, key):
        return self.dsem[key[1]] if isinstance(key, tuple) else self.sem[key]

    def _wait(self, e, key, val, gen=None):
        if key == e and e == "pe":
            return
        if gen is not None and gen < self.gen:
            return
        w = self.waited[e]
        if w.get(key, 0) >= val:
            return
        self.eng[e].wait_ge(self._semof(key), val)
        w[key] = val
        self.nins += 1

    def _deps(self, e, R, W):
        for b in R:
            if b.lw is not None:
                self._wait(e, *b.lw)
        for b in W:
            if b.lw is not None:
                self._wait(e, *b.lw)
            for k, (v, g) in b.rd.items():
                self._wait(e, k, v, g)

    def _done(self, key, val, R, W):
        g = None if isinstance(key, tuple) else self.gen
        for b in R:
            o = b.rd.get(key)
            if o is None or o[1] != g or o[0] < val:
                b.rd[key] = (val, g)
        for b in W:
            b.lw = (key, val, g)
            b.rd = {}

    def op(self, e, fn, R=(), W=()):
        self._deps(e, R, W)
        ins = fn(self.eng[e])
        self.cnt[e] += 1
        ins.then_inc(self.sem[e], 1)
        self._done(e, self.cnt[e], R, W)
        self.nins += 1
        return ins

    def dma(self, q, out, in_, R=(), W=(), **kw):
        i = self.dnext
        self.dnext = (self.dnext + 1) % self.ndma
        key = ("dma", i)
        if self.dcnt[i] > 0:
            self._wait(q, key, self.dcnt[i])
        self._deps(q, R, W)
        ins = self.eng[q].dma_start(out=out, in_=in_, **kw)
        self.dcnt[i] += 16
        ins.then_inc(self.dsem[i], 16)
        self._done(key, self.dcnt[i], R, W)
        self.nins += 1
        return ins

    def idma(self, R=(), W=(), **kw):
        q = "pool"
        if not hasattr(self, "ipend"):
            self.ipend = []
        while len(self.ipend) >= self.imax:
            kk, vv = self.ipend.pop(0)
            self._wait("pool", kk, vv)
        i = self.dnext
        self.dnext = (self.dnext + 1) % self.ndma
        key = ("dma", i)
        if self.dcnt[i] > 0:
            self._wait(q, key, self.dcnt[i])
        self._deps(q, R, W)
        ins = self.nc.gpsimd.indirect_dma_start(**kw)
        self.dcnt[i] += 16
        ins.then_inc(self.dsem[i], 16)
        self.ipend.append((key, self.dcnt[i]))
        self._done(key, self.dcnt[i], R, W)
        self.nins += 1
        return ins

    def barrier(self):
        for e in ("pe", "act", "dve", "pool", "sp"):
            for o in ("pe", "act", "dve", "pool"):
                if o != e and self.cnt[o] > 0:
                    self._wait(e, o, self.cnt[o])
            for i in range(self.ndma):
                if self.dcnt[i] > 0:
                    self._wait(e, ("dma", i), self.dcnt[i])

    def regen(self):
        self.barrier()
        self.gen += 1
        for n in ("pe", "act", "dve", "pool"):
            self.sem[n] = self.top_es.enter_context(self.nc.semaphore("s_%s_g%d" % (n, self.gen)))
            self.cnt[n] = 0
        for e in self.waited:
            for n in ("pe", "act", "dve", "pool"):
                self.waited[e].pop(n, None)

    def finish(self, outs):
        for b in outs:
            if b.lw is not None:
                self._wait("sp", *b.lw)


import os
SKIP = os.environ.get('SKIP', '').split(',')
D = 1024
DIN = 2112
NB = 2
EPS = 1e-6


def build(T, stop_after=99, dbg=()):
    nc = bass.Bass("TRN2", target_bir_lowering=False)
    es = ExitStack()
    k = K(nc, es)
    NT = T // 128
    NTOK = NB * T

    def inp(name, shape, dt=F32):
        return Buf(nc.dram_tensor(name, list(shape), dt, kind="ExternalInput").ap(), name)

    x_d = inp("x", [NB * T, D])
    c_d = inp("c", [NB, D])
    ada_w_d = inp("ada_w", [D, 6 * D])
    ada_b_d = inp("ada_b", [1, 6 * D])
    norm_mix_d = inp("norm_mix", [1, D])
    w_in_d = inp("w_in", [D, DIN])
    outs = {}

    def outp(name, shape, dt=F32):
        b = Buf(nc.dram_tensor(name, list(shape), dt, kind="ExternalOutput").ap(), name)
        outs[name] = b
        return b

    ident_f = k.sb("ident_f", [128, 128], F32)
    ident_b = k.sb("ident_b", [128, 128], BF16)
    iot = k.sb("iot", [128, 128], I32)
    k.op("pool", lambda e: e.iota(iot[:], pattern=[[1, 128]], base=0, channel_multiplier=-1), W=[iot])
    k.op("dve", lambda e: e.tensor_scalar(ident_f[:], iot[:], 0.0, None, op0=ALU.is_equal), R=[iot], W=[ident_f])
    k.op("dve", lambda e: e.tensor_copy(ident_b[:], ident_f[:]), R=[ident_f], W=[ident_b])

    MOD = [[k.sb("mod%d_%d" % (b, w), [128, D]) for w in range(6)] for b in range(NB)]
    with ExitStack() as es0:
        k.es = es0
        cT = k.sb("cT", [128, NB, 8])
        cS = k.sb("cS", [128, NB, 8])
        with nc.allow_non_contiguous_dma(reason="tiny c transpose load"):
            for b in range(NB):
                k.dma("sp", cT[:, b, :], c_d[b, :].rearrange("(j p) -> p j", p=128), W=[cT])
        k.op("act", lambda e: e.activation(cS[:], cT[:], ACT.Silu), R=[cT], W=[cS])
        cB = [[k.sb("cB%d_%d" % (b, j), [128, 128]) for j in range(8)] for b in range(NB)]
        for b in range(NB):
            for j in range(8):
                k.op("dve", lambda e: e.tensor_copy(cB[b][j][:], cS[:, b, j:j + 1].to_broadcast([128, 128])),
                     R=[cS], W=[cB[b][j]])
        awb = [k.sb("awb%d" % i, [128, 8, 512]) for i in range(2)]
        abb = [k.sb("abb%d" % i, [128, 512]) for i in range(2)]
        pm = [k.ps("pm%d" % i, [128, 512]) for i in range(2)]
        for cb in range(12):
            aw = awb[cb % 2]
            ab = abb[cb % 2]
            k.dma("sp", aw[:], ada_w_d[:, cb * 512:(cb + 1) * 512].rearrange("(j p) n -> p j n", p=128), W=[aw])
            k.dma("sp", ab[:], ada_b_d[0:1, cb * 512:(cb + 1) * 512].partition_broadcast(128), W=[ab])
            for b in range(NB):
                p = pm[b]
                for j in range(8):
                    k.op("pe", lambda e: e.matmul(p[:], cB[b][j][:], aw[:, j, :], start=(j == 0), stop=(j == 7)),
                         R=[cB[b][j], aw], W=[p])
                dst = MOD[b][cb // 2]
                k.op("dve", lambda e: e.tensor_tensor(dst[:, (cb % 2) * 512:(cb % 2 + 1) * 512], p[:], ab[:], op=ALU.add),
                     R=[p, ab], W=[dst])
        nmb = k.sb("nmb", [128, D])
        k.dma("sp", nmb[:], norm_mix_d[0:1, :].partition_broadcast(128), W=[nmb])
        for b in range(NB):
            m = MOD[b][1]
            k.op("dve", lambda e: e.scalar_tensor_tensor(m[:], m[:], 1.0, nmb[:], op0=ALU.add, op1=ALU.mult),
                 R=[m, nmb], W=[m])
    k.es = es
    k.regen()
    if "mod" in dbg:
        o = outp("dbg_mod", [NB, 6, D])
        for b in range(NB):
            for w in range(6):
                k.dma("sp", o[b, w:w + 1, :], MOD[b][w][0:1, :], R=[MOD[b][w]], W=[o])
    if stop_after <= 0:
        k.finish(list(outs.values()))
        return nc, es

    U_d = outp("U", [NTOK, DIN]) if "U" in dbg else k.dram("U", [NTOK, DIN])
    with ExitStack() as es1:
        k.es = es1
        win = k.sb("win", [128, 8, DIN], BF16)
        wst = [k.sb("wst%d" % i, [128, DIN]) for i in range(2)]
        for j in range(8):
            s = wst[j % 2]
            k.dma("sp", s[:], w_in_d[j * 128:(j + 1) * 128, :], W=[s])
            k.op("pool" if j % 2 else "dve", lambda e: e.tensor_copy(win[:, j, :], s[:]), R=[s], W=[win])
        xt = [k.sb("xt%d" % i, [128, D]) for i in range(2)]
        junk = k.sb("junk", [128, D])
        ssq = [k.sb("ssq%d" % i, [128, 1]) for i in range(2)]
        rstd = [k.sb("rstd%d" % i, [128, 1]) for i in range(2)]
        hn = k.sb("hn", [128, D])
        hb = [k.sb("hb%d" % i, [128, D], BF16) for i in range(2)]
        hT = [k.sb("hT%d" % i, [128, 8, 128], BF16) for i in range(2)]
        ut = [k.sb("ut%d" % i, [128, DIN]) for i in range(2)]
        pT = [k.ps("pT%d" % i, [128, 8, 128], BF16) for i in range(2)]
        pu = [k.ps("pu%d" % i, [128, 512]) for i in range(3)]
        npu = 0
        for i in range(NB * NT):
            b = i // NT
            x_ = xt[i % 2]; sq = ssq[i % 2]; rs = rstd[i % 2]; h_ = hb[i % 2]; hT_ = hT[i % 2]; u_ = ut[i % 2]; pT_ = pT[i % 2]
            k.dma("sp", x_[:], x_d[i * 128:(i + 1) * 128, :], W=[x_])
            k.op("act", lambda e: e.activation(junk[:], x_[:], ACT.Square, accum_out=sq[:]), R=[x_], W=[junk, sq])
            k.op("dve", lambda e: e.tensor_scalar(rs[:], sq[:], 1.0 / D, EPS, op0=ALU.mult, op1=ALU.add), R=[sq], W=[rs])
            k.op("act", lambda e: e.sqrt(rs[:], rs[:]), R=[rs], W=[rs])
            k.op("dve", lambda e: e.reciprocal(rs[:], rs[:]), R=[rs], W=[rs])
            k.op("dve", lambda e: e.scalar_tensor_tensor(hn[:], x_[:], rs[:], MOD[b][1][:], op0=ALU.mult, op1=ALU.mult),
                 R=[x_, rs, MOD[b][1]], W=[hn])
            k.op("dve", lambda e: e.tensor_tensor(h_[:], hn[:], MOD[b][0][:], op=ALU.add), R=[hn, MOD[b][0]], W=[h_])
            for j in range(8):
                k.op("pe", lambda e: e.transpose(pT_[:, j, :], h_[:, j * 128:(j + 1) * 128], ident_b[:]),
                     R=[h_, ident_b], W=[pT_])
            k.op("act", lambda e: e.copy(hT_[:], pT_[:]), R=[pT_], W=[hT_])
            for cbi, (c0, c1) in enumerate([(0, 512), (512, 1024), (1024, 1536), (1536, 2048), (2048, 2112)]):
                p = pu[npu % 3]; npu += 1
                for j in range(8):
                    k.op("pe", lambda e: e.matmul(p[:, 0:c1 - c0], hT_[:, j, :], win[:, j, c0:c1], start=(j == 0), stop=(j == 7)),
                         R=[hT_, win], W=[p])
                k.op("dve" if cbi % 2 else "act",
                     (lambda e: e.tensor_copy(u_[:, c0:c1], p[:, 0:c1 - c0])) if cbi % 2 else
                     (lambda e: e.copy(u_[:, c0:c1], p[:, 0:c1 - c0])), R=[p], W=[u_])
            k.dma("sp", U_d[i * 128:(i + 1) * 128, :], u_[:], R=[u_], W=[U_d])
    k.es = es
    k.regen()
    if stop_after <= 1:
        k.finish(list(outs.values()))
        return nc, es


    rwkv_mu_d = inp("rwkv_mu", [1, 1696]); decay_w0_d = inp("decay_w0", [1, 512]); decay_up_d = inp("decay_up", [32, 512])
    iclr_a0_d = inp("iclr_a0", [1, 512]); iclr_up_d = inp("iclr_up", [32, 512]); gate_up_d = inp("gate_up", [96, 512])
    k_k_d = inp("rwkv_k_k", [1, 512]); k_a_d = inp("rwkv_k_a", [1, 512]); r_k_d = inp("rwkv_r_k", [1, 512])
    ROWS_d = outp("ROWS", [NB, T, 5, 512]) if "ROWS" in dbg else None
    ROWSW_d = k.dram("ROWSW", [NB, T, 512])
    ROWSB_d = k.dram("ROWSB", [NB, T, 4, 512], BF16)
    VT_d = outp("VT", [128, NT, 8, 128]) if "VT" in dbg else k.dram("VT", [128, NT, 8, 128])
    BON_d = outp("BON", [NTOK, 512]) if "BON" in dbg else k.dram("BON", [NTOK, 512])
    G_d = outp("G", [NTOK, 512]) if "G" in dbg else k.dram("G", [NTOK, 512])
    with ExitStack() as es2:
        k.es = es2
        def bc(name, src, n):
            t_ = k.sb(name, [128, n])
            k.dma("sp", t_[:], src[0:1, :].partition_broadcast(128), W=[t_])
            return t_
        mu_b = bc("mu_b", rwkv_mu_d, 1696); w0_b = bc("w0_b", decay_w0_d, 512); a0_b = bc("a0_b", iclr_a0_d, 512)
        kk_b = bc("kk_b", k_k_d, 512); ka_b = bc("ka_b", k_a_d, 512); rk_b = bc("rk_b", r_k_d, 512)
        dup = k.sb("dup", [32, 512]); iup = k.sb("iup", [32, 512]); gup = k.sb("gup", [96, 512])
        k.dma("sp", dup[:], decay_up_d[:], W=[dup]); k.dma("sp", iup[:], iclr_up_d[:], W=[iup]); k.dma("sp", gup[:], gate_up_d[:], W=[gup])
        uu = [k.sb("uu%d" % i, [128, 1696]) for i in range(2)]
        up_ = [k.sb("up%d" % i, [128, 1696]) for i in range(2)]
        us = k.sb("us", [128, 1696])
        Z = k.sb("Z", [128, 160]); ZT = k.sb("ZT", [96, 3, 128])
        rows = [k.sb("rows%d" % i, [128, 5, 512]) for i in range(2)]
        rowsb = [k.sb("rowsb%d" % i, [128, 4, 512], BF16) for i in range(2)]
        gt = [k.sb("gt%d" % i, [128, 512]) for i in range(2)]
        bon = [k.sb("bon%d" % i, [128, 512]) for i in range(2)]
        at = k.sb("at", [128, 512]); t5 = k.sb("t5", [128, 512]); t6 = k.sb("t6", [128, 512])
        s8 = k.sb("s8", [128, 8]); b8 = k.sb("b8", [128, 8])
        vts = [k.sb("vts%d" % i, [64, 8, 128]) for i in range(2)]
        pZ = k.ps("pZ", [128, 3, 128]); pl = k.ps("pl", [128, 3, 512]); pv = k.ps("pv", [64, 8, 128])
        for i in range(NB * NT):
            b = i // NT; it = i % NT
            u_ = uu[i % 2]; p_ = up_[i % 2]; rw = rows[i % 2]; g_ = gt[i % 2]; bo = bon[i % 2]; vt_ = vts[i % 2]
            k.dma("sp", u_[:], U_d[i * 128:(i + 1) * 128, 0:1696], R=[U_d], W=[u_])
            if it == 0:
                k.op("pool", lambda e: e.memset(p_[0:1, :], 0.0), W=[p_])
                k.dma("sp", p_[1:128, :], U_d[i * 128:i * 128 + 127, 0:1696], R=[U_d], W=[p_])
            else:
                k.dma("sp", p_[:], U_d[i * 128 - 1:i * 128 + 127, 0:1696], R=[U_d], W=[p_])
            k.op("pool", lambda e: e.tensor_tensor(p_[:], p_[:], u_[:], op=ALU.subtract), R=[p_, u_], W=[p_])
            k.op("pool", lambda e: e.tensor_tensor(p_[:], p_[:], mu_b[:], op=ALU.mult), R=[p_, mu_b], W=[p_])
            k.op("dve", lambda e: e.tensor_tensor(us[:], p_[:], u_[:], op=ALU.add), R=[p_, u_], W=[us])
            r_ = us[:, 0:512]; kx = us[:, 512:1024]; v_ = us[:, 1024:1536]
            k.op("act", lambda e: e.activation(Z[:, 0:32], us[:, 1536:1568], ACT.Tanh), R=[us], W=[Z])
            k.op("act", lambda e: e.copy(Z[:, 32:64], us[:, 1568:1600]), R=[us], W=[Z])
            k.op("act", lambda e: e.activation(Z[:, 64:160], us[:, 1600:1696], ACT.Sigmoid), R=[us], W=[Z])
            k.op("pe", lambda e: e.transpose(pZ[0:32, 0, :], Z[:, 0:32], ident_f[:]), R=[Z, ident_f], W=[pZ])
            k.op("pe", lambda e: e.transpose(pZ[0:32, 1, :], Z[:, 32:64], ident_f[:]), R=[Z, ident_f], W=[pZ])
            k.op("pe", lambda e: e.transpose(pZ[0:96, 2, :], Z[:, 64:160], ident_f[:]), R=[Z, ident_f], W=[pZ])
            k.op("dve", lambda e: e.tensor_copy(ZT[0:32, 0:2, :], pZ[0:32, 0:2, :]), R=[pZ], W=[ZT])
            k.op("dve", lambda e: e.tensor_copy(ZT[0:96, 2, :], pZ[0:96, 2, :]), R=[pZ], W=[ZT])
            k.op("pe", lambda e: e.matmul(pl[:, 0, :], ZT[0:32, 0, :], dup[:], start=True, stop=True), R=[ZT, dup], W=[pl])
            k.op("pe", lambda e: e.matmul(pl[:, 1, :], ZT[0:32, 1, :], iup[:], start=True, stop=True), R=[ZT, iup], W=[pl])
            k.op("pe", lambda e: e.matmul(pl[:, 2, :], ZT[0:96, 2, :], gup[:], start=True, stop=True), R=[ZT, gup], W=[pl])
            k.op("dve", lambda e: e.tensor_tensor(t5[:], pl[:, 0, :], w0_b[:], op=ALU.add), R=[pl, w0_b], W=[t5])
            k.op("act", lambda e: e.activation(t5[:], t5[:], ACT.Sigmoid), R=[t5], W=[t5])
            k.op("act", lambda e: e.activation(rw[:, 0, :], t5[:], ACT.Exp, scale=-float(np.exp(-0.5))), R=[t5], W=[rw])
            k.op("dve", lambda e: e.tensor_tensor(at[:], pl[:, 1, :], a0_b[:], op=ALU.add), R=[pl, a0_b], W=[at])
            k.op("act", lambda e: e.activation(at[:], at[:], ACT.Sigmoid), R=[at], W=[at])
            k.op("act", lambda e: e.copy(g_[:], pl[:, 2, :]), R=[pl], W=[g_])
            k.op("dve", lambda e: e.tensor_tensor(rw[:, 1, :], kx, kk_b[:], op=ALU.mult), R=[us, kk_b], W=[rw])
            k.op("pool", lambda e: e.tensor_tensor(t6[:], rw[:, 1, :], rw[:, 1, :], op=ALU.mult), R=[rw], W=[t6])
            k.op("dve", lambda e: e.tensor_reduce(s8[:], t6[:].rearrange("p (h n) -> p h n", h=8), axis=AX.X, op=ALU.add), R=[t6], W=[s8])
            k.op("dve", lambda e: e.tensor_scalar(s8[:], s8[:], 1e-24, None, op0=ALU.max), R=[s8], W=[s8])
            k.op("act", lambda e: e.sqrt(s8[:], s8[:]), R=[s8], W=[s8])
            k.op("dve", lambda e: e.reciprocal(s8[:], s8[:]), R=[s8], W=[s8])
            k.op("dve", lambda e: e.tensor_tensor(rw[:, 1, :].rearrange("p (h n) -> p h n", h=8), rw[:, 1, :].rearrange("p (h n) -> p h n", h=8),
                                                  s8[:].unsqueeze(2).to_broadcast([128, 8, 64]), op=ALU.mult), R=[rw, s8], W=[rw])
            k.op("pool", lambda e: e.tensor_tensor(rw[:, 2, :], rw[:, 1, :], at[:], op=ALU.mult), R=[rw, at], W=[rw])
            k.op("dve", lambda e: e.scalar_tensor_tensor(t5[:], at[:], -1.0, ka_b[:], op0=ALU.add, op1=ALU.mult), R=[at, ka_b], W=[t5])
            k.op("dve", lambda e: e.scalar_tensor_tensor(rw[:, 3, :], t5[:], 1.0, kx, op0=ALU.add, op1=ALU.mult), R=[t5, us], W=[rw])
            k.op("act", lambda e: e.copy(rw[:, 4, :], r_), R=[us], W=[rw])
            k.op("pool", lambda e: e.tensor_tensor(t6[:], rw[:, 3, :], r_, op=ALU.mult), R=[rw, us], W=[t6])
            k.op("pool", lambda e: e.tensor_tensor(t6[:], t6[:], rk_b[:], op=ALU.mult), R=[t6, rk_b], W=[t6])
            k.op("dve", lambda e: e.tensor_reduce(b8[:], t6[:].rearrange("p (h n) -> p h n", h=8), axis=AX.X, op=ALU.add), R=[t6], W=[b8])
            k.op("dve", lambda e: e.tensor_tensor(bo[:].rearrange("p (h n) -> p h n", h=8), v_.rearrange("p (h n) -> p h n", h=8),
                                                  b8[:].unsqueeze(2).to_broadcast([128, 8, 64]), op=ALU.mult), R=[us, b8], W=[bo])
            for h in range(8):
                k.op("pe", lambda e: e.transpose(pv[:, h, :], us[:, 1024 + h * 64:1024 + (h + 1) * 64], ident_f[:]), R=[us, ident_f], W=[pv])
            k.op("act", lambda e: e.copy(vt_[:], pv[:]), R=[pv], W=[vt_])
            rwb = rowsb[i % 2]
            k.op("act", lambda e: e.copy(rwb[:], rw[:, 1:5, :]), R=[rw], W=[rwb])
            if ROWS_d is not None:
                k.dma("sp", ROWS_d[b, it * 128:(it + 1) * 128, :, :], rw[:], R=[rw], W=[ROWS_d])
            k.dma("sp", ROWSW_d[b, it * 128:(it + 1) * 128, :], rw[:, 0, :], R=[rw], W=[ROWSW_d])
            k.dma("sp", ROWSB_d[b, it * 128:(it + 1) * 128, :, :], rwb[:], R=[rwb], W=[ROWSB_d])
            k.dma("sp", VT_d[b * 64:(b + 1) * 64, it, :, :], vt_[:], R=[vt_], W=[VT_d])
            k.dma("sp", BON_d[i * 128:(i + 1) * 128, :], bo[:], R=[bo], W=[BON_d])
            k.dma("sp", G_d[i * 128:(i + 1) * 128, :], g_[:], R=[g_], W=[G_d])
    k.es = es
    k.regen()
    if stop_after <= 2:
        k.finish(list(outs.values()))
        return nc, es


    TBS = 8
    YT_d = outp("YT", [128, T, 8]) if "YT" in dbg else k.dram("YT", [128, T, 8])
    with ExitStack() as es3:
        k.es = es3
        S = k.sb("S", [128, 512])
        t1 = k.sb("t1", [128, 512]); t2 = k.sb("t2", [128, 512])
        t3 = [k.sb("t3_%d" % i, [128, 512]) for i in range(2)]
        t4 = [k.sb("t4_%d" % i, [128, 512]) for i in range(2)]
        sa = k.sb("sa", [128, 8])
        RWt = [es3.enter_context(nc.sbuf_tensor("RW%d" % i, [128, TBS, 512], F32)) for i in range(2)]
        RBt = [es3.enter_context(nc.sbuf_tensor("RB%d" % i, [128, TBS, 4, 512], BF16)) for i in range(2)]
        RB0 = [Buf(t_, "rb0") for t_ in RBt]; RB1 = [Buf(t_, "rb1") for t_ in RBt]
        RW0 = [Buf(t_, "rw0") for t_ in RWt]; RW1 = [Buf(t_, "rw1") for t_ in RWt]
        VTc = [k.sb("VTc%d" % i, [128, 8, 128]) for i in range(2)]
        Yc = [k.sb("Yc%d" % i, [128, 128, 8]) for i in range(2)]
        k.op("dve", lambda e: e.memset(S[:], 0.0), W=[S])
        v3 = lambda ap: ap.rearrange("p (h n) -> p h n", h=8)
        nblk = 0; nst = 0
        for it in range(NT):
            vt_ = VTc[it % 2]; y_ = Yc[it % 2]
            k.dma("sp", vt_[:], VT_d[:, it, :, :], R=[VT_d], W=[vt_])
            for blk in range(128 // TBS):
                t0 = it * 128 + blk * TBS
                rbt = RBt[nblk % 2]; rb0 = RB0[nblk % 2]; rb1 = RB1[nblk % 2]
                rwt = RWt[nblk % 2]; rw0 = RW0[nblk % 2]; rw1 = RW1[nblk % 2]; nblk += 1
                k.dma("sp", rbt[0:64].rearrange("p t f n -> p (t f n)"),
                      ROWSB_d[0:1, t0:t0 + TBS].rearrange("o t f n -> o (t f n)").partition_broadcast(64), R=[ROWSB_d], W=[rb0])
                k.dma("act", rbt[64:128].rearrange("p t f n -> p (t f n)"),
                      ROWSB_d[1:2, t0:t0 + TBS].rearrange("o t f n -> o (t f n)").partition_broadcast(64), R=[ROWSB_d], W=[rb1])
                k.dma("sp", rwt[0:64].rearrange("p t n -> p (t n)"),
                      ROWSW_d[0:1, t0:t0 + TBS].rearrange("o t n -> o (t n)").partition_broadcast(64), R=[ROWSW_d], W=[rw0])
                k.dma("act", rwt[64:128].rearrange("p t n -> p (t n)"),
                      ROWSW_d[1:2, t0:t0 + TBS].rearrange("o t n -> o (t n)").partition_broadcast(64), R=[ROWSW_d], W=[rw1])
                RB = [rb0, rb1]; RWB = [rw0, rw1]
                for s_ in range(TBS):
                    tl = blk * TBS + s_
                    Wb = rwt[:, s_, :]; KKb = rbt[:, s_, 0, :]; KKAb = rbt[:, s_, 1, :]; Kb = rbt[:, s_, 2, :]; Rb = rbt[:, s_, 3, :]
                    t3_ = t3[nst % 2]; t4_ = t4[nst % 2]; nst += 1
                    k.op("pool", lambda e: e.tensor_tensor(v3(t3_[:]), v3(Kb), vt_[:, :, tl].unsqueeze(2).to_broadcast([128, 8, 64]), op=ALU.mult),
                         R=RB + [vt_], W=[t3_])
                    k.op("dve", lambda e: e.tensor_tensor(t1[:], S[:], KKb, op=ALU.mult), R=[S] + RB, W=[t1])
                    k.op("dve", lambda e: e.tensor_reduce(sa[:], v3(t1[:]), axis=AX.X, op=ALU.add, negate=True), R=[t1], W=[sa])
                    k.op("dve", lambda e: e.tensor_tensor(S[:], S[:], Wb, op=ALU.mult), R=[S] + RWB, W=[S])
                    k.op("dve", lambda e: e.tensor_tensor(v3(t2[:]), v3(KKAb), sa[:].unsqueeze(2).to_broadcast([128, 8, 64]), op=ALU.mult),
                         R=RB + [sa], W=[t2])
                    k.op("dve", lambda e: e.tensor_tensor(S[:], S[:], t2[:], op=ALU.add), R=[S, t2], W=[S])
                    k.op("dve", lambda e: e.tensor_tensor(S[:], S[:], t3_[:], op=ALU.add), R=[S, t3_], W=[S])
                    k.op("pool", lambda e: e.tensor_tensor(t4_[:], S[:], Rb, op=ALU.mult), R=[S] + RB, W=[t4_])
                    k.op("dve", lambda e: e.tensor_reduce(y_[:, tl, :], v3(t4_[:]), axis=AX.X, op=ALU.add), R=[t4_], W=[y_])
            k.dma("sp", YT_d[:, it * 128:(it + 1) * 128, :], y_[:], R=[y_], W=[YT_d])
            if it % 12 == 11 and it != NT - 1:
                k.regen()
    k.es = es
    k.regen()
    if stop_after <= 3:
        k.finish(list(outs.values()))
        return nc, es

    ln_w_d = inp("ln_x_w", [1, 512]); ln_b_d = inp("ln_x_b", [1, 512])
    YCAT_d = outp("YCAT", [NTOK, 1024]) if "YCAT" in dbg else k.dram("YCAT", [NTOK, 1024])
    with ExitStack() as es4:
        k.es = es4
        lnw_b = k.sb("lnw_b", [128, 512]); lnb_b = k.sb("lnb_b", [128, 512])
        k.dma("sp", lnw_b[:], ln_w_d[0:1, :].partition_broadcast(128), W=[lnw_b])
        k.dma("sp", lnb_b[:], ln_b_d[0:1, :].partition_broadcast(128), W=[lnb_b])
        yc = [k.sb("ycp%d" % i, [128, 128, 8]) for i in range(2)]
        py = k.ps("py", [128, 8, 128])
        ytm = k.sb("ytm", [128, 8, 128])
        yb = k.sb("yb", [128, 8, 64]); yq = k.sb("yq", [128, 8, 64])
        m8 = k.sb("m8", [128, 8]); v8 = k.sb("v8", [128, 8])
        bo = [k.sb("pbo%d" % i, [128, 512]) for i in range(2)]; g_ = [k.sb("pg%d" % i, [128, 512]) for i in range(2)]
        yo = [k.sb("yo%d" % i, [128, 512]) for i in range(2)]
        n = 0
        for it in range(NT):
            y_ = yc[it % 2]
            k.dma("sp", y_[:], YT_d[:, it * 128:(it + 1) * 128, :], R=[YT_d], W=[y_])
            for h in range(8):
                k.op("pe", lambda e: e.transpose(py[:, h, :], y_[:, :, h], ident_f[:]), R=[y_, ident_f], W=[py])
            k.op("act", lambda e: e.copy(ytm[:], py[:]), R=[py], W=[ytm])
            for b in range(NB):
                i = b * NT + it
                bo_ = bo[n % 2]; gg = g_[n % 2]; yo_ = yo[n % 2]; n += 1
                k.dma("sp", bo_[:], BON_d[i * 128:(i + 1) * 128, :], R=[BON_d], W=[bo_])
                k.dma("sp", gg[:], G_d[i * 128:(i + 1) * 128, :], R=[G_d], W=[gg])
                ysl = ytm[:, :, b * 64:(b + 1) * 64]
                k.op("dve", lambda e: e.tensor_reduce(m8[:], ysl, axis=AX.X, op=ALU.add), R=[ytm], W=[m8])
                k.op("dve", lambda e: e.tensor_scalar(m8[:], m8[:], 1.0 / 64, None, op0=ALU.mult), R=[m8], W=[m8])
                k.op("dve", lambda e: e.tensor_tensor(yb[:], ysl, m8[:].unsqueeze(2).to_broadcast([128, 8, 64]), op=ALU.subtract), R=[ytm, m8], W=[yb])
                k.op("pool", lambda e: e.tensor_tensor(yq[:], yb[:], yb[:], op=ALU.mult), R=[yb], W=[yq])
                k.op("dve", lambda e: e.tensor_reduce(v8[:], yq[:], axis=AX.X, op=ALU.add), R=[yq], W=[v8])
                k.op("dve", lambda e: e.tensor_scalar(v8[:], v8[:], 1.0 / 64, 64e-5, op0=ALU.mult, op1=ALU.add), R=[v8], W=[v8])
                k.op("act", lambda e: e.sqrt(v8[:], v8[:]), R=[v8], W=[v8])
                k.op("dve", lambda e: e.reciprocal(v8[:], v8[:]), R=[v8], W=[v8])
                k.op("dve", lambda e: e.tensor_tensor(yb[:], yb[:], v8[:].unsqueeze(2).to_broadcast([128, 8, 64]), op=ALU.mult), R=[yb, v8], W=[yb])
                ybf = yb[:].rearrange("p h n -> p (h n)")
                k.op("dve", lambda e: e.tensor_tensor(yo_[:], ybf, lnw_b[:], op=ALU.mult), R=[yb, lnw_b], W=[yo_])
                k.op("pool", lambda e: e.tensor_tensor(yo_[:], yo_[:], lnb_b[:], op=ALU.add), R=[yo_, lnb_b], W=[yo_])
                k.op("pool", lambda e: e.tensor_tensor(yo_[:], yo_[:], bo_[:], op=ALU.add), R=[yo_, bo_], W=[yo_])
                k.op("dve", lambda e: e.tensor_tensor(yo_[:], yo_[:], gg[:], op=ALU.mult), R=[yo_, gg], W=[yo_])
                k.dma("sp", YCAT_d[i * 128:(i + 1) * 128, 0:512], yo_[:], R=[yo_], W=[YCAT_d])
    k.es = es
    k.regen()
    if stop_after <= 4:
        k.finish(list(outs.values()))
        return nc, es


    pos_d = inp("positions", [NTOK, 1], I32)
    qan_d = inp("q_a_norm", [1, 256]); wqb_d = inp("w_q_b", [256, 768]); kvan_d = inp("kv_a_norm", [1, 128])
    wkvb_d = inp("w_kv_b", [128, 1024]); qn_d = inp("q_norm", [1, 96]); kn_d = inp("k_norm", [1, 96])
    QT_d = outp("QT", [NB, 96, 8, T], BF16) if "QT" in dbg else k.dram("QT", [NB, 96, 8, T], BF16)
    KT_d = outp("KT", [NB, 96, 8, T], BF16) if "KT" in dbg else k.dram("KT", [NB, 96, 8, T], BF16)
    V_d = outp("V", [NB, T, 8, 66], BF16) if "V" in dbg else k.dram("V", [NB, T, 8, 66], BF16)
    TWO_PI = float(2 * np.pi)
    with ExitStack() as es5:
        k.es = es5
        def bc(name, src, n):
            t_ = k.sb(name, [128, n])
            k.dma("sp", t_[:], src[0:1, :].partition_broadcast(128), W=[t_])
            return t_
        qan_b = bc("qan_b", qan_d, 256); kvan_b = bc("kvan_b", kvan_d, 128); qn96 = bc("qn96", qn_d, 96); kn96 = bc("kn96", kn_d, 96)
        qn_b = k.sb("qn_b", [128, 8, 96]); kn_b = k.sb("kn_b", [128, 8, 96])
        k.op("dve", lambda e: e.tensor_copy(qn_b[:], qn96[:].unsqueeze(1).to_broadcast([128, 8, 96])), R=[qn96], W=[qn_b])
        k.op("dve", lambda e: e.tensor_copy(kn_b[:], kn96[:].unsqueeze(1).to_broadcast([128, 8, 96])), R=[kn96], W=[kn_b])
        wq_st = k.sb("wq_st", [128, 2, 768]); wq = k.sb("wq", [128, 2, 768], BF16)
        k.dma("sp", wq_st[:], wqb_d[:].rearrange("(c p) n -> p c n", p=128), W=[wq_st])
        k.op("dve", lambda e: e.tensor_copy(wq[:], wq_st[:]), R=[wq_st], W=[wq])
        wkv_st = k.sb("wkv_st", [128, 1024]); wkv = k.sb("wkv", [128, 1024], BF16)
        k.dma("sp", wkv_st[:], wkvb_d[:], W=[wkv_st])
        k.op("dve", lambda e: e.tensor_copy(wkv[:], wkv_st[:]), R=[wkv_st], W=[wkv])
        ji = k.sb("ji", [128, 16], I32); invf = k.sb("invf", [128, 16])
        k.op("pool", lambda e: e.iota(ji[:], pattern=[[1, 16]], base=0, channel_multiplier=0), W=[ji])
        k.op("dve", lambda e: e.tensor_copy(invf[:], ji[:]), R=[ji], W=[invf])
        k.op("act", lambda e: e.activation(invf[:], invf[:], ACT.Exp, scale=-float(np.log(10000.0) / 16)), R=[invf], W=[invf])
        um = [k.sb("um%d" % i, [128, 416]) for i in range(2)]
        posi = k.sb("posi", [128, 1], I32); posf = k.sb("posf", [128, 1])
        ang = k.sb("ang", [128, 32]); angi = k.sb("angi", [128, 32], I32); angf = k.sb("angf", [128, 32]); msk = k.sb("msk", [128, 32])
        cs = k.sb("cs", [128, 32])
        junkm = k.sb("junkm", [128, 256]); s1 = k.sb("s1", [128, 1]); s2 = k.sb("s2", [128, 1])
        qlb = k.sb("qlb", [128, 256], BF16); kvb = k.sb("kvb", [128, 128], BF16)
        qlT = k.sb("qlT", [128, 2, 128], BF16); kvT = k.sb("kvT", [128, 128], BF16)
        pq = k.ps("pq", [128, 2, 512]); pkv = k.ps("pkv", [128, 2, 512])
        ptr = k.ps("ptr", [128, 3, 128], BF16)
        ptq = k.ps("ptq", [96, 8, 128], BF16)
        qf = k.sb("qf", [128, 8, 96]); kf = k.sb("kf", [128, 8, 96]); sqt = k.sb("sqt", [128, 8, 96])
        r8 = k.sb("r8", [128, 8])
        ra = k.sb("ra", [128, 8, 16]); rb_ = k.sb("rb_", [128, 8, 16]); rc = k.sb("rc", [128, 8, 16]); rd_ = k.sb("rd_", [128, 8, 16])
        qfb = k.sb("qfb", [128, 8, 96], BF16)
        qTs = [k.sb("qTs%d" % i, [96, 8, 128], BF16) for i in range(2)]
        kTs = [k.sb("kTs%d" % i, [96, 8, 128], BF16) for i in range(2)]
        vs = [k.sb("vs%d" % i, [128, 8, 66], BF16) for i in range(2)]
        for vv_ in vs:
            k.op("pool", lambda e: e.memset(vv_[:, :, 64:66], 1.0), W=[vv_])

        def headnorm_rope(xf, gain_b, outT, dstT, b, it):
            k.op("pool", lambda e: e.tensor_tensor(sqt[:], xf[:], xf[:], op=ALU.mult), R=[xf], W=[sqt])
            k.op("dve", lambda e: e.tensor_reduce(r8[:], sqt[:], axis=AX.X, op=ALU.add), R=[sqt], W=[r8])
            k.op("dve", lambda e: e.tensor_scalar(r8[:], r8[:], 1.0 / 96, EPS, op0=ALU.mult, op1=ALU.add), R=[r8], W=[r8])
            k.op("act", lambda e: e.sqrt(r8[:], r8[:]), R=[r8], W=[r8])
            k.op("dve", lambda e: e.reciprocal(r8[:], r8[:]), R=[r8], W=[r8])
            k.op("dve", lambda e: e.tensor_tensor(xf[:], xf[:], r8[:].unsqueeze(2).to_broadcast([128, 8, 96]), op=ALU.mult), R=[xf, r8], W=[xf])
            k.op("dve", lambda e: e.tensor_tensor(xf[:], xf[:], gain_b[:], op=ALU.mult), R=[xf, gain_b], W=[xf])
            sinb = cs[:, 0:16].unsqueeze(1).to_broadcast([128, 8, 16]); cosb = cs[:, 16:32].unsqueeze(1).to_broadcast([128, 8, 16])
            x1 = xf[:, :, 64:80]; x2 = xf[:, :, 80:96]
            k.op("dve", lambda e: e.tensor_tensor(ra[:], x1, cosb, op=ALU.mult), R=[xf, cs], W=[ra])
            k.op("dve", lambda e: e.tensor_tensor(rb_[:], x2, sinb, op=ALU.mult), R=[xf, cs], W=[rb_])
            k.op("dve", lambda e: e.tensor_tensor(rc[:], x2, cosb, op=ALU.mult), R=[xf, cs], W=[rc])
            k.op("dve", lambda e: e.tensor_tensor(rd_[:], x1, sinb, op=ALU.mult), R=[xf, cs], W=[rd_])
            k.op("dve", lambda e: e.tensor_tensor(x1, ra[:], rb_[:], op=ALU.subtract), R=[ra, rb_], W=[xf])
            k.op("dve", lambda e: e.tensor_tensor(x2, rc[:], rd_[:], op=ALU.add), R=[rc, rd_], W=[xf])
            k.op("act", lambda e: e.copy(qfb[:], xf[:]), R=[xf], W=[qfb])
            for h in range(8):
                k.op("pe", lambda e: e.transpose(ptq[:, h, :], qfb[:, h, :], ident_b[:]), R=[qfb, ident_b], W=[ptq])
            k.op("act", lambda e: e.copy(outT[:], ptq[:]), R=[ptq], W=[outT])
            if "noQT" not in SKIP:
                k.dma("sp", dstT[b, :, :, it * 128:(it + 1) * 128], outT[:], R=[outT], W=[dstT])

        for i in range(NB * NT):
            b = i // NT; it = i % NT
            u_ = um[i % 2]
            k.dma("sp", u_[:], U_d[i * 128:(i + 1) * 128, 1696:2112], R=[U_d], W=[u_])
            k.dma("sp", posi[:], pos_d[i * 128:(i + 1) * 128, :], W=[posi])
            if "noROPE" not in SKIP:
                k.op("dve", lambda e: e.tensor_copy(posf[:], posi[:]), R=[posi], W=[posf])
                k.op("dve", lambda e: e.tensor_scalar(ang[:, 0:16], invf[:], posf[:], None, op0=ALU.mult), R=[invf, posf], W=[ang])
                k.op("dve", lambda e: e.tensor_scalar(ang[:, 16:32], ang[:, 0:16], float(np.pi / 2), None, op0=ALU.add), R=[ang], W=[ang])
                k.op("dve", lambda e: e.tensor_scalar(angf[:], ang[:], 1.0 / TWO_PI, None, op0=ALU.mult), R=[ang], W=[angf])
                k.op("dve", lambda e: e.tensor_copy(angi[:], angf[:]), R=[angf], W=[angi])
                k.op("dve", lambda e: e.tensor_copy(angf[:], angi[:]), R=[angi], W=[angf])
                k.op("dve", lambda e: e.scalar_tensor_tensor(ang[:], angf[:], -TWO_PI, ang[:], op0=ALU.mult, op1=ALU.add), R=[angf, ang], W=[ang])
                k.op("dve", lambda e: e.tensor_scalar(msk[:], ang[:], float(np.pi), -TWO_PI, op0=ALU.is_gt, op1=ALU.mult), R=[ang], W=[msk])
                k.op("dve", lambda e: e.tensor_tensor(ang[:], ang[:], msk[:], op=ALU.add), R=[ang, msk], W=[ang])
                k.op("dve", lambda e: e.tensor_scalar(msk[:], ang[:], -float(np.pi), TWO_PI, op0=ALU.is_lt, op1=ALU.mult), R=[ang], W=[msk])
                k.op("dve", lambda e: e.tensor_tensor(ang[:], ang[:], msk[:], op=ALU.add), R=[ang, msk], W=[ang])
                k.op("act", lambda e: e.activation(cs[:], ang[:], ACT.Sin), R=[ang], W=[cs])
            if "noLAT" not in SKIP:
                k.op("act", lambda e: e.activation(junkm[:, 0:256], u_[:, 0:256], ACT.Square, accum_out=s1[:]), R=[u_], W=[junkm, s1])
                k.op("dve", lambda e: e.tensor_scalar(s1[:], s1[:], 1.0 / 256, EPS, op0=ALU.mult, op1=ALU.add), R=[s1], W=[s1])
                k.op("act", lambda e: e.sqrt(s1[:], s1[:]), R=[s1], W=[s1])
                k.op("dve", lambda e: e.reciprocal(s1[:], s1[:]), R=[s1], W=[s1])
                k.op("dve", lambda e: e.scalar_tensor_tensor(qlb[:], u_[:, 0:256], s1[:], qan_b[:], op0=ALU.mult, op1=ALU.mult), R=[u_, s1, qan_b], W=[qlb])
                k.op("act", lambda e: e.activation(junkm[:, 0:128], u_[:, 256:384], ACT.Square, accum_out=s2[:]), R=[u_], W=[junkm, s2])
                k.op("dve", lambda e: e.tensor_scalar(s2[:], s2[:], 1.0 / 128, EPS, op0=ALU.mult, op1=ALU.add), R=[s2], W=[s2])
                k.op("act", lambda e: e.sqrt(s2[:], s2[:]), R=[s2], W=[s2])
                k.op("dve", lambda e: e.reciprocal(s2[:], s2[:]), R=[s2], W=[s2])
                k.op("dve", lambda e: e.scalar_tensor_tensor(kvb[:], u_[:, 256:384], s2[:], kvan_b[:], op0=ALU.mult, op1=ALU.mult), R=[u_, s2, kvan_b], W=[kvb])
                k.op("pe", lambda e: e.transpose(ptr[:, 0, :], qlb[:, 0:128], ident_b[:]), R=[qlb, ident_b], W=[ptr])
                k.op("pe", lambda e: e.transpose(ptr[:, 1, :], qlb[:, 128:256], ident_b[:]), R=[qlb, ident_b], W=[ptr])
                k.op("pe", lambda e: e.transpose(ptr[:, 2, :], kvb[:], ident_b[:]), R=[kvb, ident_b], W=[ptr])
                k.op("act", lambda e: e.copy(qlT[:], ptr[:, 0:2, :]), R=[ptr], W=[qlT])
                k.op("act", lambda e: e.copy(kvT[:], ptr[:, 2, :]), R=[ptr], W=[kvT])
                if "noA" not in SKIP:
                    for c in range(2):
                        k.op("pe", lambda e: e.matmul(pq[:, 0, :], qlT[:, c, :], wq[:, c, 0:512], start=(c == 0), stop=(c == 1)), R=[qlT, wq], W=[pq])
                    for c in range(2):
                        k.op("pe", lambda e: e.matmul(pq[:, 1, 0:256], qlT[:, c, :], wq[:, c, 512:768], start=(c == 0), stop=(c == 1)), R=[qlT, wq], W=[pq])
                    k.op("pe", lambda e: e.matmul(pkv[:, 0, :], kvT[:], wkv[:, 0:512], start=True, stop=True), R=[kvT, wkv], W=[pkv])
                    k.op("pe", lambda e: e.matmul(pkv[:, 1, :], kvT[:], wkv[:, 512:1024], start=True, stop=True), R=[kvT, wkv], W=[pkv])
                    qflat = qf[:].rearrange("p h d -> p (h d)")
                    k.op("act", lambda e: e.copy(qflat[:, 0:512], pq[:, 0, :]), R=[pq], W=[qf])
                    k.op("act", lambda e: e.copy(qflat[:, 512:768], pq[:, 1, 0:256]), R=[pq], W=[qf])
                    kv3 = pkv[:].rearrange("p c (h e) -> p (c h) e", e=128)
                    k.op("dve", lambda e: e.tensor_copy(kf[:, :, 0:64], kv3[:, :, 0:64]), R=[pkv], W=[kf])
                    k.op("dve", lambda e: e.tensor_copy(kf[:, :, 64:96], u_[:, 384:416].unsqueeze(1).to_broadcast([128, 8, 32])), R=[u_], W=[kf])
            v_ = vs[i % 2]
            if "noLAT" not in SKIP and "noA" not in SKIP and "noVC" not in SKIP:
                k.op("dve", lambda e: e.tensor_copy(v_[:, :, 0:64], kv3[:, :, 64:128]), R=[pkv], W=[v_])
            if "noV" not in SKIP:
                k.dma("sp", V_d[b, it * 128:(it + 1) * 128, :, :], v_[:], R=[v_], W=[V_d])
            if "noHN" not in SKIP:
                headnorm_rope(qf, qn_b, qTs[i % 2], QT_d, b, it)
                headnorm_rope(kf, kn_b, kTs[i % 2], KT_d, b, it)
    k.es = es
    k.regen()
    if stop_after <= 5:
        k.finish(list(outs.values()))
        return nc, es


    QS = min(4, NT)
    NJ = NT // QS
    QW = QS * 128
    with ExitStack() as es6:
        k.es = es6
        MASK = []
        mi = k.sb("mi", [128, QW], I32)
        for r_ in range(QS):
            m_ = k.sb("mask%d" % r_, [128, QW], BF16)
            k.op("pool", lambda e: e.iota(mi[:], pattern=[[1, QW]], base=-r_ * 128, channel_multiplier=-1), R=[], W=[mi])
            k.op("dve", lambda e: e.tensor_scalar(m_[:], mi[:], 0.0, None, op0=ALU.is_ge), R=[mi], W=[m_])
            MASK.append(m_)
        Vall = k.sb("Vall", [128, NT, 8, 66], BF16)
        QTs = [k.sb("QTs%d" % i, [96, T], BF16) for i in range(2)]
        KTs = [k.sb("KTs%d" % i, [96, T], BF16) for i in range(2)]
        pTs = [k.sb("pTs%d" % i, [128, QW], BF16) for i in range(3)]
        ps_ = [k.ps("ps%d" % i, [128, 512]) for i in range(2)]
        po = [k.ps("po%d" % i, [128, 512]) for i in range(QS)]
        rec = k.sb("rec", [128, 1])
        ym = [k.sb("ym%d" % i, [128, 64]) for i in range(4)]
        SC = float(96 ** -0.5)
        nbh = 0; npt = 0; nym = 0
        for b in range(NB):
            for kb in range(NT):
                k.dma("sp", Vall[:, kb, :, :], V_d[b, kb * 128:(kb + 1) * 128, :, :], R=[V_d], W=[Vall])
            for h in range(8):
                Q_ = QTs[nbh % 2]; K_ = KTs[nbh % 2]; nbh += 1
                k.dma("sp", Q_[:], QT_d[b, :, h, :], R=[QT_d], W=[Q_])
                k.dma("sp", K_[:], KT_d[b, :, h, :], R=[KT_d], W=[K_])
                for J in range(NJ):
                    nkb = QS * J + QS
                    for kb in range(nkb):
                        p_ = ps_[npt % 2]; pT = pTs[npt % 3]; npt += 1
                        k.op("pe", lambda e: e.matmul(p_[:, 0:QW], K_[:, kb * 128:(kb + 1) * 128], Q_[:, J * QW:(J + 1) * QW], start=True, stop=True),
                             R=[K_, Q_], W=[p_])
                        k.op("act", lambda e: e.activation(pT[:], p_[:, 0:QW], ACT.Exp, scale=SC), R=[p_], W=[pT])
                        r_ = kb - QS * J
                        if r_ >= 0:
                            k.op("dve", lambda e: e.tensor_tensor(pT[:], pT[:], MASK[r_][:], op=ALU.mult), R=[pT, MASK[r_]], W=[pT])
                        for qb in range(QS):
                            if QS * J + qb >= kb:
                                k.op("pe", lambda e: e.matmul(po[qb][:, 0:65], pT[:, qb * 128:(qb + 1) * 128], Vall[:, kb, h, 0:65],
                                                              start=(kb == 0), stop=(kb == QS * J + qb)), R=[pT, Vall], W=[po[qb]])
                    for qb in range(QS):
                        y_ = ym[nym % 4]; nym += 1
                        k.op("dve", lambda e: e.reciprocal(rec[:], po[qb][:, 64:65]), R=[po[qb]], W=[rec])
                        k.op("dve", lambda e: e.tensor_scalar(y_[:], po[qb][:, 0:64], rec[:], None, op0=ALU.mult), R=[po[qb], rec], W=[y_])
                        i = b * NT + QS * J + qb
                        k.dma("sp", YCAT_d[i * 128:(i + 1) * 128, 512 + h * 64:512 + (h + 1) * 64], y_[:], R=[y_], W=[YCAT_d])
    k.es = es
    k.regen()
    if stop_after <= 6:
        k.finish(list(outs.values()))
        return nc, es


    NE = 256
    BLK = 256
    LOGB = 8
    NBLK = (NTOK * 8 + NE * (BLK - 1)) // BLK
    NROWS = NBLK * BLK
    w_out_d = inp("w_out", [D, D]); norm_ffn_d = inp("norm_ffn", [1, D]); w_router_d = inp("w_router", [D, NE]); rbias_d = inp("router_bias", [1, NE])
    X1_d = outp("X1", [NTOK, D]) if "X1" in dbg else k.dram("X1", [NTOK, D])
    H2T_d = outp("H2T", [128, 8, NTOK], BF16) if "H2T" in dbg else k.dram("H2T", [128, 8, NTOK], BF16)
    XBUF_d = k.dram("XBUF", [NROWS, D], BF16)
    YBUF_d = k.dram("YBUF", [NROWS, D], BF16)
    H2B_d = k.dram("H2B", [NTOK, D], BF16)
    dbgH2 = outp("H2", [NTOK, D]) if "H2" in dbg else None
    dbgG = outp("GATE", [NTOK, NE]) if "GATE" in dbg else None
    dbgDW = outp("DW", [NTOK, 16]) if "DW" in dbg else None
    NTT = NB * NT
    BREG = nc.gpsimd.to_reg(NROWS - 1)
    DESTI = k.sb("DESTI", [128, NTT, 8], I32)
    WK = k.sb("WK", [128, NTT, 8])
    IDXG = k.sb("IDXG", [128, NBLK], I32)
    with ExitStack() as es7:
        k.es = es7
        nfb = k.sb("nfb", [128, D])
        k.dma("sp", nfb[:], norm_ffn_d[0:1, :].partition_broadcast(128), W=[nfb])
        for b in range(NB):
            m = MOD[b][4]
            k.op("dve", lambda e: e.scalar_tensor_tensor(m[:], m[:], 1.0, nfb[:], op0=ALU.add, op1=ALU.mult), R=[m, nfb], W=[m])
        rb_b = k.sb("rb_b", [128, NE])
        k.dma("sp", rb_b[:], rbias_d[0:1, :].partition_broadcast(128), W=[rb_b])
        wo = k.sb("wo", [128, 8, D], BF16); wr = k.sb("wr", [128, 8, NE])
        wst = [k.sb("wost%d" % i, [128, D]) for i in range(2)]
        for j in range(8):
            s_ = wst[j % 2]
            k.dma("sp", s_[:], w_out_d[j * 128:(j + 1) * 128, :], W=[s_])
            k.op("pool" if j % 2 else "dve", lambda e: e.tensor_copy(wo[:, j, :], s_[:]), R=[s_], W=[wo])
        k.dma("sp", wr[:], w_router_d[:].rearrange("(j p) n -> p j n", p=128), W=[wr])
        UT = k.sb("UT", [128, 128], BF16); ONESB = k.sb("ONESB", [128, 128], BF16); ones256 = k.sb("ones256", [128, NE])
        uti = k.sb("uti", [128, 128], I32)
        k.op("pool", lambda e: e.iota(uti[:], pattern=[[1, 128]], base=0, channel_multiplier=-1), W=[uti])
        k.op("dve", lambda e: e.tensor_scalar(UT[:], uti[:], 0.0, None, op0=ALU.is_gt), R=[uti], W=[UT])
        k.op("dve", lambda e: e.memset(ONESB[:], 1.0), W=[ONESB])
        k.op("dve", lambda e: e.memset(ones256[:], 1.0), W=[ones256])
        ecap_i = k.sb("ecap_i", [128, NE], I32); eidx = k.sb("eidx", [128, NE])
        k.op("pool", lambda e: e.iota(ecap_i[:], pattern=[[1, NE]], base=0, channel_multiplier=0), W=[ecap_i])
        k.op("dve", lambda e: e.tensor_copy(eidx[:], ecap_i[:]), R=[ecap_i], W=[eidx])
        EK = k.sb("EK", [128, NTT, 8]); RK = k.sb("RK", [128, NTT, 8])
        carry = k.sb("carry", [128, NE])
        k.op("dve", lambda e: e.memset(carry[:], 0.0), W=[carry])
        yc_ = [k.sb("ycat%d" % i, [128, D]) for i in range(2)]
        ycb = k.sb("ycb", [128, D], BF16); ycT = k.sb("ycT", [128, 8, 128], BF16)
        xt = [k.sb("x3_%d" % i, [128, D]) for i in range(2)]
        x1 = [k.sb("x1_%d" % i, [128, D]) for i in range(2)]
        junk = k.sb("junk3", [128, D]); ssq = k.sb("ssq3", [128, 1])
        h2 = k.sb("h2", [128, D]); h2b = [k.sb("h2b%d" % i, [128, D], BF16) for i in range(2)]
        h2T = k.sb("h2T", [128, 8, 128]); h2Tb = [k.sb("h2Tb%d" % i, [128, 8, 128], BF16) for i in range(2)]
        pT3 = k.ps("pT3", [128, 8, 128], BF16); pTf = k.ps("pTf", [128, 8, 128])
        pp = k.ps("pp", [128, 2, 512]); plg = k.ps("plg", [128, NE]); prk = k.ps("prk", [128, 2, NE])
        sc = k.sb("sc", [128, NE]); sel = k.sb("sel", [128, NE]); selm = k.sb("selm", [128, NE])
        m8g = k.sb("m8g", [128, 8, 8]); gs = k.sb("gs", [128, 8]); gm8 = k.sb("gm8", [128, 8]); gmask = k.sb("gmask", [128, 8]); em8 = k.sb("em8", [128, 8])
        emask = k.sb("emask", [128, NE]); emb = k.sb("emb", [128, NE], BF16); Gm = k.sb("Gm", [128, NE]); wsum = k.sb("wsum", [128, 1])
        pos = k.sb("pos", [128, NE]); dfull = k.sb("dfull", [128, NE]); ovf = k.sb("ovf", [128, NE]); slot = k.sb("slot", [128, NE])
        junk2 = k.sb("junk2", [128, NE]); destf = k.sb("destf", [128, 8])
        for i in range(NTT):
            b = i // NT
            y_ = yc_[i % 2]; x_ = xt[i % 2]; x1_ = x1[i % 2]; hb_ = h2b[i % 2]; hTb_ = h2Tb[i % 2]
            k.dma("sp", y_[:], YCAT_d[i * 128:(i + 1) * 128, :], R=[YCAT_d], W=[y_])
            k.dma("sp", x_[:], x_d[i * 128:(i + 1) * 128, :], W=[x_])
            k.op("act", lambda e: e.copy(ycb[:], y_[:]), R=[y_], W=[ycb])
            for j in range(8):
                k.op("pe", lambda e: e.transpose(pT3[:, j, :], ycb[:, j * 128:(j + 1) * 128], ident_b[:]), R=[ycb, ident_b], W=[pT3])
            k.op("act", lambda e: e.copy(ycT[:], pT3[:]), R=[pT3], W=[ycT])
            for hh in range(2):
                for j in range(8):
                    k.op("pe", lambda e: e.matmul(pp[:, hh, :], ycT[:, j, :], wo[:, j, hh * 512:(hh + 1) * 512], start=(j == 0), stop=(j == 7)), R=[ycT, wo], W=[pp])
            ppf = pp[:].rearrange("p a n -> p (a n)")
            k.op("dve", lambda e: e.tensor_tensor(x1_[:], ppf, MOD[b][2][:], op=ALU.mult), R=[pp, MOD[b][2]], W=[x1_])
            k.op("pool", lambda e: e.tensor_tensor(x1_[:], x1_[:], x_[:], op=ALU.add), R=[x1_, x_], W=[x1_])
            k.dma("sp", X1_d[i * 128:(i + 1) * 128, :], x1_[:], R=[x1_], W=[X1_d])
            k.op("act", lambda e: e.activation(junk[:], x1_[:], ACT.Square, accum_out=ssq[:]), R=[x1_], W=[junk, ssq])
            k.op("dve", lambda e: e.tensor_scalar(ssq[:], ssq[:], 1.0 / D, EPS, op0=ALU.mult, op1=ALU.add), R=[ssq], W=[ssq])
            k.op("act", lambda e: e.sqrt(ssq[:], ssq[:]), R=[ssq], W=[ssq])
            k.op("dve", lambda e: e.reciprocal(ssq[:], ssq[:]), R=[ssq], W=[ssq])
            k.op("dve", lambda e: e.scalar_tensor_tensor(h2[:], x1_[:], ssq[:], MOD[b][4][:], op0=ALU.mult, op1=ALU.mult), R=[x1_, ssq, MOD[b][4]], W=[h2])
            k.op("pool", lambda e: e.tensor_tensor(h2[:], h2[:], MOD[b][3][:], op=ALU.add), R=[h2, MOD[b][3]], W=[h2])
            k.op("act", lambda e: e.copy(hb_[:], h2[:]), R=[h2], W=[hb_])
            if dbgH2 is not None:
                k.dma("sp", dbgH2[i * 128:(i + 1) * 128, :], h2[:], R=[h2], W=[dbgH2])
            for j in range(8):
                k.op("pe", lambda e: e.transpose(pTf[:, j, :], h2[:, j * 128:(j + 1) * 128], ident_f[:]), R=[h2, ident_f], W=[pTf])
            k.op("dve", lambda e: e.tensor_copy(h2T[:], pTf[:]), R=[pTf], W=[h2T])
            k.op("pool", lambda e: e.tensor_copy(hTb_[:], h2T[:]), R=[h2T], W=[hTb_])
            if "noH2T" not in SKIP:
                k.dma("sp", H2T_d[:, :, i * 128:(i + 1) * 128], hTb_[:], R=[hTb_], W=[H2T_d])
            if "noRT" not in SKIP:
                for j in range(8):
                    k.op("pe", lambda e: e.matmul(plg[:], h2T[:, j, :], wr[:, j, :], start=(j == 0), stop=(j == 7)), R=[h2T, wr], W=[plg])
                k.op("act", lambda e: e.activation(sc[:], plg[:], ACT.Sigmoid), R=[plg], W=[sc])
                k.op("dve", lambda e: e.tensor_tensor(sel[:], sc[:], rb_b[:], op=ALU.add), R=[sc, rb_b], W=[sel])
                for g in range(8):
                    k.op("dve", lambda e: e.max(m8g[:, g, :], sel[:, g * 32:(g + 1) * 32]), R=[sel], W=[m8g])
                k.op("dve", lambda e: e.tensor_tensor(gs[:], m8g[:, :, 0], m8g[:, :, 1], op=ALU.add), R=[m8g], W=[gs])
                k.op("dve", lambda e: e.max(gm8[:], gs[:]), R=[gs], W=[gm8])
                k.op("dve", lambda e: e.tensor_scalar(gmask[:], gs[:], gm8[:, 3:4], None, op0=ALU.is_ge), R=[gs, gm8], W=[gmask])
                k.op("dve", lambda e: e.scalar_tensor_tensor(selm[:].rearrange("p (g n) -> p g n", g=8), sel[:].rearrange("p (g n) -> p g n", g=8), 2.0,
                                                             gmask[:].unsqueeze(2).to_broadcast([128, 8, 32]), op0=ALU.add, op1=ALU.mult), R=[sel, gmask], W=[selm])
                k.op("dve", lambda e: e.max(em8[:], selm[:]), R=[selm], W=[em8])
                k.op("dve", lambda e: e.tensor_scalar(emask[:], selm[:], em8[:, 7:8], None, op0=ALU.is_ge), R=[selm, em8], W=[emask])
                k.op("act", lambda e: e.copy(emb[:], emask[:]), R=[emask], W=[emb])
                k.op("dve", lambda e: e.scalar_tensor_tensor(Gm[:], sc[:], 1.0, emask[:], op0=ALU.mult, op1=ALU.mult, accum_out=wsum[:]), R=[sc, emask], W=[Gm, wsum])
                k.op("dve", lambda e: e.reciprocal(wsum[:], wsum[:]), R=[wsum], W=[wsum])
                k.op("dve", lambda e: e.tensor_scalar(Gm[:], Gm[:], wsum[:], 2.5, op0=ALU.mult, op1=ALU.mult), R=[Gm, wsum], W=[Gm])
                if dbgG is not None:
                    k.dma("sp", dbgG[i * 128:(i + 1) * 128, :], Gm[:], R=[Gm], W=[dbgG])
            if "noRT" not in SKIP and "noRK" not in SKIP:
                k.op("pe", lambda e: e.matmul(prk[:, 0, :], UT[:], emb[:], start=True, stop=True), R=[UT, emb], W=[prk])
                k.op("pe", lambda e: e.matmul(prk[:, 1, :], ONESB[:], emb[:], start=True, stop=True), R=[ONESB, emb], W=[prk])
                k.op("dve", lambda e: e.tensor_tensor(pos[:], prk[:, 0, :], carry[:], op=ALU.add), R=[prk, carry], W=[pos])
                k.op("dve", lambda e: e.tensor_tensor(carry[:], prk[:, 1, :], carry[:], op=ALU.add), R=[prk, carry], W=[carry])
                k.op("dve", lambda e: e.tensor_tensor_scan(slot[:], ones256[:], emask[:], 0.0, op0=ALU.mult, op1=ALU.add), R=[ones256, emask], W=[slot])
                k.op("dve", lambda e: e.tensor_tensor(slot[:], slot[:], emask[:], op=ALU.mult), R=[slot, emask], W=[slot])
                for ks in range(8):
                    k.op("dve", lambda e: e.scalar_tensor_tensor(junk2[:], slot[:], float(ks + 1), eidx[:], op0=ALU.is_equal, op1=ALU.mult, accum_out=EK[:, i, ks:ks + 1]),
                         R=[slot, eidx], W=[junk2, EK])
                    k.op("dve", lambda e: e.scalar_tensor_tensor(junk2[:], slot[:], float(ks + 1), pos[:], op0=ALU.is_equal, op1=ALU.mult, accum_out=RK[:, i, ks:ks + 1]),
                         R=[slot, pos], W=[junk2, RK])
                    k.op("dve", lambda e: e.scalar_tensor_tensor(junk2[:], slot[:], float(ks + 1), Gm[:], op0=ALU.is_equal, op1=ALU.mult, accum_out=WK[:, i, ks:ks + 1]),
                         R=[slot, Gm], W=[junk2, WK])
            k.dma("sp", H2B_d[i * 128:(i + 1) * 128, :], hb_[:], R=[hb_], W=[H2B_d])
        cnt_i = k.sb("cnt_i", [128, NE], I32); padc = k.sb("padc", [128, NE]); pend = k.sb("pend", [128, NE]); pstart = k.sb("pstart", [128, NE])
        k.op("dve", lambda e: e.tensor_scalar(padc[:], carry[:], float(BLK - 1), None, op0=ALU.add), R=[carry], W=[padc])
        k.op("dve", lambda e: e.tensor_copy(cnt_i[:], padc[:]), R=[padc], W=[cnt_i])
        k.op("dve", lambda e: e.tensor_scalar(cnt_i[:], cnt_i[:], LOGB, LOGB, op0=ALU.arith_shift_right, op1=ALU.logical_shift_left), R=[cnt_i], W=[cnt_i])
        k.op("dve", lambda e: e.tensor_copy(padc[:], cnt_i[:]), R=[cnt_i], W=[padc])
        k.op("dve", lambda e: e.tensor_tensor_scan(pend[:], ones256[:], padc[:], 0.0, op0=ALU.mult, op1=ALU.add), R=[ones256, padc], W=[pend])
        k.op("dve", lambda e: e.tensor_tensor(pstart[:], pend[:], padc[:], op=ALU.subtract), R=[pend, padc], W=[pstart])
        bexp = k.sb("bexp", [128, NBLK])
        for j in range(NBLK):
            k.op("dve", lambda e: e.tensor_scalar(junk2[:], pend[:], float(BLK * j), 0.0, op0=ALU.is_le, op1=ALU.add, accum_out=bexp[:, j:j + 1]), R=[pend], W=[junk2, bexp])
        k.op("dve", lambda e: e.tensor_scalar(bexp[:], bexp[:], float(NE - 1), None, op0=ALU.min), R=[bexp], W=[bexp])
        bgi = k.sb("bgi", [128, 1], I32); bgf = k.sb("bgf", [128, 1])
        k.op("pool", lambda e: e.iota(bgi[:], pattern=[[1, 1]], base=0, channel_multiplier=1), W=[bgi])
        k.op("dve", lambda e: e.tensor_copy(bgf[:], bgi[:]), R=[bgi], W=[bgf])
        idxf = k.sb("idxf", [128, NBLK])
        k.op("dve", lambda e: e.tensor_scalar(idxf[:], bexp[:], 128.0, bgf[:, 0:1], op0=ALU.mult, op1=ALU.add), R=[bexp, bgf], W=[idxf])
        k.op("dve", lambda e: e.tensor_copy(IDXG[:], idxf[:]), R=[idxf], W=[IDXG])
        if "BEXP" in dbg:
            o = outp("BEXP", [1, NBLK]); k.dma("sp", o[0:1, :], bexp[0:1, :], R=[bexp], W=[o])
            o2 = outp("PSTART", [1, NE]); k.dma("sp", o2[0:1, :], pstart[0:1, :], R=[pstart], W=[o2])
        hbb = [k.sb("hbb%d" % i, [128, D], BF16) for i in range(2)]
        for i in range(NTT):
            hb_ = hbb[i % 2]
            k.dma("sp", hb_[:], H2B_d[i * 128:(i + 1) * 128, :], R=[H2B_d], W=[hb_])
            for ks in range(8):
                k.op("dve", lambda e: e.scalar_tensor_tensor(junk2[:], eidx[:], EK[:, i, ks:ks + 1], pstart[:], op0=ALU.is_equal, op1=ALU.mult, accum_out=destf[:, ks:ks + 1]),
                     R=[eidx, EK, pstart], W=[junk2, destf])
            k.op("dve", lambda e: e.tensor_tensor(destf[:], destf[:], RK[:, i, :], op=ALU.add), R=[destf, RK], W=[destf])
            k.op("dve", lambda e: e.tensor_copy(DESTI[:, i, :], destf[:]), R=[destf], W=[DESTI])
            if dbgDW is not None:
                k.dma("sp", dbgDW[i * 128:(i + 1) * 128, 0:8], destf[:], R=[destf], W=[dbgDW])
                k.dma("sp", dbgDW[i * 128:(i + 1) * 128, 8:16], WK[:, i, :], R=[WK], W=[dbgDW])
            for ks in range(8):
                k.idma(out=XBUF_d[:, :], out_offset=bass.IndirectOffsetOnAxis(ap=DESTI[:, i, ks:ks + 1], axis=0), in_=hb_[:, :], in_offset=None,
                       bounds_check=BREG, oob_is_err=False, R=[hb_, DESTI], W=[XBUF_d])
        if True:
            o = outp("CNT", [1, NE])
            k.dma("sp", o[0:1, :], carry[0:1, :], R=[carry], W=[o])
    k.es = es
    k.regen()
    if stop_after <= 7:
        k.finish(list(outs.values()))
        return nc, es


    wegu_d = inp("w_e_gate_up", [NE * 128, 8 * 512]); wed_d = inp("w_e_down", [NE * 128, 2 * D])
    NBX = int(os.environ.get("NBX", str(NBLK)))
    CT = BLK // 128
    with ExitStack() as es8:
        k.es = es8
        k.imax = 12
        gst = [k.sb("gst%d" % i, [128, 8, 512]) for i in range(2)]
        dst_ = [k.sb("dst%d" % i, [128, 2, D]) for i in range(2)]
        wgu = [k.sb("wgu%d" % i, [128, 8, 512], BF16) for i in range(2)]
        wd = [k.sb("wd%d" % i, [128, 2, D], BF16) for i in range(2)]
        xin = [k.sb("xin%d" % i, [128, D], BF16) for i in range(4)]
        xT = [k.sb("xT%d" % i, [128, 8, BLK], BF16) for i in range(2)]
        sg = [k.sb("sg%d" % i, [128, BLK]) for i in range(2)]
        actT = [k.sb("actT%d" % i, [128, 2, BLK], BF16) for i in range(2)]
        yo = [k.sb("yo4_%d" % i, [128, D], BF16) for i in range(4)]
        pTx = k.ps("pTx", [128, 8, 128], BF16)
        pu = [k.ps("pu4_%d" % i, [128, 512]) for i in range(4)]
        pd = k.ps("pd", [128, 2, 512])
        nx = 0; ny = 0
        for jb in range(NBX):
            g_ = gst[jb % 2]; d_ = dst_[jb % 2]; wg = wgu[jb % 2]; wd_ = wd[jb % 2]; xT_ = xT[jb % 2]; aT = actT[jb % 2]
            k.idma(out=g_[:].rearrange("p j n -> p (j n)"), out_offset=None, in_=wegu_d[:, :], in_offset=bass.IndirectOffsetOnAxis(ap=IDXG[:, jb:jb + 1], axis=0),
                   R=[wegu_d, IDXG], W=[g_])
            k.idma(out=d_[:].rearrange("p c n -> p (c n)"), out_offset=None, in_=wed_d[:, :], in_offset=bass.IndirectOffsetOnAxis(ap=IDXG[:, jb:jb + 1], axis=0),
                   R=[wed_d, IDXG], W=[d_])
            k.op("dve", lambda e: e.tensor_copy(wg[:, 0:5, :], g_[:, 0:5, :]), R=[g_], W=[wg])
            k.op("act", lambda e: e.copy(wg[:, 5:8, :], g_[:, 5:8, :]), R=[g_], W=[wg])
            k.op("act", lambda e: e.copy(wd_[:], d_[:]), R=[d_], W=[wd_])
            for tt in range(CT):
                xi = xin[nx % 4]; nx += 1
                k.dma("sp", xi[:], XBUF_d[jb * BLK + tt * 128:jb * BLK + (tt + 1) * 128, :], R=[XBUF_d], W=[xi])
                for j in range(8):
                    k.op("pe", lambda e: e.transpose(pTx[:, j, :], xi[:, j * 128:(j + 1) * 128], ident_b[:]), R=[xi, ident_b], W=[pTx])
                k.op("act" if tt % 2 else "dve", (lambda e: e.copy(xT_[:, :, tt * 128:(tt + 1) * 128], pTx[:])) if tt % 2 else
                     (lambda e: e.tensor_copy(xT_[:, :, tt * 128:(tt + 1) * 128], pTx[:])), R=[pTx], W=[xT_])
            for c in range(4):
                for j in range(8):
                    k.op("pe", lambda e: e.matmul(pu[c][:, 0:BLK], wg[:, j, c * 128:(c + 1) * 128], xT_[:, j, :], start=(j == 0), stop=(j == 7)), R=[wg, xT_], W=[pu[c]])
            for c in range(2):
                s_ = sg[c]
                k.op("act", lambda e: e.activation(s_[:], pu[c][:, 0:BLK], ACT.Silu), R=[pu[c]], W=[s_])
                k.op("dve", lambda e: e.tensor_tensor(aT[:, c, :], s_[:], pu[2 + c][:, 0:BLK], op=ALU.mult), R=[s_, pu[2 + c]], W=[aT])
            for tt in range(CT):
                for hh in range(2):
                    for c in range(2):
                        k.op("pe", lambda e: e.matmul(pd[:, hh, :], aT[:, c, tt * 128:(tt + 1) * 128], wd_[:, c, hh * 512:(hh + 1) * 512], start=(c == 0), stop=(c == 1)), R=[aT, wd_], W=[pd])
                y_ = yo[ny % 4]; ny += 1
                k.op("dve", lambda e: e.tensor_copy(y_[:], pd[:].rearrange("p a n -> p (a n)")), R=[pd], W=[y_])
                k.dma("sp", YBUF_d[jb * BLK + tt * 128:jb * BLK + (tt + 1) * 128, :], y_[:], R=[y_], W=[YBUF_d])
        k.imax = 6
    k.es = es
    k.regen()
    if stop_after <= 8:
        k.finish(list(outs.values()))
        return nc, es

    wsgu_d = inp("w_sh_gate_up", [D, 512]); wsd_d = inp("w_sh_down", [256, D])
    OUT_d = outp("out", [NTOK, D])
    TB5 = min(4, NTT)
    with ExitStack() as es9:
        k.es = es9
        gst = k.sb("sgst", [128, 8, 512]); dst_ = k.sb("sdst", [128, 2, D])
        wg = k.sb("swgu", [128, 8, 512], BF16); wd_ = k.sb("swd", [128, 2, D], BF16)
        k.dma("sp", gst[:], wsgu_d[:].rearrange("(j p) n -> p j n", p=128), W=[gst])
        k.dma("sp", dst_[:], wsd_d[:].rearrange("(c p) n -> p c n", p=128), W=[dst_])
        k.op("dve", lambda e: e.tensor_copy(wg[:], gst[:]), R=[gst], W=[wg])
        k.op("pool", lambda e: e.tensor_copy(wd_[:], dst_[:]), R=[dst_], W=[wd_])
        xTs = [k.sb("xTs%d" % i, [128, 8, TB5 * 128], BF16) for i in range(2)]
        sg = [k.sb("sg5_%d" % i, [128, TB5 * 128]) for i in range(2)]
        aT5 = [k.sb("aT5_%d" % i, [128, 2, TB5 * 128], BF16) for i in range(2)]
        yg = [k.sb("yg%d" % i, [128, D], BF16) for i in range(4)]
        for y_ in yg:
            k.op("pool", lambda e: e.memset(y_[:], 0.0), W=[y_])
        acc = [k.sb("acc%d" % i, [128, D]) for i in range(2)]
        x1t = [k.sb("x1t%d" % i, [128, D]) for i in range(2)]
        pu = [k.ps("pu5_%d" % i, [128, 512]) for i in range(4)]
        pd = [k.ps("pd5_%d" % i, [128, 2, 512]) for i in range(2)]
        nyg = 0
        for blk in range(NTT // TB5):
            xT_ = xTs[blk % 2]; aT = aT5[blk % 2]
            W5 = TB5 * 128
            k.dma("sp", xT_[:], H2T_d[:, :, blk * W5:(blk + 1) * W5], R=[H2T_d], W=[xT_])
            for c in range(4):
                for j in range(8):
                    k.op("pe", lambda e: e.matmul(pu[c][:, 0:W5], wg[:, j, c * 128:(c + 1) * 128], xT_[:, j, :], start=(j == 0), stop=(j == 7)), R=[wg, xT_], W=[pu[c]])
            for c in range(2):
                s_ = sg[c]
                k.op("act", lambda e: e.activation(s_[:], pu[c][:, 0:W5], ACT.Silu), R=[pu[c]], W=[s_])
                k.op("dve", lambda e: e.tensor_tensor(aT[:, c, :], s_[:], pu[2 + c][:, 0:W5], op=ALU.mult), R=[s_, pu[2 + c]], W=[aT])
            for tt in range(TB5):
                i = blk * TB5 + tt; b = i // NT
                pd_ = pd[i % 2]; a_ = acc[i % 2]; x1_ = x1t[i % 2]
                for hh in range(2):
                    for c in range(2):
                        k.op("pe", lambda e: e.matmul(pd_[:, hh, :], aT[:, c, tt * 128:(tt + 1) * 128], wd_[:, c, hh * 512:(hh + 1) * 512], start=(c == 0), stop=(c == 1)), R=[aT, wd_], W=[pd_])
                k.dma("sp", x1_[:], X1_d[i * 128:(i + 1) * 128, :], R=[X1_d], W=[x1_])
                k.op("act", lambda e: e.copy(a_[:], pd_[:].rearrange("p a n -> p (a n)")), R=[pd_], W=[a_])
                for ks in range(8):
                    y_ = yg[nyg % 4]; nyg += 1
                    k.idma(out=y_[:, :], out_offset=None, in_=YBUF_d[:, :], in_offset=bass.IndirectOffsetOnAxis(ap=DESTI[:, i, ks:ks + 1], axis=0),
                           bounds_check=BREG, oob_is_err=False, R=[YBUF_d, DESTI], W=[y_])
                    k.op("dve", lambda e: e.scalar_tensor_tensor(a_[:], y_[:], WK[:, i, ks:ks + 1], a_[:], op0=ALU.mult, op1=ALU.add), R=[y_, WK, a_], W=[a_])
                k.op("dve", lambda e: e.tensor_tensor(a_[:], a_[:], MOD[b][5][:], op=ALU.mult), R=[a_, MOD[b][5]], W=[a_])
                k.op("pool", lambda e: e.tensor_tensor(a_[:], a_[:], x1_[:], op=ALU.add), R=[a_, x1_], W=[a_])
                k.dma("sp", OUT_d[i * 128:(i + 1) * 128, :], a_[:], R=[a_], W=[OUT_d])
    k.es = es
    k.regen()
    k.finish(list(outs.values()))
    return nc, es


_CACHE = {}


def kernel(**inputs):
    T = 4096
    ncores = 8
    if "nc" not in _CACHE:
        _CACHE["nc"] = build(T)
    nc, es = _CACHE["nc"]
    names = [a.memorylocations[0].name for a in nc.allocations
             if hasattr(a, "kind") and a.kind == "ExternalInput" and a.memorylocations[0].name != "partition_id"]
    shared = {}
    for n in names:
        if n in ("x", "c", "positions"):
            continue
        a = np.asarray(inputs[n])[0]
        if n == "rwkv_r_k":
            a = a.reshape(1, 512)
        elif n == "w_e_gate_up":
            a = a.reshape(256, 8, 128, 512).transpose(0, 2, 1, 3).reshape(256 * 128, 8 * 512)
        elif n == "w_e_down":
            a = a.reshape(256, 2, 128, 1024).transpose(0, 2, 1, 3).reshape(256 * 128, 2 * 1024)
        elif a.ndim == 1:
            a = a.reshape(1, -1)
        shared[n] = np.ascontiguousarray(a)
    x = np.asarray(inputs["x"]); c = np.asarray(inputs["c"]); pos = np.asarray(inputs["positions"])
    in_maps = []
    for cid in range(ncores):
        m = dict(shared)
        m["x"] = np.ascontiguousarray(x[2 * cid:2 * cid + 2].reshape(2 * T, 1024))
        m["c"] = np.ascontiguousarray(c[2 * cid:2 * cid + 2])
        m["positions"] = np.ascontiguousarray(pos[2 * cid:2 * cid + 2].reshape(2 * T, 1).astype(np.int32))
        in_maps.append(m)
    res = run_bass_kernel_spmd(nc, in_maps, core_ids=list(range(ncores)))
    try:
        print("max expert count per core:", [int(r["CNT"].max()) for r in res.results], flush=True)
    except Exception:
        pass
    out = np.stack([r["out"].reshape(2, T, 1024) for r in res.results], 0).reshape(16, T, 1024)
    return out.astype(np.float32)
```

```python
import os
import numpy as np
from contextlib import ExitStack
import concourse.bass as bass
import concourse.mybir as mybir
from concourse.bass_utils import run_bass_kernel_spmd


F32 = mybir.dt.float32
F32R = mybir.dt.float32r
BF16 = mybir.dt.bfloat16
I32 = mybir.dt.int32
U32 = mybir.dt.uint32
ACT = mybir.ActivationFunctionType
ALU = mybir.AluOpType
AX = mybir.AxisListType


class Buf:
    __slots__ = ("t", "lw", "rd", "name")

    def __init__(self, t, name=""):
        self.t = t
        self.lw = None
        self.rd = {}
        self.name = name

    def __getitem__(self, k):
        return self.t[k]


class K:
    def __init__(self, nc, es, ndma=24):
        self.nc = nc
        self.es = es
        self.eng = {"pe": nc.tensor, "act": nc.scalar, "dve": nc.vector, "pool": nc.gpsimd, "sp": nc.sync}
        self.sem = {}
        self.cnt = {}
        for n in ("pe", "act", "dve", "pool"):
            self.sem[n] = es.enter_context(nc.semaphore("s_" + n))
            self.cnt[n] = 0
        self.ndma = ndma
        self.dsem = [es.enter_context(nc.semaphore("s_dma%d" % i)) for i in range(ndma)]
        self.dcnt = [0] * ndma
        self.dnext = 0
        self.waited = {n: {} for n in ("pe", "act", "dve", "pool", "sp")}
        self.nins = 0
        self.imax = 6
        self.gen = 0
        self.top_es = es

    def sb(self, name, shape, dt=F32):
        return Buf(self.es.enter_context(self.nc.sbuf_tensor(name, list(shape), dt)), name)

    def ps(self, name, shape, dt=F32):
        return Buf(self.es.enter_context(self.nc.psum_tensor(name, list(shape), dt)), name)

    def dram(self, name, shape, dt=F32, kind="Internal"):
        return Buf(self.nc.dram_tensor(name, list(shape), dt, kind=kind).ap(), name)

    def _semof(self, key):
        return self.dsem[key[1]] if isinstance(key, tuple) else self.sem[key]

    def _wait(self, e, key, val, gen=None):
        if key == e and e == "pe":
            return
        if gen is not None and gen < self.gen:
            return
        w = self.waited[e]
        if w.get(key, 0) >= val:
            return
        self.eng[e].wait_ge(self._semof(key), val)
        w[key] = val
        self.nins += 1

    def _deps(self, e, R, W):
        for b in R:
            if b.lw is not None:
                self._wait(e, *b.lw)
        for b in W:
            if b.lw is not None:
                self._wait(e, *b.lw)
            for k, (v, g) in b.rd.items():
                self._wait(e, k, v, g)

    def _done(self, key, val, R, W):
        g = None if isinstance(key, tuple) else self.gen
        for b in R:
            o = b.rd.get(key)
            if o is None or o[1] != g or o[0] < val:
                b.rd[key] = (val, g)
        for b in W:
            b.lw = (key, val, g)
            b.rd = {}

    def op(self, e, fn, R=(), W=()):
        self._deps(e, R, W)
        ins = fn(self.eng[e])
        self.cnt[e] += 1
        ins.then_inc(self.sem[e], 1)
        self._done(e, self.cnt[e], R, W)
        self.nins += 1
        return ins

    def dma(self, q, out, in_, R=(), W=(), **kw):
        i = self.dnext
        self.dnext = (self.dnext + 1) % self.ndma
        key = ("dma", i)
        if self.dcnt[i] > 0:
            self._wait(q, key, self.dcnt[i])
        self._deps(q, R, W)
        ins = self.eng[q].dma_start(out=out, in_=in_, **kw)
        self.dcnt[i] += 16
        ins.then_inc(self.dsem[i], 16)
        self._done(key, self.dcnt[i], R, W)
        self.nins += 1
        return ins

    def idma(self, R=(), W=(), **kw):
        q = "pool"
        if not hasattr(self, "ipend"):
            self.ipend = []
        while len(self.ipend) >= self.imax:
            kk, vv = self.ipend.pop(0)
            self._wait("pool", kk, vv)
        i = self.dnext
        self.dnext = (self.dnext + 1) % self.ndma
        key = ("dma", i)
        if self.dcnt[i] > 0:
            self._wait(q, key, self.dcnt[i])
        self._deps(q, R, W)
        ins = self.nc.gpsimd.indirect_dma_start(**kw)
        self.dcnt[i] += 16
        ins.then_inc(self.dsem[i], 16)
        self.ipend.append((key, self.dcnt[i]))
        self._done(key, self.dcnt[i], R, W)
        self.nins += 1
        return ins

    def barrier(self):
        for e in ("pe", "act", "dve", "pool", "sp"):
            for o in ("pe", "act", "dve", "pool"):
                if o != e and self.cnt[o] > 0:
                    self._wait(e, o, self.cnt[o])
            for i in range(self.ndma):
                if self.dcnt[i] > 0:
                    self._wait(e, ("dma", i), self.dcnt[i])

    def regen(self):
        self.barrier()
        self.gen += 1
        for n in ("pe", "act", "dve", "pool"):
            self.sem[n] = self.top_es.enter_context(self.nc.semaphore("s_%s_g%d" % (n, self.gen)))
            self.cnt[n] = 0
        for e in self.waited:
            for n in ("pe", "act", "dve", "pool"):
                self.waited[e].pop(n, None)

    def finish(self, outs):
        for b in outs:
            if b.lw is not None:
                self._wait("sp", *b.lw)


import os
SKIP = os.environ.get('SKIP', '').split(',')
D = 1024
DIN = 2112
NB = 2
EPS = 1e-6


def build(T, stop_after=99, dbg=()):
    nc = bass.Bass("TRN2", target_bir_lowering=False)
    es = ExitStack()
    k = K(nc, es)
    NT = T // 128
    NTOK = NB * T

    def inp(name, shape, dt=F32):
        return Buf(nc.dram_tensor(name, list(shape), dt, kind="ExternalInput").ap(), name)

    x_d = inp("x", [NB * T, D])
    c_d = inp("c", [NB, D])
    ada_w_d = inp("ada_w", [D, 6 * D])
    ada_b_d = inp("ada_b", [1, 6 * D])
    norm_mix_d = inp("norm_mix", [1, D])
    w_in_d = inp("w_in", [D, DIN])
    outs = {}

    def outp(name, shape, dt=F32):
        b = Buf(nc.dram_tensor(name, list(shape), dt, kind="ExternalOutput").ap(), name)
        outs[name] = b
        return b

    ident_f = k.sb("ident_f", [128, 128], F32)
    ident_b = k.sb("ident_b", [128, 128], BF16)
    iot = k.sb("iot", [128, 128], I32)
    k.op("pool", lambda e: e.iota(iot[:], pattern=[[1, 128]], base=0, channel_multiplier=-1), W=[iot])
    k.op("dve", lambda e: e.tensor_scalar(ident_f[:], iot[:], 0.0, None, op0=ALU.is_equal), R=[iot], W=[ident_f])
    k.op("dve", lambda e: e.tensor_copy(ident_b[:], ident_f[:]), R=[ident_f], W=[ident_b])

    MOD = [[k.sb("mod%d_%d" % (b, w), [128, D]) for w in range(6)] for b in range(NB)]
    with ExitStack() as es0:
        k.es = es0
        cT = k.sb("cT", [128, NB, 8])
        cS = k.sb("cS", [128, NB, 8])
        with nc.allow_non_contiguous_dma(reason="tiny c transpose load"):
            for b in range(NB):
                k.dma("sp", cT[:, b, :], c_d[b, :].rearrange("(j p) -> p j", p=128), W=[cT])
        k.op("act", lambda e: e.activation(cS[:], cT[:], ACT.Silu), R=[cT], W=[cS])
        cB = [[k.sb("cB%d_%d" % (b, j), [128, 128]) for j in range(8)] for b in range(NB)]
        for b in range(NB):
            for j in range(8):
                k.op("dve", lambda e: e.tensor_copy(cB[b][j][:], cS[:, b, j:j + 1].to_broadcast([128, 128])),
                     R=[cS], W=[cB[b][j]])
        awb = [k.sb("awb%d" % i, [128, 8, 512]) for i in range(2)]
        abb = [k.sb("abb%d" % i, [128, 512]) for i in range(2)]
        pm = [k.ps("pm%d" % i, [128, 512]) for i in range(2)]
        for cb in range(12):
            aw = awb[cb % 2]
            ab = abb[cb % 2]
            k.dma("sp", aw[:], ada_w_d[:, cb * 512:(cb + 1) * 512].rearrange("(j p) n -> p j n", p=128), W=[aw])
            k.dma("sp", ab[:], ada_b_d[0:1, cb * 512:(cb + 1) * 512].partition_broadcast(128), W=[ab])
            for b in range(NB):
                p = pm[b]
                for j in range(8):
                    k.op("pe", lambda e: e.matmul(p[:], cB[b][j][:], aw[:, j, :], start=(j == 0), stop=(j == 7)),
                         R=[cB[b][j], aw], W=[p])
                dst = MOD[b][cb // 2]
                k.op("dve", lambda e: e.tensor_tensor(dst[:, (cb % 2) * 512:(cb % 2 + 1) * 512], p[:], ab[:], op=ALU.add),
                     R=[p, ab], W=[dst])
        nmb = k.sb("nmb", [128, D])
        k.dma("sp", nmb[:], norm_mix_d[0:1, :].partition_broadcast(128), W=[nmb])
        for b in range(NB):
            m = MOD[b][1]
            k.op("dve", lambda e: e.scalar_tensor_tensor(m[:], m[:], 1.0, nmb[:], op0=ALU.add, op1=ALU.mult),
                 R=[m, nmb], W=[m])
    k.es = es
    k.regen()
    if "mod" in dbg:
        o = outp("dbg_mod", [NB, 6, D])
        for b in range(NB):
            for w in range(6):
                k.dma("sp", o[b, w:w + 1, :], MOD[b][w][0:1, :], R=[MOD[b][w]], W=[o])
    if stop_after <= 0:
        k.finish(list(outs.values()))
        return nc, es

    U_d = outp("U", [NTOK, DIN]) if "U" in dbg else k.dram("U", [NTOK, DIN])
    with ExitStack() as es1:
        k.es = es1
        win = k.sb("win", [128, 8, DIN], BF16)
        wst = [k.sb("wst%d" % i, [128, DIN]) for i in range(2)]
        for j in range(8):
            s = wst[j % 2]
            k.dma("sp", s[:], w_in_d[j * 128:(j + 1) * 128, :], W=[s])
            k.op("pool" if j % 2 else "dve", lambda e: e.tensor_copy(win[:, j, :], s[:]), R=[s], W=[win])
        xt = [k.sb("xt%d" % i, [128, D]) for i in range(2)]
        junk = k.sb("junk", [128, D])
        ssq = [k.sb("ssq%d" % i, [128, 1]) for i in range(2)]
        rstd = [k.sb("rstd%d" % i, [128, 1]) for i in range(2)]
        hn = k.sb("hn", [128, D])
        hb = [k.sb("hb%d" % i, [128, D], BF16) for i in range(2)]
        hT = [k.sb("hT%d" % i, [128, 8, 128], BF16) for i in range(2)]
        ut = [k.sb("ut%d" % i, [128, DIN]) for i in range(2)]
        pT = [k.ps("pT%d" % i, [128, 8, 128], BF16) for i in range(2)]
        pu = [k.ps("pu%d" % i, [128, 512]) for i in range(3)]
        npu = 0
        for i in range(NB * NT):
            b = i // NT
            x_ = xt[i % 2]; sq = ssq[i % 2]; rs = rstd[i % 2]; h_ = hb[i % 2]; hT_ = hT[i % 2]; u_ = ut[i % 2]; pT_ = pT[i % 2]
            k.dma("sp", x_[:], x_d[i * 128:(i + 1) * 128, :], W=[x_])
            k.op("act", lambda e: e.activation(junk[:], x_[:], ACT.Square, accum_out=sq[:]), R=[x_], W=[junk, sq])
            k.op("dve", lambda e: e.tensor_scalar(rs[:], sq[:], 1.0 / D, EPS, op0=ALU.mult, op1=ALU.add), R=[sq], W=[rs])
            k.op("act", lambda e: e.sqrt(rs[:], rs[:]), R=[rs], W=[rs])
            k.op("dve", lambda e: e.reciprocal(rs[:], rs[:]), R=[rs], W=[rs])
            k.op("dve", lambda e: e.scalar_tensor_tensor(hn[:], x_[:], rs[:], MOD[b][1][:], op0=ALU.mult, op1=ALU.mult),
                 R=[x_, rs, MOD[b][1]], W=[hn])
            k.op("dve", lambda e: e.tensor_tensor(h_[:], hn[:], MOD[b][0][:], op=ALU.add), R=[hn, MOD[b][0]], W=[h_])
            for j in range(8):
                k.op("pe", lambda e: e.transpose(pT_[:, j, :], h_[:, j * 128:(j + 1) * 128], ident_b[:]),
                     R=[h_, ident_b], W=[pT_])
            k.op("act", lambda e: e.copy(hT_[:], pT_[:]), R=[pT_], W=[hT_])
            for cbi, (c0, c1) in enumerate([(0, 512), (512, 1024), (1024, 1536), (1536, 2048), (2048, 2112)]):
                p = pu[npu % 3]; npu += 1
                for j in range(8):
                    k.op("pe", lambda e: e.matmul(p[:, 0:c1 - c0], hT_[:, j, :], win[:, j, c0:c1], start=(j == 0), stop=(j == 7)),
                         R=[hT_, win], W=[p])
                k.op("dve" if cbi % 2 else "act",
                     (lambda e: e.tensor_copy(u_[:, c0:c1], p[:, 0:c1 - c0])) if cbi % 2 else
                     (lambda e: e.copy(u_[:, c0:c1], p[:, 0:c1 - c0])), R=[p], W=[u_])
            k.dma("sp", U_d[i * 128:(i + 1) * 128, :], u_[:], R=[u_], W=[U_d])
    k.es = es
    k.regen()
    if stop_after <= 1:
        k.finish(list(outs.values()))
        return nc, es


    rwkv_mu_d = inp("rwkv_mu", [1, 1696]); decay_w0_d = inp("decay_w0", [1, 512]); decay_up_d = inp("decay_up", [32, 512])
    iclr_a0_d = inp("iclr_a0", [1, 512]); iclr_up_d = inp("iclr_up", [32, 512]); gate_up_d = inp("gate_up", [96, 512])
    k_k_d = inp("rwkv_k_k", [1, 512]); k_a_d = inp("rwkv_k_a", [1, 512]); r_k_d = inp("rwkv_r_k", [1, 512])
    ROWS_d = outp("ROWS", [NB, T, 5, 512]) if "ROWS" in dbg else None
    ROWSW_d = k.dram("ROWSW", [NB, T, 512])
    ROWSB_d = k.dram("ROWSB", [NB, T, 4, 512], BF16)
    VT_d = outp("VT", [128, NT, 8, 128]) if "VT" in dbg else k.dram("VT", [128, NT, 8, 128])
    BON_d = outp("BON", [NTOK, 512]) if "BON" in dbg else k.dram("BON", [NTOK, 512])
    G_d = outp("G", [NTOK, 512]) if "G" in dbg else k.dram("G", [NTOK, 512])
    with ExitStack() as es2:
        k.es = es2
        def bc(name, src, n):
            t_ = k.sb(name, [128, n])
            k.dma("sp", t_[:], src[0:1, :].partition_broadcast(128), W=[t_])
            return t_
        mu_b = bc("mu_b", rwkv_mu_d, 1696); w0_b = bc("w0_b", decay_w0_d, 512); a0_b = bc("a0_b", iclr_a0_d, 512)
        kk_b = bc("kk_b", k_k_d, 512); ka_b = bc("ka_b", k_a_d, 512); rk_b = bc("rk_b", r_k_d, 512)
        dup = k.sb("dup", [32, 512]); iup = k.sb("iup", [32, 512]); gup = k.sb("gup", [96, 512])
        k.dma("sp", dup[:], decay_up_d[:], W=[dup]); k.dma("sp", iup[:], iclr_up_d[:], W=[iup]); k.dma("sp", gup[:], gate_up_d[:], W=[gup])
        uu = [k.sb("uu%d" % i, [128, 1696]) for i in range(2)]
        up_ = [k.sb("up%d" % i, [128, 1696]) for i in range(2)]
        us = k.sb("us", [128, 1696])
        Z = k.sb("Z", [128, 160]); ZT = k.sb("ZT", [96, 3, 128])
        rows = [k.sb("rows%d" % i, [128, 5, 512]) for i in range(2)]
        rowsb = [k.sb("rowsb%d" % i, [128, 4, 512], BF16) for i in range(2)]
        gt = [k.sb("gt%d" % i, [128, 512]) for i in range(2)]
        bon = [k.sb("bon%d" % i, [128, 512]) for i in range(2)]
        at = k.sb("at", [128, 512]); t5 = k.sb("t5", [128, 512]); t6 = k.sb("t6", [128, 512])
        s8 = k.sb("s8", [128, 8]); b8 = k.sb("b8", [128, 8])
        vts = [k.sb("vts%d" % i, [64, 8, 128]) for i in range(2)]
        pZ = k.ps("pZ", [128, 3, 128]); pl = k.ps("pl", [128, 3, 512]); pv = k.ps("pv", [64, 8, 128])
        for i in range(NB * NT):
            b = i // NT; it = i % NT
            u_ = uu[i % 2]; p_ = up_[i % 2]; rw = rows[i % 2]; g_ = gt[i % 2]; bo = bon[i % 2]; vt_ = vts[i % 2]
            k.dma("sp", u_[:], U_d[i * 128:(i + 1) * 128, 0:1696], R=[U_d], W=[u_])
            if it == 0:
                k.op("pool", lambda e: e.memset(p_[0:1, :], 0.0), W=[p_])
                k.dma("sp", p_[1:128, :], U_d[i * 128:i * 128 + 127, 0:1696], R=[U_d], W=[p_])
            else:
                k.dma("sp", p_[:], U_d[i * 128 - 1:i * 128 + 127, 0:1696], R=[U_d], W=[p_])
            k.op("pool", lambda e: e.tensor_tensor(p_[:], p_[:], u_[:], op=ALU.subtract), R=[p_, u_], W=[p_])
            k.op("pool", lambda e: e.tensor_tensor(p_[:], p_[:], mu_b[:], op=ALU.mult), R=[p_, mu_b], W=[p_])
            k.op("dve", lambda e: e.tensor_tensor(us[:], p_[:], u_[:], op=ALU.add), R=[p_, u_], W=[us])
            r_ = us[:, 0:512]; kx = us[:, 512:1024]; v_ = us[:, 1024:1536]
            k.op("act", lambda e: e.activation(Z[:, 0:32], us[:, 1536:1568], ACT.Tanh), R=[us], W=[Z])
            k.op("act", lambda e: e.copy(Z[:, 32:64], us[:, 1568:1600]), R=[us], W=[Z])
            k.op("act", lambda e: e.activation(Z[:, 64:160], us[:, 1600:1696], ACT.Sigmoid), R=[us], W=[Z])
            k.op("pe", lambda e: e.transpose(pZ[0:32, 0, :], Z[:, 0:32], ident_f[:]), R=[Z, ident_f], W=[pZ])
            k.op("pe", lambda e: e.transpose(pZ[0:32, 1, :], Z[:, 32:64], ident_f[:]), R=[Z, ident_f], W=[pZ])
            k.op("pe", lambda e: e.transpose(pZ[0:96, 2, :], Z[:, 64:160], ident_f[:]), R=[Z, ident_f], W=[pZ])
            k.op("dve", lambda e: e.tensor_copy(ZT[0:32, 0:2, :], pZ[0:32, 0:2, :]), R=[pZ], W=[ZT])
            k.op("dve", lambda e: e.tensor_copy(ZT[0:96, 2, :], pZ[0:96, 2, :]), R=[pZ], W=[ZT])
            k.op("pe", lambda e: e.matmul(pl[:, 0, :], ZT[0:32, 0, :], dup[:], start=True, stop=True), R=[ZT, dup], W=[pl])
            k.op("pe", lambda e: e.matmul(pl[:, 1, :], ZT[0:32, 1, :], iup[:], start=True, stop=True), R=[ZT, iup], W=[pl])
            k.op("pe", lambda e: e.matmul(pl[:, 2, :], ZT[0:96, 2, :], gup[:], start=True, stop=True), R=[ZT, gup], W=[pl])
            k.op("dve", lambda e: e.tensor_tensor(t5[:], pl[:, 0, :], w0_b[:], op=ALU.add), R=[pl, w0_b], W=[t5])
            k.op("act", lambda e: e.activation(t5[:], t5[:], ACT.Sigmoid), R=[t5], W=[t5])
            k.op("act", lambda e: e.activation(rw[:, 0, :], t5[:], ACT.Exp, scale=-float(np.exp(-0.5))), R=[t5], W=[rw])
            k.op("dve", lambda e: e.tensor_tensor(at[:], pl[:, 1, :], a0_b[:], op=ALU.add), R=[pl, a0_b], W=[at])
            k.op("act", lambda e: e.activation(at[:], at[:], ACT.Sigmoid), R=[at], W=[at])
            k.op("act", lambda e: e.copy(g_[:], pl[:, 2, :]), R=[pl], W=[g_])
            k.op("dve", lambda e: e.tensor_tensor(rw[:, 1, :], kx, kk_b[:], op=ALU.mult), R=[us, kk_b], W=[rw])
            k.op("pool", lambda e: e.tensor_tensor(t6[:], rw[:, 1, :], rw[:, 1, :], op=ALU.mult), R=[rw], W=[t6])
            k.op("dve", lambda e: e.tensor_reduce(s8[:], t6[:].rearrange("p (h n) -> p h n", h=8), axis=AX.X, op=ALU.add), R=[t6], W=[s8])
            k.op("dve", lambda e: e.tensor_scalar(s8[:], s8[:], 1e-24, None, op0=ALU.max), R=[s8], W=[s8])
            k.op("act", lambda e: e.sqrt(s8[:], s8[:]), R=[s8], W=[s8])
            k.op("dve", lambda e: e.reciprocal(s8[:], s8[:]), R=[s8], W=[s8])
            k.op("dve", lambda e: e.tensor_tensor(rw[:, 1, :].rearrange("p (h n) -> p h n", h=8), rw[:, 1, :].rearrange("p (h n) -> p h n", h=8),
                                                  s8[:].unsqueeze(2).to_broadcast([128, 8, 64]), op=ALU.mult), R=[rw, s8], W=[rw])
            k.op("pool", lambda e: e.tensor_tensor(rw[:, 2, :], rw[:, 1, :], at[:], op=ALU.mult), R=[rw, at], W=[rw])
            k.op("dve", lambda e: e.scalar_tensor_tensor(t5[:], at[:], -1.0, ka_b[:], op0=ALU.add, op1=ALU.mult), R=[at, ka_b], W=[t5])
            k.op("dve", lambda e: e.scalar_tensor_tensor(rw[:, 3, :], t5[:], 1.0, kx, op0=ALU.add, op1=ALU.mult), R=[t5, us], W=[rw])
            k.op("act", lambda e: e.copy(rw[:, 4, :], r_), R=[us], W=[rw])
            k.op("pool", lambda e: e.tensor_tensor(t6[:], rw[:, 3, :], r_, op=ALU.mult), R=[rw, us], W=[t6])
            k.op("pool", lambda e: e.tensor_tensor(t6[:], t6[:], rk_b[:], op=ALU.mult), R=[t6, rk_b], W=[t6])
            k.op("dve", lambda e: e.tensor_reduce(b8[:], t6[:].rearrange("p (h n) -> p h n", h=8), axis=AX.X, op=ALU.add), R=[t6], W=[b8])
            k.op("dve", lambda e: e.tensor_tensor(bo[:].rearrange("p (h n) -> p h n", h=8), v_.rearrange("p (h n) -> p h n", h=8),
                                                  b8[:].unsqueeze(2).to_broadcast([128, 8, 64]), op=ALU.mult), R=[us, b8], W=[bo])
            for h in range(8):
                k.op("pe", lambda e: e.transpose(pv[:, h, :], us[:, 1024 + h * 64:1024 + (h + 1) * 64], ident_f[:]), R=[us, ident_f], W=[pv])
            k.op("act", lambda e: e.copy(vt_[:], pv[:]), R=[pv], W=[vt_])
            rwb = rowsb[i % 2]
            k.op("act", lambda e: e.copy(rwb[:], rw[:, 1:5, :]), R=[rw], W=[rwb])
            if ROWS_d is not None:
                k.dma("sp", ROWS_d[b, it * 128:(it + 1) * 128, :, :], rw[:], R=[rw], W=[ROWS_d])
            k.dma("sp", ROWSW_d[b, it * 128:(it + 1) * 128, :], rw[:, 0, :], R=[rw], W=[ROWSW_d])
            k.dma("sp", ROWSB_d[b, it * 128:(it + 1) * 128, :, :], rwb[:], R=[rwb], W=[ROWSB_d])
            k.dma("sp", VT_d[b * 64:(b + 1) * 64, it, :, :], vt_[:], R=[vt_], W=[VT_d])
            k.dma("sp", BON_d[i * 128:(i + 1) * 128, :], bo[:], R=[bo], W=[BON_d])
            k.dma("sp", G_d[i * 128:(i + 1) * 128, :], g_[:], R=[g_], W=[G_d])
    k.es = es
    k.regen()
    if stop_after <= 2:
        k.finish(list(outs.values()))
        return nc, es


    TBS = 8
    YT_d = outp("YT", [128, T, 8]) if "YT" in dbg else k.dram("YT", [128, T, 8])
    with ExitStack() as es3:
        k.es = es3
        S = k.sb("S", [128, 512])
        t1 = k.sb("t1", [128, 512]); t2 = k.sb("t2", [128, 512])
        t3 = [k.sb("t3_%d" % i, [128, 512]) for i in range(2)]
        t4 = [k.sb("t4_%d" % i, [128, 512]) for i in range(2)]
        sa = k.sb("sa", [128, 8])
        RWt = [es3.enter_context(nc.sbuf_tensor("RW%d" % i, [128, TBS, 512], F32)) for i in range(2)]
        RBt = [es3.enter_context(nc.sbuf_tensor("RB%d" % i, [128, TBS, 4, 512], BF16)) for i in range(2)]
        RB0 = [Buf(t_, "rb0") for t_ in RBt]; RB1 = [Buf(t_, "rb1") for t_ in RBt]
        RW0 = [Buf(t_, "rw0") for t_ in RWt]; RW1 = [Buf(t_, "rw1") for t_ in RWt]
        VTc = [k.sb("VTc%d" % i, [128, 8, 128]) for i in range(2)]
        Yc = [k.sb("Yc%d" % i, [128, 128, 8]) for i in range(2)]
        k.op("dve", lambda e: e.memset(S[:], 0.0), W=[S])
        v3 = lambda ap: ap.rearrange("p (h n) -> p h n", h=8)
        nblk = 0; nst = 0; pend_y = None
        SW = [k.sb("SW%d" % i, [128, 512]) for i in range(2)]
        for it in range(NT):
            vt_ = VTc[it % 2]; y_ = Yc[it % 2]
            k.dma("sp", vt_[:], VT_d[:, it, :, :], R=[VT_d], W=[vt_])
            for blk in range(128 // TBS):
                t0 = it * 128 + blk * TBS
                rbt = RBt[nblk % 2]; rb0 = RB0[nblk % 2]; rb1 = RB1[nblk % 2]
                rwt = RWt[nblk % 2]; rw0 = RW0[nblk % 2]; rw1 = RW1[nblk % 2]; nblk += 1
                k.dma("sp", rbt[0:64].rearrange("p t f n -> p (t f n)"),
                      ROWSB_d[0:1, t0:t0 + TBS].rearrange("o t f n -> o (t f n)").partition_broadcast(64), R=[ROWSB_d], W=[rb0])
                k.dma("act", rbt[64:128].rearrange("p t f n -> p (t f n)"),
                      ROWSB_d[1:2, t0:t0 + TBS].rearrange("o t f n -> o (t f n)").partition_broadcast(64), R=[ROWSB_d], W=[rb1])
                k.dma("sp", rwt[0:64].rearrange("p t n -> p (t n)"),
                      ROWSW_d[0:1, t0:t0 + TBS].rearrange("o t n -> o (t n)").partition_broadcast(64), R=[ROWSW_d], W=[rw0])
                k.dma("act", rwt[64:128].rearrange("p t n -> p (t n)"),
                      ROWSW_d[1:2, t0:t0 + TBS].rearrange("o t n -> o (t n)").partition_broadcast(64), R=[ROWSW_d], W=[rw1])
                RB = [rb0, rb1]; RWB = [rw0, rw1]
                for s_ in range(TBS):
                    tl = blk * TBS + s_
                    Wb = rwt[:, s_, :]; KKb = rbt[:, s_, 0, :]; KKAb = rbt[:, s_, 1, :]; Kb = rbt[:, s_, 2, :]; Rb = rbt[:, s_, 3, :]
                    t3_ = t3[nst % 2]; t4_ = t4[nst % 2]; sw_ = SW[nst % 2]; nst += 1
                    def emit_t3(buf, sidx):
                        Kb_ = rbt[:, sidx, 2, :]
                        k.op("pool", lambda e: e.tensor_tensor(v3(buf[:]), v3(Kb_), vt_[:, :, blk * TBS + sidx].unsqueeze(2).to_broadcast([128, 8, 64]), op=ALU.mult),
                             R=RB + [vt_], W=[buf])
                    if s_ == 0:
                        emit_t3(t3_, s_)
                    if pend_y is not None:
                        pt4, pRb, pRB, pyd, ptl = pend_y
                        k.op("pool", lambda e: e.tensor_tensor(pt4[:], S[:], pRb, op=ALU.mult), R=[S] + pRB, W=[pt4])
                    k.op("dve", lambda e: e.tensor_tensor(t1[:], S[:], KKb, op=ALU.mult), R=[S] + RB, W=[t1])
                    k.op("dve", lambda e: e.tensor_reduce(sa[:], v3(t1[:]), axis=AX.X, op=ALU.add, negate=True), R=[t1], W=[sa])
                    k.op("pool", lambda e: e.tensor_tensor(v3(t2[:]), v3(KKAb), sa[:].unsqueeze(2).to_broadcast([128, 8, 64]), op=ALU.mult),
                         R=RB + [sa], W=[t2])
                    if s_ + 1 < TBS:
                        emit_t3(t3[nst % 2], s_ + 1)
                    k.op("dve", lambda e: e.tensor_tensor(S[:], S[:], Wb, op=ALU.mult), R=[S] + RWB, W=[S])
                    if pend_y is not None:
                        k.op("dve", lambda e: e.tensor_reduce(pyd[:, ptl, :], v3(pt4[:]), axis=AX.X, op=ALU.add), R=[pt4], W=[pyd])
                    k.op("dve", lambda e: e.tensor_tensor(S[:], S[:], t2[:], op=ALU.add), R=[S, t2], W=[S])
                    k.op("dve", lambda e: e.tensor_tensor(S[:], S[:], t3_[:], op=ALU.add), R=[S, t3_], W=[S])
                    pend_y = (t4_, Rb, RB, y_, tl)
            pt4, pRb, pRB, pyd, ptl = pend_y
            k.op("pool", lambda e: e.tensor_tensor(pt4[:], S[:], pRb, op=ALU.mult), R=[S] + pRB, W=[pt4])
            k.op("dve", lambda e: e.tensor_reduce(pyd[:, ptl, :], v3(pt4[:]), axis=AX.X, op=ALU.add), R=[pt4], W=[pyd])
            pend_y = None
            k.dma("sp", YT_d[:, it * 128:(it + 1) * 128, :], y_[:], R=[y_], W=[YT_d])
            if it % 12 == 11 and it != NT - 1:
                k.regen()
    k.es = es
    k.regen()
    if stop_after <= 3:
        k.finish(list(outs.values()))
        return nc, es

    ln_w_d = inp("ln_x_w", [1, 512]); ln_b_d = inp("ln_x_b", [1, 512])
    YCAT_d = outp("YCAT", [NTOK, 1024]) if "YCAT" in dbg else k.dram("YCAT", [NTOK, 1024])
    with ExitStack() as es4:
        k.es = es4
        lnw_b = k.sb("lnw_b", [128, 512]); lnb_b = k.sb("lnb_b", [128, 512])
        k.dma("sp", lnw_b[:], ln_w_d[0:1, :].partition_broadcast(128), W=[lnw_b])
        k.dma("sp", lnb_b[:], ln_b_d[0:1, :].partition_broadcast(128), W=[lnb_b])
        yc = [k.sb("ycp%d" % i, [128, 128, 8]) for i in range(2)]
        py = k.ps("py", [128, 8, 128])
        ytm = k.sb("ytm", [128, 8, 128])
        yb = k.sb("yb", [128, 8, 64]); yq = k.sb("yq", [128, 8, 64])
        m8 = k.sb("m8", [128, 8]); v8 = k.sb("v8", [128, 8])
        bo = [k.sb("pbo%d" % i, [128, 512]) for i in range(2)]; g_ = [k.sb("pg%d" % i, [128, 512]) for i in range(2)]
        yo = [k.sb("yo%d" % i, [128, 512]) for i in range(2)]
        n = 0
        for it in range(NT):
            y_ = yc[it % 2]
            k.dma("sp", y_[:], YT_d[:, it * 128:(it + 1) * 128, :], R=[YT_d], W=[y_])
            for h in range(8):
                k.op("pe", lambda e: e.transpose(py[:, h, :], y_[:, :, h], ident_f[:]), R=[y_, ident_f], W=[py])
            k.op("act", lambda e: e.copy(ytm[:], py[:]), R=[py], W=[ytm])
            for b in range(NB):
                i = b * NT + it
                bo_ = bo[n % 2]; gg = g_[n % 2]; yo_ = yo[n % 2]; n += 1
                k.dma("sp", bo_[:], BON_d[i * 128:(i + 1) * 128, :], R=[BON_d], W=[bo_])
                k.dma("sp", gg[:], G_d[i * 128:(i + 1) * 128, :], R=[G_d], W=[gg])
                ysl = ytm[:, :, b * 64:(b + 1) * 64]
                k.op("dve", lambda e: e.tensor_reduce(m8[:], ysl, axis=AX.X, op=ALU.add), R=[ytm], W=[m8])
                k.op("dve", lambda e: e.tensor_scalar(m8[:], m8[:], 1.0 / 64, None, op0=ALU.mult), R=[m8], W=[m8])
                k.op("dve", lambda e: e.tensor_tensor(yb[:], ysl, m8[:].unsqueeze(2).to_broadcast([128, 8, 64]), op=ALU.subtract), R=[ytm, m8], W=[yb])
                k.op("pool", lambda e: e.tensor_tensor(yq[:], yb[:], yb[:], op=ALU.mult), R=[yb], W=[yq])
                k.op("dve", lambda e: e.tensor_reduce(v8[:], yq[:], axis=AX.X, op=ALU.add), R=[yq], W=[v8])
                k.op("dve", lambda e: e.tensor_scalar(v8[:], v8[:], 1.0 / 64, 64e-5, op0=ALU.mult, op1=ALU.add), R=[v8], W=[v8])
                k.op("act", lambda e: e.sqrt(v8[:], v8[:]), R=[v8], W=[v8])
                k.op("dve", lambda e: e.reciprocal(v8[:], v8[:]), R=[v8], W=[v8])
                k.op("dve", lambda e: e.tensor_tensor(yb[:], yb[:], v8[:].unsqueeze(2).to_broadcast([128, 8, 64]), op=ALU.mult), R=[yb, v8], W=[yb])
                ybf = yb[:].rearrange("p h n -> p (h n)")
                k.op("dve", lambda e: e.tensor_tensor(yo_[:], ybf, lnw_b[:], op=ALU.mult), R=[yb, lnw_b], W=[yo_])
                k.op("pool", lambda e: e.tensor_tensor(yo_[:], yo_[:], lnb_b[:], op=ALU.add), R=[yo_, lnb_b], W=[yo_])
                k.op("pool", lambda e: e.tensor_tensor(yo_[:], yo_[:], bo_[:], op=ALU.add), R=[yo_, bo_], W=[yo_])
                k.op("dve", lambda e: e.tensor_tensor(yo_[:], yo_[:], gg[:], op=ALU.mult), R=[yo_, gg], W=[yo_])
                k.dma("sp", YCAT_d[i * 128:(i + 1) * 128, 0:512], yo_[:], R=[yo_], W=[YCAT_d])
    k.es = es
    k.regen()
    if stop_after <= 4:
        k.finish(list(outs.values()))
        return nc, es


    pos_d = inp("positions", [NTOK, 1], I32)
    qan_d = inp("q_a_norm", [1, 256]); wqb_d = inp("w_q_b", [256, 768]); kvan_d = inp("kv_a_norm", [1, 128])
    wkvb_d = inp("w_kv_b", [128, 1024]); qn_d = inp("q_norm", [1, 96]); kn_d = inp("k_norm", [1, 96])
    QT_d = outp("QT", [NB, 96, 8, T], BF16) if "QT" in dbg else k.dram("QT", [NB, 96, 8, T], BF16)
    KT_d = outp("KT", [NB, 96, 8, T], BF16) if "KT" in dbg else k.dram("KT", [NB, 96, 8, T], BF16)
    V_d = outp("V", [NB, T, 8, 66], BF16) if "V" in dbg else k.dram("V", [NB, T, 8, 66], BF16)
    TWO_PI = float(2 * np.pi)
    with ExitStack() as es5:
        k.es = es5
        def bc(name, src, n):
            t_ = k.sb(name, [128, n])
            k.dma("sp", t_[:], src[0:1, :].partition_broadcast(128), W=[t_])
            return t_
        qan_b = bc("qan_b", qan_d, 256); kvan_b = bc("kvan_b", kvan_d, 128); qn96 = bc("qn96", qn_d, 96); kn96 = bc("kn96", kn_d, 96)
        qn_b = k.sb("qn_b", [128, 8, 96]); kn_b = k.sb("kn_b", [128, 8, 96])
        k.op("dve", lambda e: e.tensor_copy(qn_b[:], qn96[:].unsqueeze(1).to_broadcast([128, 8, 96])), R=[qn96], W=[qn_b])
        k.op("dve", lambda e: e.tensor_copy(kn_b[:], kn96[:].unsqueeze(1).to_broadcast([128, 8, 96])), R=[kn96], W=[kn_b])
        wq_st = k.sb("wq_st", [128, 2, 768]); wq = k.sb("wq", [128, 2, 768], BF16)
        k.dma("sp", wq_st[:], wqb_d[:].rearrange("(c p) n -> p c n", p=128), W=[wq_st])
        k.op("dve", lambda e: e.tensor_copy(wq[:], wq_st[:]), R=[wq_st], W=[wq])
        wkv_st = k.sb("wkv_st", [128, 1024]); wkv = k.sb("wkv", [128, 1024], BF16)
        k.dma("sp", wkv_st[:], wkvb_d[:], W=[wkv_st])
        k.op("dve", lambda e: e.tensor_copy(wkv[:], wkv_st[:]), R=[wkv_st], W=[wkv])
        ji = k.sb("ji", [128, 16], I32); invf = k.sb("invf", [128, 16])
        k.op("pool", lambda e: e.iota(ji[:], pattern=[[1, 16]], base=0, channel_multiplier=0), W=[ji])
        k.op("dve", lambda e: e.tensor_copy(invf[:], ji[:]), R=[ji], W=[invf])
        k.op("act", lambda e: e.activation(invf[:], invf[:], ACT.Exp, scale=-float(np.log(10000.0) / 16)), R=[invf], W=[invf])
        um = [k.sb("um%d" % i, [128, 416]) for i in range(2)]
        posi = k.sb("posi", [128, 1], I32); posf = k.sb("posf", [128, 1])
        ang = k.sb("ang", [128, 32]); angi = k.sb("angi", [128, 32], I32); angf = k.sb("angf", [128, 32]); msk = k.sb("msk", [128, 32])
        cs = k.sb("cs", [128, 32])
        junkm = k.sb("junkm", [128, 256]); s1 = k.sb("s1", [128, 1]); s2 = k.sb("s2", [128, 1])
        qlb = k.sb("qlb", [128, 256], BF16); kvb = k.sb("kvb", [128, 128], BF16)
        qlT = k.sb("qlT", [128, 2, 128], BF16); kvT = k.sb("kvT", [128, 128], BF16)
        pq = k.ps("pq", [128, 2, 512]); pkv = k.ps("pkv", [128, 2, 512])
        ptr = k.ps("ptr", [128, 3, 128], BF16)
        ptq = k.ps("ptq", [96, 8, 128], BF16)
        qf = k.sb("qf", [128, 8, 96]); kf = k.sb("kf", [128, 8, 96]); sqt = k.sb("sqt", [128, 8, 96])
        r8 = k.sb("r8", [128, 8])
        ra = k.sb("ra", [128, 8, 16]); rb_ = k.sb("rb_", [128, 8, 16]); rc = k.sb("rc", [128, 8, 16]); rd_ = k.sb("rd_", [128, 8, 16])
        qfb = k.sb("qfb", [128, 8, 96], BF16)
        qTs = [k.sb("qTs%d" % i, [96, 8, 128], BF16) for i in range(2)]
        kTs = [k.sb("kTs%d" % i, [96, 8, 128], BF16) for i in range(2)]
        vs = [k.sb("vs%d" % i, [128, 8, 66], BF16) for i in range(2)]
        for vv_ in vs:
            k.op("pool", lambda e: e.memset(vv_[:, :, 64:66], 1.0), W=[vv_])

        def headnorm_rope(xf, gain_b, outT, dstT, b, it):
            k.op("pool", lambda e: e.tensor_tensor(sqt[:], xf[:], xf[:], op=ALU.mult), R=[xf], W=[sqt])
            k.op("dve", lambda e: e.tensor_reduce(r8[:], sqt[:], axis=AX.X, op=ALU.add), R=[sqt], W=[r8])
            k.op("dve", lambda e: e.tensor_scalar(r8[:], r8[:], 1.0 / 96, EPS, op0=ALU.mult, op1=ALU.add), R=[r8], W=[r8])
            k.op("act", lambda e: e.sqrt(r8[:], r8[:]), R=[r8], W=[r8])
            k.op("dve", lambda e: e.reciprocal(r8[:], r8[:]), R=[r8], W=[r8])
            k.op("dve", lambda e: e.tensor_tensor(xf[:], xf[:], r8[:].unsqueeze(2).to_broadcast([128, 8, 96]), op=ALU.mult), R=[xf, r8], W=[xf])
            k.op("dve", lambda e: e.tensor_tensor(xf[:], xf[:], gain_b[:], op=ALU.mult), R=[xf, gain_b], W=[xf])
            sinb = cs[:, 0:16].unsqueeze(1).to_broadcast([128, 8, 16]); cosb = cs[:, 16:32].unsqueeze(1).to_broadcast([128, 8, 16])
            x1 = xf[:, :, 64:80]; x2 = xf[:, :, 80:96]
            k.op("dve", lambda e: e.tensor_tensor(ra[:], x1, cosb, op=ALU.mult), R=[xf, cs], W=[ra])
            k.op("dve", lambda e: e.tensor_tensor(rb_[:], x2, sinb, op=ALU.mult), R=[xf, cs], W=[rb_])
            k.op("dve", lambda e: e.tensor_tensor(rc[:], x2, cosb, op=ALU.mult), R=[xf, cs], W=[rc])
            k.op("dve", lambda e: e.tensor_tensor(rd_[:], x1, sinb, op=ALU.mult), R=[xf, cs], W=[rd_])
            k.op("dve", lambda e: e.tensor_tensor(x1, ra[:], rb_[:], op=ALU.subtract), R=[ra, rb_], W=[xf])
            k.op("dve", lambda e: e.tensor_tensor(x2, rc[:], rd_[:], op=ALU.add), R=[rc, rd_], W=[xf])
            k.op("act", lambda e: e.copy(qfb[:], xf[:]), R=[xf], W=[qfb])
            for h in range(8):
                k.op("pe", lambda e: e.transpose(ptq[:, h, :], qfb[:, h, :], ident_b[:]), R=[qfb, ident_b], W=[ptq])
            k.op("act", lambda e: e.copy(outT[:], ptq[:]), R=[ptq], W=[outT])
            if "noQT" not in SKIP:
                k.dma("sp", dstT[b, :, :, it * 128:(it + 1) * 128], outT[:], R=[outT], W=[dstT])

        for i in range(NB * NT):
            b = i // NT; it = i % NT
            u_ = um[i % 2]
            k.dma("sp", u_[:], U_d[i * 128:(i + 1) * 128, 1696:2112], R=[U_d], W=[u_])
            k.dma("sp", posi[:], pos_d[i * 128:(i + 1) * 128, :], W=[posi])
            if "noROPE" not in SKIP:
                k.op("dve", lambda e: e.tensor_copy(posf[:], posi[:]), R=[posi], W=[posf])
                k.op("dve", lambda e: e.tensor_scalar(ang[:, 0:16], invf[:], posf[:], None, op0=ALU.mult), R=[invf, posf], W=[ang])
                k.op("dve", lambda e: e.tensor_scalar(ang[:, 16:32], ang[:, 0:16], float(np.pi / 2), None, op0=ALU.add), R=[ang], W=[ang])
                k.op("dve", lambda e: e.tensor_scalar(angf[:], ang[:], 1.0 / TWO_PI, None, op0=ALU.mult), R=[ang], W=[angf])
                k.op("dve", lambda e: e.tensor_copy(angi[:], angf[:]), R=[angf], W=[angi])
                k.op("dve", lambda e: e.tensor_copy(angf[:], angi[:]), R=[angi], W=[angf])
                k.op("dve", lambda e: e.scalar_tensor_tensor(ang[:], angf[:], -TWO_PI, ang[:], op0=ALU.mult, op1=ALU.add), R=[angf, ang], W=[ang])
                k.op("dve", lambda e: e.tensor_scalar(msk[:], ang[:], float(np.pi), -TWO_PI, op0=ALU.is_gt, op1=ALU.mult), R=[ang], W=[msk])
                k.op("dve", lambda e: e.tensor_tensor(ang[:], ang[:], msk[:], op=ALU.add), R=[ang, msk], W=[ang])
                k.op("dve", lambda e: e.tensor_scalar(msk[:], ang[:], -float(np.pi), TWO_PI, op0=ALU.is_lt, op1=ALU.mult), R=[ang], W=[msk])
                k.op("dve", lambda e: e.tensor_tensor(ang[:], ang[:], msk[:], op=ALU.add), R=[ang, msk], W=[ang])
                k.op("act", lambda e: e.activation(cs[:], ang[:], ACT.Sin), R=[ang], W=[cs])
            if "noLAT" not in SKIP:
                k.op("act", lambda e: e.activation(junkm[:, 0:256], u_[:, 0:256], ACT.Square, accum_out=s1[:]), R=[u_], W=[junkm, s1])
                k.op("dve", lambda e: e.tensor_scalar(s1[:], s1[:], 1.0 / 256, EPS, op0=ALU.mult, op1=ALU.add), R=[s1], W=[s1])
                k.op("act", lambda e: e.sqrt(s1[:], s1[:]), R=[s1], W=[s1])
                k.op("dve", lambda e: e.reciprocal(s1[:], s1[:]), R=[s1], W=[s1])
                k.op("dve", lambda e: e.scalar_tensor_tensor(qlb[:], u_[:, 0:256], s1[:], qan_b[:], op0=ALU.mult, op1=ALU.mult), R=[u_, s1, qan_b], W=[qlb])
                k.op("act", lambda e: e.activation(junkm[:, 0:128], u_[:, 256:384], ACT.Square, accum_out=s2[:]), R=[u_], W=[junkm, s2])
                k.op("dve", lambda e: e.tensor_scalar(s2[:], s2[:], 1.0 / 128, EPS, op0=ALU.mult, op1=ALU.add), R=[s2], W=[s2])
                k.op("act", lambda e: e.sqrt(s2[:], s2[:]), R=[s2], W=[s2])
                k.op("dve", lambda e: e.reciprocal(s2[:], s2[:]), R=[s2], W=[s2])
                k.op("dve", lambda e: e.scalar_tensor_tensor(kvb[:], u_[:, 256:384], s2[:], kvan_b[:], op0=ALU.mult, op1=ALU.mult), R=[u_, s2, kvan_b], W=[kvb])
                k.op("pe", lambda e: e.transpose(ptr[:, 0, :], qlb[:, 0:128], ident_b[:]), R=[qlb, ident_b], W=[ptr])
                k.op("pe", lambda e: e.transpose(ptr[:, 1, :], qlb[:, 128:256], ident_b[:]), R=[qlb, ident_b], W=[ptr])
                k.op("pe", lambda e: e.transpose(ptr[:, 2, :], kvb[:], ident_b[:]), R=[kvb, ident_b], W=[ptr])
                k.op("act", lambda e: e.copy(qlT[:], ptr[:, 0:2, :]), R=[ptr], W=[qlT])
                k.op("act", lambda e: e.copy(kvT[:], ptr[:, 2, :]), R=[ptr], W=[kvT])
                if "noA" not in SKIP:
                    for c in range(2):
                        k.op("pe", lambda e: e.matmul(pq[:, 0, :], qlT[:, c, :], wq[:, c, 0:512], start=(c == 0), stop=(c == 1)), R=[qlT, wq], W=[pq])
                    for c in range(2):
                        k.op("pe", lambda e: e.matmul(pq[:, 1, 0:256], qlT[:, c, :], wq[:, c, 512:768], start=(c == 0), stop=(c == 1)), R=[qlT, wq], W=[pq])
                    k.op("pe", lambda e: e.matmul(pkv[:, 0, :], kvT[:], wkv[:, 0:512], start=True, stop=True), R=[kvT, wkv], W=[pkv])
                    k.op("pe", lambda e: e.matmul(pkv[:, 1, :], kvT[:], wkv[:, 512:1024], start=True, stop=True), R=[kvT, wkv], W=[pkv])
                    qflat = qf[:].rearrange("p h d -> p (h d)")
                    k.op("act", lambda e: e.copy(qflat[:, 0:512], pq[:, 0, :]), R=[pq], W=[qf])
                    k.op("act", lambda e: e.copy(qflat[:, 512:768], pq[:, 1, 0:256]), R=[pq], W=[qf])
                    kv3 = pkv[:].rearrange("p c (h e) -> p (c h) e", e=128)
                    k.op("dve", lambda e: e.tensor_copy(kf[:, :, 0:64], kv3[:, :, 0:64]), R=[pkv], W=[kf])
                    k.op("dve", lambda e: e.tensor_copy(kf[:, :, 64:96], u_[:, 384:416].unsqueeze(1).to_broadcast([128, 8, 32])), R=[u_], W=[kf])
            v_ = vs[i % 2]
            if "noLAT" not in SKIP and "noA" not in SKIP and "noVC" not in SKIP:
                k.op("dve", lambda e: e.tensor_copy(v_[:, :, 0:64], kv3[:, :, 64:128]), R=[pkv], W=[v_])
            if "noV" not in SKIP:
                k.dma("sp", V_d[b, it * 128:(it + 1) * 128, :, :], v_[:], R=[v_], W=[V_d])
            if "noHN" not in SKIP:
                headnorm_rope(qf, qn_b, qTs[i % 2], QT_d, b, it)
                headnorm_rope(kf, kn_b, kTs[i % 2], KT_d, b, it)
    k.es = es
    k.regen()
    if stop_after <= 5:
        k.finish(list(outs.values()))
        return nc, es


    QS = min(4, NT)
    NJ = NT // QS
    QW = QS * 128
    with ExitStack() as es6:
        k.es = es6
        MASK = []
        mi = k.sb("mi", [128, QW], I32)
        for r_ in range(QS):
            m_ = k.sb("mask%d" % r_, [128, QW], BF16)
            k.op("pool", lambda e: e.iota(mi[:], pattern=[[1, QW]], base=-r_ * 128, channel_multiplier=-1), R=[], W=[mi])
            k.op("dve", lambda e: e.tensor_scalar(m_[:], mi[:], 0.0, None, op0=ALU.is_ge), R=[mi], W=[m_])
            MASK.append(m_)
        Vall = k.sb("Vall", [128, NT, 8, 66], BF16)
        QTs = [k.sb("QTs%d" % i, [96, T], BF16) for i in range(2)]
        KTs = [k.sb("KTs%d" % i, [96, T], BF16) for i in range(2)]
        pTs = [k.sb("pTs%d" % i, [128, QW], BF16) for i in range(3)]
        ps_ = [k.ps("ps%d" % i, [128, 512]) for i in range(2)]
        po = [k.ps("po%d" % i, [128, 512]) for i in range(QS)]
        rec = k.sb("rec", [128, 1])
        ym = [k.sb("ym%d" % i, [128, 64]) for i in range(4)]
        SC = float(96 ** -0.5)
        nbh = 0; npt = 0; nym = 0
        for b in range(NB):
            for kb in range(NT):
                k.dma("sp", Vall[:, kb, :, :], V_d[b, kb * 128:(kb + 1) * 128, :, :], R=[V_d], W=[Vall])
            for h in range(8):
                Q_ = QTs[nbh % 2]; K_ = KTs[nbh % 2]; nbh += 1
                k.dma("sp", Q_[:], QT_d[b, :, h, :], R=[QT_d], W=[Q_])
                k.dma("sp", K_[:], KT_d[b, :, h, :], R=[KT_d], W=[K_])
                its = [(J, kb) for J in range(NJ) for kb in range(QS * J + QS)]
                def emit_qk(n):
                    J, kb = its[n]
                    p_ = ps_[(npt0 + n) % 2]
                    k.op("pe", lambda e: e.matmul(p_[:, 0:QW], K_[:, kb * 128:(kb + 1) * 128], Q_[:, J * QW:(J + 1) * QW], start=True, stop=True),
                         R=[K_, Q_], W=[p_])
                npt0 = npt
                emit_qk(0)
                for n, (J, kb) in enumerate(its):
                    if n + 1 < len(its):
                        emit_qk(n + 1)
                    p_ = ps_[(npt0 + n) % 2]; pT = pTs[(npt0 + n) % 3]
                    k.op("act", lambda e: e.activation(pT[:], p_[:, 0:QW], ACT.Exp, scale=SC), R=[p_], W=[pT])
                    r_ = kb - QS * J
                    if r_ >= 0:
                        k.op("dve", lambda e: e.tensor_tensor(pT[:], pT[:], MASK[r_][:], op=ALU.mult), R=[pT, MASK[r_]], W=[pT])
                    for qb in range(QS):
                        if QS * J + qb >= kb:
                            k.op("pe", lambda e: e.matmul(po[qb][:, 0:65], pT[:, qb * 128:(qb + 1) * 128], Vall[:, kb, h, 0:65],
                                                          start=(kb == 0), stop=(kb == QS * J + qb)), R=[pT, Vall], W=[po[qb]])
                    if kb == QS * J + QS - 1:
                        for qb in range(QS):
                            y_ = ym[nym % 4]; nym += 1
                            k.op("dve", lambda e: e.reciprocal(rec[:], po[qb][:, 64:65]), R=[po[qb]], W=[rec])
                            k.op("dve", lambda e: e.tensor_scalar(y_[:], po[qb][:, 0:64], rec[:], None, op0=ALU.mult), R=[po[qb], rec], W=[y_])
                            i = b * NT + QS * J + qb
                            k.dma("sp", YCAT_d[i * 128:(i + 1) * 128, 512 + h * 64:512 + (h + 1) * 64], y_[:], R=[y_], W=[YCAT_d])
                npt += len(its)
    k.es = es
    k.regen()
    if stop_after <= 6:
        k.finish(list(outs.values()))
        return nc, es


    NE = 256
    BLK = 256
    LOGB = 8
    NBLK = (NTOK * 8 + NE * (BLK - 1)) // BLK
    NROWS = NBLK * BLK
    w_out_d = inp("w_out", [D, D]); norm_ffn_d = inp("norm_ffn", [1, D]); w_router_d = inp("w_router", [D, NE]); rbias_d = inp("router_bias", [1, NE])
    X1_d = outp("X1", [NTOK, D]) if "X1" in dbg else k.dram("X1", [NTOK, D])
    H2T_d = outp("H2T", [128, 8, NTOK], BF16) if "H2T" in dbg else k.dram("H2T", [128, 8, NTOK], BF16)
    XBUF_d = k.dram("XBUF", [NROWS, D], BF16)
    YBUF_d = k.dram("YBUF", [NROWS, D], BF16)
    H2B_d = k.dram("H2B", [NTOK, D], BF16)
    dbgH2 = outp("H2", [NTOK, D]) if "H2" in dbg else None
    dbgG = outp("GATE", [NTOK, NE]) if "GATE" in dbg else None
    dbgDW = outp("DW", [NTOK, 16]) if "DW" in dbg else None
    NTT = NB * NT
    BREG = nc.gpsimd.to_reg(NROWS - 1)
    DESTI = k.sb("DESTI", [128, NTT, 8], I32)
    WK = k.sb("WK", [128, NTT, 8])
    IDXG = k.sb("IDXG", [128, NBLK], I32)
    with ExitStack() as es7:
        k.es = es7
        nfb = k.sb("nfb", [128, D])
        k.dma("sp", nfb[:], norm_ffn_d[0:1, :].partition_broadcast(128), W=[nfb])
        for b in range(NB):
            m = MOD[b][4]
            k.op("dve", lambda e: e.scalar_tensor_tensor(m[:], m[:], 1.0, nfb[:], op0=ALU.add, op1=ALU.mult), R=[m, nfb], W=[m])
        rb_b = k.sb("rb_b", [128, NE])
        k.dma("sp", rb_b[:], rbias_d[0:1, :].partition_broadcast(128), W=[rb_b])
        wo = k.sb("wo", [128, 8, D], BF16); wr = k.sb("wr", [128, 8, NE])
        wst = [k.sb("wost%d" % i, [128, D]) for i in range(2)]
        for j in range(8):
            s_ = wst[j % 2]
            k.dma("sp", s_[:], w_out_d[j * 128:(j + 1) * 128, :], W=[s_])
            k.op("pool" if j % 2 else "dve", lambda e: e.tensor_copy(wo[:, j, :], s_[:]), R=[s_], W=[wo])
        k.dma("sp", wr[:], w_router_d[:].rearrange("(j p) n -> p j n", p=128), W=[wr])
        UT = k.sb("UT", [128, 128], BF16); ONESB = k.sb("ONESB", [128, 128], BF16); ones256 = k.sb("ones256", [128, NE])
        uti = k.sb("uti", [128, 128], I32)
        k.op("pool", lambda e: e.iota(uti[:], pattern=[[1, 128]], base=0, channel_multiplier=-1), W=[uti])
        k.op("dve", lambda e: e.tensor_scalar(UT[:], uti[:], 0.0, None, op0=ALU.is_gt), R=[uti], W=[UT])
        k.op("dve", lambda e: e.memset(ONESB[:], 1.0), W=[ONESB])
        k.op("dve", lambda e: e.memset(ones256[:], 1.0), W=[ones256])
        ecap_i = k.sb("ecap_i", [128, NE], I32); eidx = k.sb("eidx", [128, NE])
        k.op("pool", lambda e: e.iota(ecap_i[:], pattern=[[1, NE]], base=0, channel_multiplier=0), W=[ecap_i])
        k.op("dve", lambda e: e.tensor_copy(eidx[:], ecap_i[:]), R=[ecap_i], W=[eidx])
        EK = k.sb("EK", [128, NTT, 8]); RK = k.sb("RK", [128, NTT, 8])
        carry = k.sb("carry", [128, NE])
        k.op("dve", lambda e: e.memset(carry[:], 0.0), W=[carry])
        yc_ = [k.sb("ycat%d" % i, [128, D]) for i in range(2)]
        ycb = k.sb("ycb", [128, D], BF16); ycT = k.sb("ycT", [128, 8, 128], BF16)
        xt = [k.sb("x3_%d" % i, [128, D]) for i in range(2)]
        x1 = [k.sb("x1_%d" % i, [128, D]) for i in range(2)]
        junk = k.sb("junk3", [128, D]); ssq = k.sb("ssq3", [128, 1])
        h2 = k.sb("h2", [128, D]); h2b = [k.sb("h2b%d" % i, [128, D], BF16) for i in range(2)]
        h2T = k.sb("h2T", [128, 8, 128]); h2Tb = [k.sb("h2Tb%d" % i, [128, 8, 128], BF16) for i in range(2)]
        pT3 = k.ps("pT3", [128, 8, 128], BF16); pTf = k.ps("pTf", [128, 8, 128])
        pp = k.ps("pp", [128, 2, 512]); plg = k.ps("plg", [128, NE]); prk = k.ps("prk", [128, 2, NE])
        sc = k.sb("sc", [128, NE]); sel = k.sb("sel", [128, NE]); selm = k.sb("selm", [128, NE])
        m8g = k.sb("m8g", [128, 8, 8]); gs = k.sb("gs", [128, 8]); gm8 = k.sb("gm8", [128, 8]); gmask = k.sb("gmask", [128, 8]); em8 = k.sb("em8", [128, 8])
        emask = k.sb("emask", [128, NE]); emb = k.sb("emb", [128, NE], BF16); Gm = k.sb("Gm", [128, NE]); wsum = k.sb("wsum", [128, 1])
        pos = k.sb("pos", [128, NE]); dfull = k.sb("dfull", [128, NE]); ovf = k.sb("ovf", [128, NE]); slot = k.sb("slot", [128, NE])
        junk2 = k.sb("junk2", [128, NE]); destf = k.sb("destf", [128, 8])
        for i in range(NTT):
            b = i // NT
            y_ = yc_[i % 2]; x_ = xt[i % 2]; x1_ = x1[i % 2]; hb_ = h2b[i % 2]; hTb_ = h2Tb[i % 2]
            k.dma("sp", y_[:], YCAT_d[i * 128:(i + 1) * 128, :], R=[YCAT_d], W=[y_])
            k.dma("sp", x_[:], x_d[i * 128:(i + 1) * 128, :], W=[x_])
            k.op("act", lambda e: e.copy(ycb[:], y_[:]), R=[y_], W=[ycb])
            for j in range(8):
                k.op("pe", lambda e: e.transpose(pT3[:, j, :], ycb[:, j * 128:(j + 1) * 128], ident_b[:]), R=[ycb, ident_b], W=[pT3])
            k.op("act", lambda e: e.copy(ycT[:], pT3[:]), R=[pT3], W=[ycT])
            for hh in range(2):
                for j in range(8):
                    k.op("pe", lambda e: e.matmul(pp[:, hh, :], ycT[:, j, :], wo[:, j, hh * 512:(hh + 1) * 512], start=(j == 0), stop=(j == 7)), R=[ycT, wo], W=[pp])
            ppf = pp[:].rearrange("p a n -> p (a n)")
            k.op("dve", lambda e: e.tensor_tensor(x1_[:], ppf, MOD[b][2][:], op=ALU.mult), R=[pp, MOD[b][2]], W=[x1_])
            k.op("pool", lambda e: e.tensor_tensor(x1_[:], x1_[:], x_[:], op=ALU.add), R=[x1_, x_], W=[x1_])
            k.dma("sp", X1_d[i * 128:(i + 1) * 128, :], x1_[:], R=[x1_], W=[X1_d])
            k.op("act", lambda e: e.activation(junk[:], x1_[:], ACT.Square, accum_out=ssq[:]), R=[x1_], W=[junk, ssq])
            k.op("dve", lambda e: e.tensor_scalar(ssq[:], ssq[:], 1.0 / D, EPS, op0=ALU.mult, op1=ALU.add), R=[ssq], W=[ssq])
            k.op("act", lambda e: e.sqrt(ssq[:], ssq[:]), R=[ssq], W=[ssq])
            k.op("dve", lambda e: e.reciprocal(ssq[:], ssq[:]), R=[ssq], W=[ssq])
            k.op("dve", lambda e: e.scalar_tensor_tensor(h2[:], x1_[:], ssq[:], MOD[b][4][:], op0=ALU.mult, op1=ALU.mult), R=[x1_, ssq, MOD[b][4]], W=[h2])
            k.op("pool", lambda e: e.tensor_tensor(h2[:], h2[:], MOD[b][3][:], op=ALU.add), R=[h2, MOD[b][3]], W=[h2])
            k.op("act", lambda e: e.copy(hb_[:], h2[:]), R=[h2], W=[hb_])
            if dbgH2 is not None:
                k.dma("sp", dbgH2[i * 128:(i + 1) * 128, :], h2[:], R=[h2], W=[dbgH2])
            for j in range(8):
                k.op("pe", lambda e: e.transpose(pTf[:, j, :], h2[:, j * 128:(j + 1) * 128], ident_f[:]), R=[h2, ident_f], W=[pTf])
            k.op("dve", lambda e: e.tensor_copy(h2T[:], pTf[:]), R=[pTf], W=[h2T])
            k.op("pool", lambda e: e.tensor_copy(hTb_[:], h2T[:]), R=[h2T], W=[hTb_])
            if "noH2T" not in SKIP:
                k.dma("sp", H2T_d[:, :, i * 128:(i + 1) * 128], hTb_[:], R=[hTb_], W=[H2T_d])
            if "noRT" not in SKIP:
                for j in range(8):
                    k.op("pe", lambda e: e.matmul(plg[:], h2T[:, j, :], wr[:, j, :], start=(j == 0), stop=(j == 7)), R=[h2T, wr], W=[plg])
                k.op("act", lambda e: e.activation(sc[:], plg[:], ACT.Sigmoid), R=[plg], W=[sc])
                k.op("dve", lambda e: e.tensor_tensor(sel[:], sc[:], rb_b[:], op=ALU.add), R=[sc, rb_b], W=[sel])
                for g in range(8):
                    k.op("dve", lambda e: e.max(m8g[:, g, :], sel[:, g * 32:(g + 1) * 32]), R=[sel], W=[m8g])
                k.op("dve", lambda e: e.tensor_tensor(gs[:], m8g[:, :, 0], m8g[:, :, 1], op=ALU.add), R=[m8g], W=[gs])
                k.op("dve", lambda e: e.max(gm8[:], gs[:]), R=[gs], W=[gm8])
                k.op("dve", lambda e: e.tensor_scalar(gmask[:], gs[:], gm8[:, 3:4], None, op0=ALU.is_ge), R=[gs, gm8], W=[gmask])
                k.op("dve", lambda e: e.scalar_tensor_tensor(selm[:].rearrange("p (g n) -> p g n", g=8), sel[:].rearrange("p (g n) -> p g n", g=8), 2.0,
                                                             gmask[:].unsqueeze(2).to_broadcast([128, 8, 32]), op0=ALU.add, op1=ALU.mult), R=[sel, gmask], W=[selm])
                k.op("dve", lambda e: e.max(em8[:], selm[:]), R=[selm], W=[em8])
                k.op("dve", lambda e: e.tensor_scalar(emask[:], selm[:], em8[:, 7:8], None, op0=ALU.is_ge), R=[selm, em8], W=[emask])
                k.op("act", lambda e: e.copy(emb[:], emask[:]), R=[emask], W=[emb])
                k.op("dve", lambda e: e.scalar_tensor_tensor(Gm[:], sc[:], 1.0, emask[:], op0=ALU.mult, op1=ALU.mult, accum_out=wsum[:]), R=[sc, emask], W=[Gm, wsum])
                k.op("dve", lambda e: e.reciprocal(wsum[:], wsum[:]), R=[wsum], W=[wsum])
                k.op("dve", lambda e: e.tensor_scalar(Gm[:], Gm[:], wsum[:], 2.5, op0=ALU.mult, op1=ALU.mult), R=[Gm, wsum], W=[Gm])
                if dbgG is not None:
                    k.dma("sp", dbgG[i * 128:(i + 1) * 128, :], Gm[:], R=[Gm], W=[dbgG])
            if "noRT" not in SKIP and "noRK" not in SKIP:
                k.op("pe", lambda e: e.matmul(prk[:, 0, :], UT[:], emb[:], start=True, stop=True), R=[UT, emb], W=[prk])
                k.op("pe", lambda e: e.matmul(prk[:, 1, :], ONESB[:], emb[:], start=True, stop=True), R=[ONESB, emb], W=[prk])
                k.op("dve", lambda e: e.tensor_tensor(pos[:], prk[:, 0, :], carry[:], op=ALU.add), R=[prk, carry], W=[pos])
                k.op("dve", lambda e: e.tensor_tensor(carry[:], prk[:, 1, :], carry[:], op=ALU.add), R=[prk, carry], W=[carry])
                k.op("dve", lambda e: e.tensor_tensor_scan(slot[:], ones256[:], emask[:], 0.0, op0=ALU.mult, op1=ALU.add), R=[ones256, emask], W=[slot])
                k.op("dve", lambda e: e.tensor_tensor(slot[:], slot[:], emask[:], op=ALU.mult), R=[slot, emask], W=[slot])
                for ks in range(8):
                    k.op("dve", lambda e: e.scalar_tensor_tensor(junk2[:], slot[:], float(ks + 1), eidx[:], op0=ALU.is_equal, op1=ALU.mult, accum_out=EK[:, i, ks:ks + 1]),
                         R=[slot, eidx], W=[junk2, EK])
                    k.op("dve", lambda e: e.scalar_tensor_tensor(junk2[:], slot[:], float(ks + 1), pos[:], op0=ALU.is_equal, op1=ALU.mult, accum_out=RK[:, i, ks:ks + 1]),
                         R=[slot, pos], W=[junk2, RK])
                    k.op("dve", lambda e: e.scalar_tensor_tensor(junk2[:], slot[:], float(ks + 1), Gm[:], op0=ALU.is_equal, op1=ALU.mult, accum_out=WK[:, i, ks:ks + 1]),
                         R=[slot, Gm], W=[junk2, WK])
            k.dma("sp", H2B_d[i * 128:(i + 1) * 128, :], hb_[:], R=[hb_], W=[H2B_d])
        cnt_i = k.sb("cnt_i", [128, NE], I32); padc = k.sb("padc", [128, NE]); pend = k.sb("pend", [128, NE]); pstart = k.sb("pstart", [128, NE])
        k.op("dve", lambda e: e.tensor_scalar(padc[:], carry[:], float(BLK - 1), None, op0=ALU.add), R=[carry], W=[padc])
        k.op("dve", lambda e: e.tensor_copy(cnt_i[:], padc[:]), R=[padc], W=[cnt_i])
        k.op("dve", lambda e: e.tensor_scalar(cnt_i[:], cnt_i[:], LOGB, LOGB, op0=ALU.arith_shift_right, op1=ALU.logical_shift_left), R=[cnt_i], W=[cnt_i])
        k.op("dve", lambda e: e.tensor_copy(padc[:], cnt_i[:]), R=[cnt_i], W=[padc])
        k.op("dve", lambda e: e.tensor_tensor_scan(pend[:], ones256[:], padc[:], 0.0, op0=ALU.mult, op1=ALU.add), R=[ones256, padc], W=[pend])
        k.op("dve", lambda e: e.tensor_tensor(pstart[:], pend[:], padc[:], op=ALU.subtract), R=[pend, padc], W=[pstart])
        bexp = k.sb("bexp", [128, NBLK])
        for j in range(NBLK):
            k.op("dve", lambda e: e.tensor_scalar(junk2[:], pend[:], float(BLK * j), 0.0, op0=ALU.is_le, op1=ALU.add, accum_out=bexp[:, j:j + 1]), R=[pend], W=[junk2, bexp])
        k.op("dve", lambda e: e.tensor_scalar(bexp[:], bexp[:], float(NE - 1), None, op0=ALU.min), R=[bexp], W=[bexp])
        bgi = k.sb("bgi", [128, 1], I32); bgf = k.sb("bgf", [128, 1])
        k.op("pool", lambda e: e.iota(bgi[:], pattern=[[1, 1]], base=0, channel_multiplier=1), W=[bgi])
        k.op("dve", lambda e: e.tensor_copy(bgf[:], bgi[:]), R=[bgi], W=[bgf])
        idxf = k.sb("idxf", [128, NBLK])
        k.op("dve", lambda e: e.tensor_scalar(idxf[:], bexp[:], 128.0, bgf[:, 0:1], op0=ALU.mult, op1=ALU.add), R=[bexp, bgf], W=[idxf])
        k.op("dve", lambda e: e.tensor_copy(IDXG[:], idxf[:]), R=[idxf], W=[IDXG])
        if "BEXP" in dbg:
            o = outp("BEXP", [1, NBLK]); k.dma("sp", o[0:1, :], bexp[0:1, :], R=[bexp], W=[o])
            o2 = outp("PSTART", [1, NE]); k.dma("sp", o2[0:1, :], pstart[0:1, :], R=[pstart], W=[o2])
        hbb = [k.sb("hbb%d" % i, [128, D], BF16) for i in range(2)]
        for i in range(NTT):
            hb_ = hbb[i % 2]
            k.dma("sp", hb_[:], H2B_d[i * 128:(i + 1) * 128, :], R=[H2B_d], W=[hb_])
            for ks in range(8):
                k.op("dve", lambda e: e.scalar_tensor_tensor(junk2[:], eidx[:], EK[:, i, ks:ks + 1], pstart[:], op0=ALU.is_equal, op1=ALU.mult, accum_out=destf[:, ks:ks + 1]),
                     R=[eidx, EK, pstart], W=[junk2, destf])
            k.op("dve", lambda e: e.tensor_tensor(destf[:], destf[:], RK[:, i, :], op=ALU.add), R=[destf, RK], W=[destf])
            k.op("dve", lambda e: e.tensor_copy(DESTI[:, i, :], destf[:]), R=[destf], W=[DESTI])
            if dbgDW is not None:
                k.dma("sp", dbgDW[i * 128:(i + 1) * 128, 0:8], destf[:], R=[destf], W=[dbgDW])
                k.dma("sp", dbgDW[i * 128:(i + 1) * 128, 8:16], WK[:, i, :], R=[WK], W=[dbgDW])
            for ks in range(8):
                k.idma(out=XBUF_d[:, :], out_offset=bass.IndirectOffsetOnAxis(ap=DESTI[:, i, ks:ks + 1], axis=0), in_=hb_[:, :], in_offset=None,
                       bounds_check=BREG, oob_is_err=False, R=[hb_, DESTI], W=[XBUF_d])
        if True:
            o = outp("CNT", [1, NE])
            k.dma("sp", o[0:1, :], carry[0:1, :], R=[carry], W=[o])
    k.es = es
    k.regen()
    if stop_after <= 7:
        k.finish(list(outs.values()))
        return nc, es


    wegu_d = inp("w_e_gate_up", [NE * 128, 8 * 512]); wed_d = inp("w_e_down", [NE * 128, 2 * D])
    NBX = int(os.environ.get("NBX", str(NBLK)))
    CT = BLK // 128
    with ExitStack() as es8:
        k.es = es8
        k.imax = 12
        gst = [k.sb("gst%d" % i, [128, 8, 512]) for i in range(2)]
        dst_ = [k.sb("dst%d" % i, [128, 2, D]) for i in range(2)]
        wgu = [k.sb("wgu%d" % i, [128, 8, 512], BF16) for i in range(2)]
        wd = [k.sb("wd%d" % i, [128, 2, D], BF16) for i in range(2)]
        xin = [k.sb("xin%d" % i, [128, D], BF16) for i in range(4)]
        xT = [k.sb("xT%d" % i, [128, 8, BLK], BF16) for i in range(2)]
        sg = [k.sb("sg%d" % i, [128, BLK]) for i in range(2)]
        actT = [k.sb("actT%d" % i, [128, 2, BLK], BF16) for i in range(2)]
        yo = [k.sb("yo4_%d" % i, [128, D], BF16) for i in range(4)]
        pTx = k.ps("pTx", [128, 8, 128], BF16)
        pu = [k.ps("pu4_%d" % i, [128, 512]) for i in range(4)]
        pd = k.ps("pd", [128, 2, 512])
        nx = 0; ny = 0
        for jb in range(NBX):
            g_ = gst[jb % 2]; d_ = dst_[jb % 2]; wg = wgu[jb % 2]; wd_ = wd[jb % 2]; xT_ = xT[jb % 2]; aT = actT[jb % 2]
            k.idma(out=g_[:].rearrange("p j n -> p (j n)"), out_offset=None, in_=wegu_d[:, :], in_offset=bass.IndirectOffsetOnAxis(ap=IDXG[:, jb:jb + 1], axis=0),
                   R=[wegu_d, IDXG], W=[g_])
            k.idma(out=d_[:].rearrange("p c n -> p (c n)"), out_offset=None, in_=wed_d[:, :], in_offset=bass.IndirectOffsetOnAxis(ap=IDXG[:, jb:jb + 1], axis=0),
                   R=[wed_d, IDXG], W=[d_])
            k.op("dve", lambda e: e.tensor_copy(wg[:, 0:5, :], g_[:, 0:5, :]), R=[g_], W=[wg])
            k.op("act", lambda e: e.copy(wg[:, 5:8, :], g_[:, 5:8, :]), R=[g_], W=[wg])
            k.op("act", lambda e: e.copy(wd_[:], d_[:]), R=[d_], W=[wd_])
            if jb == 0:
                for tt in range(CT):
                    k.dma("sp", xin[tt][:], XBUF_d[tt * 128:(tt + 1) * 128, :], R=[XBUF_d], W=[xin[tt]])
            if jb + 1 < NBX:
                for tt in range(CT):
                    xn = xin[((jb + 1) * CT + tt) % 4]
                    k.dma("sp", xn[:], XBUF_d[(jb + 1) * BLK + tt * 128:(jb + 1) * BLK + (tt + 1) * 128, :], R=[XBUF_d], W=[xn])
            for tt in range(CT):
                xi = xin[(jb * CT + tt) % 4]
                for j in range(8):
                    k.op("pe", lambda e: e.transpose(pTx[:, j, :], xi[:, j * 128:(j + 1) * 128], ident_b[:]), R=[xi, ident_b], W=[pTx])
                k.op("act" if tt % 2 else "dve", (lambda e: e.copy(xT_[:, :, tt * 128:(tt + 1) * 128], pTx[:])) if tt % 2 else
                     (lambda e: e.tensor_copy(xT_[:, :, tt * 128:(tt + 1) * 128], pTx[:])), R=[pTx], W=[xT_])
            for c in range(4):
                for j in range(8):
                    k.op("pe", lambda e: e.matmul(pu[c][:, 0:BLK], wg[:, j, c * 128:(c + 1) * 128], xT_[:, j, :], start=(j == 0), stop=(j == 7)), R=[wg, xT_], W=[pu[c]])
            for c in range(2):
                s_ = sg[c]
                k.op("act", lambda e: e.activation(s_[:], pu[c][:, 0:BLK], ACT.Silu), R=[pu[c]], W=[s_])
                k.op("dve", lambda e: e.tensor_tensor(aT[:, c, :], s_[:], pu[2 + c][:, 0:BLK], op=ALU.mult), R=[s_, pu[2 + c]], W=[aT])
            for tt in range(CT):
                for hh in range(2):
                    for c in range(2):
                        k.op("pe", lambda e: e.matmul(pd[:, hh, :], aT[:, c, tt * 128:(tt + 1) * 128], wd_[:, c, hh * 512:(hh + 1) * 512], start=(c == 0), stop=(c == 1)), R=[aT, wd_], W=[pd])
                y_ = yo[ny % 4]; ny += 1
                k.op("dve", lambda e: e.tensor_copy(y_[:], pd[:].rearrange("p a n -> p (a n)")), R=[pd], W=[y_])
                k.dma("sp", YBUF_d[jb * BLK + tt * 128:jb * BLK + (tt + 1) * 128, :], y_[:], R=[y_], W=[YBUF_d])
        k.imax = 6
    k.es = es
    k.regen()
    if stop_after <= 8:
        k.finish(list(outs.values()))
        return nc, es

    wsgu_d = inp("w_sh_gate_up", [D, 512]); wsd_d = inp("w_sh_down", [256, D])
    OUT_d = outp("out", [NTOK, D])
    TB5 = min(4, NTT)
    with ExitStack() as es9:
        k.es = es9
        gst = k.sb("sgst", [128, 8, 512]); dst_ = k.sb("sdst", [128, 2, D])
        wg = k.sb("swgu", [128, 8, 512], BF16); wd_ = k.sb("swd", [128, 2, D], BF16)
        k.dma("sp", gst[:], wsgu_d[:].rearrange("(j p) n -> p j n", p=128), W=[gst])
        k.dma("sp", dst_[:], wsd_d[:].rearrange("(c p) n -> p c n", p=128), W=[dst_])
        k.op("dve", lambda e: e.tensor_copy(wg[:], gst[:]), R=[gst], W=[wg])
        k.op("pool", lambda e: e.tensor_copy(wd_[:], dst_[:]), R=[dst_], W=[wd_])
        xTs = [k.sb("xTs%d" % i, [128, 8, TB5 * 128], BF16) for i in range(2)]
        sg = [k.sb("sg5_%d" % i, [128, TB5 * 128]) for i in range(2)]
        aT5 = [k.sb("aT5_%d" % i, [128, 2, TB5 * 128], BF16) for i in range(2)]
        yg = [k.sb("yg%d" % i, [128, D], BF16) for i in range(4)]
        for y_ in yg:
            k.op("pool", lambda e: e.memset(y_[:], 0.0), W=[y_])
        acc = [k.sb("acc%d" % i, [128, D]) for i in range(2)]
        x1t = [k.sb("x1t%d" % i, [128, D]) for i in range(2)]
        pu = [k.ps("pu5_%d" % i, [128, 512]) for i in range(4)]
        pd = [k.ps("pd5_%d" % i, [128, 2, 512]) for i in range(2)]
        nyg = 0
        for blk in range(NTT // TB5):
            xT_ = xTs[blk % 2]; aT = aT5[blk % 2]
            W5 = TB5 * 128
            k.dma("sp", xT_[:], H2T_d[:, :, blk * W5:(blk + 1) * W5], R=[H2T_d], W=[xT_])
            for c in range(4):
                for j in range(8):
                    k.op("pe", lambda e: e.matmul(pu[c][:, 0:W5], wg[:, j, c * 128:(c + 1) * 128], xT_[:, j, :], start=(j == 0), stop=(j == 7)), R=[wg, xT_], W=[pu[c]])
            for c in range(2):
                s_ = sg[c]
                k.op("act", lambda e: e.activation(s_[:], pu[c][:, 0:W5], ACT.Silu), R=[pu[c]], W=[s_])
                k.op("dve", lambda e: e.tensor_tensor(aT[:, c, :], s_[:], pu[2 + c][:, 0:W5], op=ALU.mult), R=[s_, pu[2 + c]], W=[aT])
            for tt in range(TB5):
                i = blk * TB5 + tt; b = i // NT
                pd_ = pd[i % 2]; a_ = acc[i % 2]; x1_ = x1t[i % 2]
                for hh in range(2):
                    for c in range(2):
                        k.op("pe", lambda e: e.matmul(pd_[:, hh, :], aT[:, c, tt * 128:(tt + 1) * 128], wd_[:, c, hh * 512:(hh + 1) * 512], start=(c == 0), stop=(c == 1)), R=[aT, wd_], W=[pd_])
                k.dma("sp", x1_[:], X1_d[i * 128:(i + 1) * 128, :], R=[X1_d], W=[x1_])
                k.op("act", lambda e: e.copy(a_[:], pd_[:].rearrange("p a n -> p (a n)")), R=[pd_], W=[a_])
                for ks in range(8):
                    y_ = yg[nyg % 4]; nyg += 1
                    k.idma(out=y_[:, :], out_offset=None, in_=YBUF_d[:, :], in_offset=bass.IndirectOffsetOnAxis(ap=DESTI[:, i, ks:ks + 1], axis=0),
                           bounds_check=BREG, oob_is_err=False, R=[YBUF_d, DESTI], W=[y_])
                    k.op("dve", lambda e: e.scalar_tensor_tensor(a_[:], y_[:], WK[:, i, ks:ks + 1], a_[:], op0=ALU.mult, op1=ALU.add), R=[y_, WK, a_], W=[a_])
                k.op("dve", lambda e: e.tensor_tensor(a_[:], a_[:], MOD[b][5][:], op=ALU.mult), R=[a_, MOD[b][5]], W=[a_])
                k.op("pool", lambda e: e.tensor_tensor(a_[:], a_[:], x1_[:], op=ALU.add), R=[a_, x1_], W=[a_])
                k.dma("sp", OUT_d[i * 128:(i + 1) * 128, :], a_[:], R=[a_], W=[OUT_d])
    k.es = es
    k.regen()
    k.finish(list(outs.values()))
    return nc, es


_CACHE = {}


def kernel(**inputs):
    T = 4096
    ncores = 8
    if "nc" not in _CACHE:
        _CACHE["nc"] = build(T)
    nc, es = _CACHE["nc"]
    names = [a.memorylocations[0].name for a in nc.allocations
             if hasattr(a, "kind") and a.kind == "ExternalInput" and a.memorylocations[0].name != "partition_id"]
    shared = {}
    for n in names:
        if n in ("x", "c", "positions"):
            continue
        a = np.asarray(inputs[n])[0]
        if n == "rwkv_r_k":
            a = a.reshape(1, 512)
        elif n == "w_e_gate_up":
            a = a.reshape(256, 8, 128, 512).transpose(0, 2, 1, 3).reshape(256 * 128, 8 * 512)
        elif n == "w_e_down":
            a = a.reshape(256, 2, 128, 1024).transpose(0, 2, 1, 3).reshape(256 * 128, 2 * 1024)
        elif a.ndim == 1:
            a = a.reshape(1, -1)
        shared[n] = np.ascontiguousarray(a)
    x = np.asarray(inputs["x"]); c = np.asarray(inputs["c"]); pos = np.asarray(inputs["positions"])
    in_maps = []
    for cid in range(ncores):
        m = dict(shared)
        m["x"] = np.ascontiguousarray(x[2 * cid:2 * cid + 2].reshape(2 * T, 1024))
        m["c"] = np.ascontiguousarray(c[2 * cid:2 * cid + 2])
        m["positions"] = np.ascontiguousarray(pos[2 * cid:2 * cid + 2].reshape(2 * T, 1).astype(np.int32))
        in_maps.append(m)
    res = run_bass_kernel_spmd(nc, in_maps, core_ids=list(range(ncores)))
    try:
        print("max expert count per core:", [int(r["CNT"].max()) for r in res.results], flush=True)
    except Exception:
        pass
    out = np.stack([r["out"].reshape(2, T, 1024) for r in res.results], 0).reshape(16, T, 1024)
    return out.astype(np.float32)
```

```python
import os
import numpy as np
from contextlib import ExitStack
import concourse.bass as bass
import concourse.mybir as mybir
from concourse.bass_utils import run_bass_kernel_spmd


F32 = mybir.dt.float32
F32R = mybir.dt.float32r
BF16 = mybir.dt.bfloat16
I32 = mybir.dt.int32
U32 = mybir.dt.uint32
ACT = mybir.ActivationFunctionType
ALU = mybir.AluOpType
AX = mybir.AxisListType


class Buf:
    __slots__ = ("t", "lw", "rd", "name")

    def __init__(self, t, name=""):
        self.t = t
        self.lw = None
        self.rd = {}
        self.name = name

    def __getitem__(self, k):
        return self.t[k]


class K:
    def __init__(self, nc, es, ndma=24):
        self.nc = nc
        self.es = es
        self.eng = {"pe": nc.tensor, "act": nc.scalar, "dve": nc.vector, "pool": nc.gpsimd, "sp": nc.sync}
        self.sem = {}
        self.cnt = {}
        for n in ("pe", "act", "dve", "pool"):
            self.sem[n] = es.enter_context(nc.semaphore("s_" + n))
            self.cnt[n] = 0
        self.ndma = ndma
        self.dsem = [es.enter_context(nc.semaphore("s_dma%d" % i)) for i in range(ndma)]
        self.dcnt = [0] * ndma
        self.dnext = 0
        self.waited = {n: {} for n in ("pe", "act", "dve", "pool", "sp")}
        self.nins = 0
        self.imax = 6
        self.gen = 0
        self.top_es = es

    def sb(self, name, shape, dt=F32):
        return Buf(self.es.enter_context(self.nc.sbuf_tensor(name, list(shape), dt)), name)

    def ps(self, name, shape, dt=F32):
        return Buf(self.es.enter_context(self.nc.psum_tensor(name, list(shape), dt)), name)

    def dram(self, name, shape, dt=F32, kind="Internal"):
        return Buf(self.nc.dram_tensor(name, list(shape), dt, kind=kind).ap(), name)

    def _semof(self, key):
        return self.dsem[key[1]] if isinstance(key, tuple) else self.sem[key]

    def _wait(self, e, key, val, gen=None):
        if key == e and e == "pe":
            return
        if gen is not None and gen < self.gen:
            return
        w = self.waited[e]
        if w.get(key, 0) >= val:
            return
        self.eng[e].wait_ge(self._semof(key), val)
        w[key] = val
        self.nins += 1

    def _deps(self, e, R, W):
        for b in R:
            if b.lw is not None:
                self._wait(e, *b.lw)
        for b in W:
            if b.lw is not None:
                self._wait(e, *b.lw)
            for k, (v, g) in b.rd.items():
                self._wait(e, k, v, g)

    def _done(self, key, val, R, W):
        g = None if isinstance(key, tuple) else self.gen
        for b in R:
            o = b.rd.get(key)
            if o is None or o[1] != g or o[0] < val:
                b.rd[key] = (val, g)
        for b in W:
            b.lw = (key, val, g)
            b.rd = {}

    def op(self, e, fn, R=(), W=()):
        self._deps(e, R, W)
        ins = fn(self.eng[e])
        self.cnt[e] += 1
        ins.then_inc(self.sem[e], 1)
        self._done(e, self.cnt[e], R, W)
        self.nins += 1
        return ins

    def dma(self, q, out, in_, R=(), W=(), **kw):
        i = self.dnext
        self.dnext = (self.dnext + 1) % self.ndma
        key = ("dma", i)
        if self.dcnt[i] > 0:
            self._wait(q, key, self.dcnt[i])
        self._deps(q, R, W)
        ins = self.eng[q].dma_start(out=out, in_=in_, **kw)
        self.dcnt[i] += 16
        ins.then_inc(self.dsem[i], 16)
        self._done(key, self.dcnt[i], R, W)
        self.nins += 1
        return ins

    def idma(self, R=(), W=(), **kw):
        q = "pool"
        if not hasattr(self, "ipend"):
            self.ipend = []
        while len(self.ipend) >= self.imax:
            kk, vv = self.ipend.pop(0)
            self._wait("pool", kk, vv)
        i = self.dnext
        self.dnext = (self.dnext + 1) % self.ndma
        key = ("dma", i)
        if self.dcnt[i] > 0:
            self._wait(q, key, self.dcnt[i])
        self._deps(q, R, W)
        ins = self.nc.gpsimd.indirect_dma_start(**kw)
        self.dcnt[i] += 16
        ins.then_inc(self.dsem[i], 16)
        self.ipend.append((key, self.dcnt[i]))
        self._done(key, self.dcnt[i], R, W)
        self.nins += 1
        return ins

    def barrier(self):
        for e in ("pe", "act", "dve", "pool", "sp"):
            for o in ("pe", "act", "dve", "pool"):
                if o != e and self.cnt[o] > 0:
                    self._wait(e, o, self.cnt[o])
            for i in range(self.ndma):
                if self.dcnt[i] > 0:
                    self._wait(e, ("dma", i), self.dcnt[i])

    def regen(self):
        self.barrier()
        self.gen += 1
        for n in ("pe", "act", "dve", "pool"):
            self.sem[n] = self.top_es.enter_context(self.nc.semaphore("s_%s_g%d" % (n, self.gen)))
            self.cnt[n] = 0
        for e in self.waited:
            for n in ("pe", "act", "dve", "pool"):
                self.waited[e].pop(n, None)

    def finish(self, outs):
        for b in outs:
            if b.lw is not None:
                self._wait("sp", *b.lw)


import os
SKIP = os.environ.get('SKIP', '').split(',')
D = 1024
DIN = 2112
NB = 2
EPS = 1e-6


def build(T, stop_after=99, dbg=()):
    nc = bass.Bass("TRN2", target_bir_lowering=False)
    es = ExitStack()
    k = K(nc, es)
    NT = T // 128
    NTOK = NB * T

    def inp(name, shape, dt=F32):
        return Buf(nc.dram_tensor(name, list(shape), dt, kind="ExternalInput").ap(), name)

    x_d = inp("x", [NB * T, D])
    c_d = inp("c", [NB, D])
    ada_w_d = inp("ada_w", [D, 6 * D])
    ada_b_d = inp("ada_b", [1, 6 * D])
    norm_mix_d = inp("norm_mix", [1, D])
    w_in_d = inp("w_in", [D, DIN])
    outs = {}

    def outp(name, shape, dt=F32):
        b = Buf(nc.dram_tensor(name, list(shape), dt, kind="ExternalOutput").ap(), name)
        outs[name] = b
        return b

    ident_f = k.sb("ident_f", [128, 128], F32)
    ident_b = k.sb("ident_b", [128, 128], BF16)
    iot = k.sb("iot", [128, 128], I32)
    k.op("pool", lambda e: e.iota(iot[:], pattern=[[1, 128]], base=0, channel_multiplier=-1), W=[iot])
    k.op("dve", lambda e: e.tensor_scalar(ident_f[:], iot[:], 0.0, None, op0=ALU.is_equal), R=[iot], W=[ident_f])
    k.op("dve", lambda e: e.tensor_copy(ident_b[:], ident_f[:]), R=[ident_f], W=[ident_b])

    MOD = [[k.sb("mod%d_%d" % (b, w), [128, D]) for w in range(6)] for b in range(NB)]
    with ExitStack() as es0:
        k.es = es0
        cT = k.sb("cT", [128, NB, 8])
        cS = k.sb("cS", [128, NB, 8])
        with nc.allow_non_contiguous_dma(reason="tiny c transpose load"):
            for b in range(NB):
                k.dma("sp", cT[:, b, :], c_d[b, :].rearrange("(j p) -> p j", p=128), W=[cT])
        k.op("act", lambda e: e.activation(cS[:], cT[:], ACT.Silu), R=[cT], W=[cS])
        cB = [[k.sb("cB%d_%d" % (b, j), [128, 128]) for j in range(8)] for b in range(NB)]
        for b in range(NB):
            for j in range(8):
                k.op("dve", lambda e: e.tensor_copy(cB[b][j][:], cS[:, b, j:j + 1].to_broadcast([128, 128])),
                     R=[cS], W=[cB[b][j]])
        awb = [k.sb("awb%d" % i, [128, 8, 512]) for i in range(2)]
        abb = [k.sb("abb%d" % i, [128, 512]) for i in range(2)]
        pm = [k.ps("pm%d" % i, [128, 512]) for i in range(2)]
        for cb in range(12):
            aw = awb[cb % 2]
            ab = abb[cb % 2]
            k.dma("sp", aw[:], ada_w_d[:, cb * 512:(cb + 1) * 512].rearrange("(j p) n -> p j n", p=128), W=[aw])
            k.dma("sp", ab[:], ada_b_d[0:1, cb * 512:(cb + 1) * 512].partition_broadcast(128), W=[ab])
            for b in range(NB):
                p = pm[b]
                for j in range(8):
                    k.op("pe", lambda e: e.matmul(p[:], cB[b][j][:], aw[:, j, :], start=(j == 0), stop=(j == 7)),
                         R=[cB[b][j], aw], W=[p])
                dst = MOD[b][cb // 2]
                k.op("dve", lambda e: e.tensor_tensor(dst[:, (cb % 2) * 512:(cb % 2 + 1) * 512], p[:], ab[:], op=ALU.add),
                     R=[p, ab], W=[dst])
        nmb = k.sb("nmb", [128, D])
        k.dma("sp", nmb[:], norm_mix_d[0:1, :].partition_broadcast(128), W=[nmb])
        for b in range(NB):
            m = MOD[b][1]
            k.op("dve", lambda e: e.scalar_tensor_tensor(m[:], m[:], 1.0, nmb[:], op0=ALU.add, op1=ALU.mult),
                 R=[m, nmb], W=[m])
    k.es = es
    k.regen()
    if "mod" in dbg:
        o = outp("dbg_mod", [NB, 6, D])
        for b in range(NB):
            for w in range(6):
                k.dma("sp", o[b, w:w + 1, :], MOD[b][w][0:1, :], R=[MOD[b][w]], W=[o])
    if stop_after <= 0:
        k.finish(list(outs.values()))
        return nc, es

    U_d = outp("U", [NTOK, DIN]) if "U" in dbg else k.dram("U", [NTOK, DIN])
    with ExitStack() as es1:
        k.es = es1
        win = k.sb("win", [128, 8, DIN], BF16)
        wst = [k.sb("wst%d" % i, [128, DIN]) for i in range(2)]
        for j in range(8):
            s = wst[j % 2]
            k.dma("sp", s[:], w_in_d[j * 128:(j + 1) * 128, :], W=[s])
            k.op("pool" if j % 2 else "dve", lambda e: e.tensor_copy(win[:, j, :], s[:]), R=[s], W=[win])
        xt = [k.sb("xt%d" % i, [128, D]) for i in range(2)]
        junk = k.sb("junk", [128, D])
        ssq = [k.sb("ssq%d" % i, [128, 1]) for i in range(2)]
        rstd = [k.sb("rstd%d" % i, [128, 1]) for i in range(2)]
        hn = k.sb("hn", [128, D])
        hb = [k.sb("hb%d" % i, [128, D], BF16) for i in range(2)]
        hT = [k.sb("hT%d" % i, [128, 8, 128], BF16) for i in range(2)]
        ut = [k.sb("ut%d" % i, [128, DIN]) for i in range(2)]
        pT = [k.ps("pT%d" % i, [128, 8, 128], BF16) for i in range(2)]
        pu = [k.ps("pu%d" % i, [128, 512]) for i in range(3)]
        npu = 0
        for i in range(NB * NT):
            b = i // NT
            x_ = xt[i % 2]; sq = ssq[i % 2]; rs = rstd[i % 2]; h_ = hb[i % 2]; hT_ = hT[i % 2]; u_ = ut[i % 2]; pT_ = pT[i % 2]
            k.dma("sp", x_[:], x_d[i * 128:(i + 1) * 128, :], W=[x_])
            k.op("act", lambda e: e.activation(junk[:], x_[:], ACT.Square, accum_out=sq[:]), R=[x_], W=[junk, sq])
            k.op("dve", lambda e: e.tensor_scalar(rs[:], sq[:], 1.0 / D, EPS, op0=ALU.mult, op1=ALU.add), R=[sq], W=[rs])
            k.op("act", lambda e: e.sqrt(rs[:], rs[:]), R=[rs], W=[rs])
            k.op("dve", lambda e: e.reciprocal(rs[:], rs[:]), R=[rs], W=[rs])
            k.op("dve", lambda e: e.scalar_tensor_tensor(hn[:], x_[:], rs[:], MOD[b][1][:], op0=ALU.mult, op1=ALU.mult),
                 R=[x_, rs, MOD[b][1]], W=[hn])
            k.op("dve", lambda e: e.tensor_tensor(h_[:], hn[:], MOD[b][0][:], op=ALU.add), R=[hn, MOD[b][0]], W=[h_])
            for j in range(8):
                k.op("pe", lambda e: e.transpose(pT_[:, j, :], h_[:, j * 128:(j + 1) * 128], ident_b[:]),
                     R=[h_, ident_b], W=[pT_])
            k.op("act", lambda e: e.copy(hT_[:], pT_[:]), R=[pT_], W=[hT_])
            for cbi, (c0, c1) in enumerate([(0, 512), (512, 1024), (1024, 1536), (1536, 2048), (2048, 2112)]):
                p = pu[npu % 3]; npu += 1
                for j in range(8):
                    k.op("pe", lambda e: e.matmul(p[:, 0:c1 - c0], hT_[:, j, :], win[:, j, c0:c1], start=(j == 0), stop=(j == 7)),
                         R=[hT_, win], W=[p])
                k.op("dve" if cbi % 2 else "act",
                     (lambda e: e.tensor_copy(u_[:, c0:c1], p[:, 0:c1 - c0])) if cbi % 2 else
                     (lambda e: e.copy(u_[:, c0:c1], p[:, 0:c1 - c0])), R=[p], W=[u_])
            k.dma("sp", U_d[i * 128:(i + 1) * 128, :], u_[:], R=[u_], W=[U_d])
    k.es = es
    k.regen()
    if stop_after <= 1:
        k.finish(list(outs.values()))
        return nc, es


    rwkv_mu_d = inp("rwkv_mu", [1, 1696]); decay_w0_d = inp("decay_w0", [1, 512]); decay_up_d = inp("decay_up", [32, 512])
    iclr_a0_d = inp("iclr_a0", [1, 512]); iclr_up_d = inp("iclr_up", [32, 512]); gate_up_d = inp("gate_up", [96, 512])
    k_k_d = inp("rwkv_k_k", [1, 512]); k_a_d = inp("rwkv_k_a", [1, 512]); r_k_d = inp("rwkv_r_k", [1, 512])
    ROWS_d = outp("ROWS", [NB, T, 5, 512]) if "ROWS" in dbg else None
    ROWSW_d = k.dram("ROWSW", [NB, T, 512])
    ROWSB_d = k.dram("ROWSB", [NB, T, 4, 512], BF16)
    VT_d = outp("VT", [128, NT, 8, 128]) if "VT" in dbg else k.dram("VT", [128, NT, 8, 128])
    BON_d = outp("BON", [NTOK, 512]) if "BON" in dbg else k.dram("BON", [NTOK, 512])
    G_d = outp("G", [NTOK, 512]) if "G" in dbg else k.dram("G", [NTOK, 512])
    with ExitStack() as es2:
        k.es = es2
        def bc(name, src, n):
            t_ = k.sb(name, [128, n])
            k.dma("sp", t_[:], src[0:1, :].partition_broadcast(128), W=[t_])
            return t_
        mu_b = bc("mu_b", rwkv_mu_d, 1696); w0_b = bc("w0_b", decay_w0_d, 512); a0_b = bc("a0_b", iclr_a0_d, 512)
        kk_b = bc("kk_b", k_k_d, 512); ka_b = bc("ka_b", k_a_d, 512); rk_b = bc("rk_b", r_k_d, 512)
        dup = k.sb("dup", [32, 512]); iup = k.sb("iup", [32, 512]); gup = k.sb("gup", [96, 512])
        k.dma("sp", dup[:], decay_up_d[:], W=[dup]); k.dma("sp", iup[:], iclr_up_d[:], W=[iup]); k.dma("sp", gup[:], gate_up_d[:], W=[gup])
        uu = [k.sb("uu%d" % i, [128, 1696]) for i in range(2)]
        up_ = [k.sb("up%d" % i, [128, 1696]) for i in range(2)]
        us = k.sb("us", [128, 1696])
        Z = k.sb("Z", [128, 160]); ZT = k.sb("ZT", [96, 3, 128])
        rows = [k.sb("rows%d" % i, [128, 5, 512]) for i in range(2)]
        rowsb = [k.sb("rowsb%d" % i, [128, 4, 512], BF16) for i in range(2)]
        gt = [k.sb("gt%d" % i, [128, 512]) for i in range(2)]
        bon = [k.sb("bon%d" % i, [128, 512]) for i in range(2)]
        at = k.sb("at", [128, 512]); t5 = k.sb("t5", [128, 512]); t6 = k.sb("t6", [128, 512])
        s8 = k.sb("s8", [128, 8]); b8 = k.sb("b8", [128, 8])
        vts = [k.sb("vts%d" % i, [64, 8, 128]) for i in range(2)]
        pZ = k.ps("pZ", [128, 3, 128]); pl = k.ps("pl", [128, 3, 512]); pv = k.ps("pv", [64, 8, 128])
        for i in range(NB * NT):
            b = i // NT; it = i % NT
            u_ = uu[i % 2]; p_ = up_[i % 2]; rw = rows[i % 2]; g_ = gt[i % 2]; bo = bon[i % 2]; vt_ = vts[i % 2]
            k.dma("sp", u_[:], U_d[i * 128:(i + 1) * 128, 0:1696], R=[U_d], W=[u_])
            if it == 0:
                k.op("pool", lambda e: e.memset(p_[0:1, :], 0.0), W=[p_])
                k.dma("sp", p_[1:128, :], U_d[i * 128:i * 128 + 127, 0:1696], R=[U_d], W=[p_])
            else:
                k.dma("sp", p_[:], U_d[i * 128 - 1:i * 128 + 127, 0:1696], R=[U_d], W=[p_])
            k.op("pool", lambda e: e.tensor_tensor(p_[:], p_[:], u_[:], op=ALU.subtract), R=[p_, u_], W=[p_])
            k.op("pool", lambda e: e.tensor_tensor(p_[:], p_[:], mu_b[:], op=ALU.mult), R=[p_, mu_b], W=[p_])
            k.op("dve", lambda e: e.tensor_tensor(us[:], p_[:], u_[:], op=ALU.add), R=[p_, u_], W=[us])
            r_ = us[:, 0:512]; kx = us[:, 512:1024]; v_ = us[:, 1024:1536]
            k.op("act", lambda e: e.activation(Z[:, 0:32], us[:, 1536:1568], ACT.Tanh), R=[us], W=[Z])
            k.op("act", lambda e: e.copy(Z[:, 32:64], us[:, 1568:1600]), R=[us], W=[Z])
            k.op("act", lambda e: e.activation(Z[:, 64:160], us[:, 1600:1696], ACT.Sigmoid), R=[us], W=[Z])
            k.op("pe", lambda e: e.transpose(pZ[0:32, 0, :], Z[:, 0:32], ident_f[:]), R=[Z, ident_f], W=[pZ])
            k.op("pe", lambda e: e.transpose(pZ[0:32, 1, :], Z[:, 32:64], ident_f[:]), R=[Z, ident_f], W=[pZ])
            k.op("pe", lambda e: e.transpose(pZ[0:96, 2, :], Z[:, 64:160], ident_f[:]), R=[Z, ident_f], W=[pZ])
            k.op("dve", lambda e: e.tensor_copy(ZT[0:32, 0:2, :], pZ[0:32, 0:2, :]), R=[pZ], W=[ZT])
            k.op("dve", lambda e: e.tensor_copy(ZT[0:96, 2, :], pZ[0:96, 2, :]), R=[pZ], W=[ZT])
            k.op("pe", lambda e: e.matmul(pl[:, 0, :], ZT[0:32, 0, :], dup[:], start=True, stop=True), R=[ZT, dup], W=[pl])
            k.op("pe", lambda e: e.matmul(pl[:, 1, :], ZT[0:32, 1, :], iup[:], start=True, stop=True), R=[ZT, iup], W=[pl])
            k.op("pe", lambda e: e.matmul(pl[:, 2, :], ZT[0:96, 2, :], gup[:], start=True, stop=True), R=[ZT, gup], W=[pl])
            k.op("dve", lambda e: e.tensor_tensor(t5[:], pl[:, 0, :], w0_b[:], op=ALU.add), R=[pl, w0_b], W=[t5])
            k.op("act", lambda e: e.activation(t5[:], t5[:], ACT.Sigmoid), R=[t5], W=[t5])
            k.op("act", lambda e: e.activation(rw[:, 0, :], t5[:], ACT.Exp, scale=-float(np.exp(-0.5))), R=[t5], W=[rw])
            k.op("dve", lambda e: e.tensor_tensor(at[:], pl[:, 1, :], a0_b[:], op=ALU.add), R=[pl, a0_b], W=[at])
            k.op("act", lambda e: e.activation(at[:], at[:], ACT.Sigmoid), R=[at], W=[at])
            k.op("act", lambda e: e.copy(g_[:], pl[:, 2, :]), R=[pl], W=[g_])
            k.op("dve", lambda e: e.tensor_tensor(rw[:, 1, :], kx, kk_b[:], op=ALU.mult), R=[us, kk_b], W=[rw])
            k.op("pool", lambda e: e.tensor_tensor(t6[:], rw[:, 1, :], rw[:, 1, :], op=ALU.mult), R=[rw], W=[t6])
            k.op("dve", lambda e: e.tensor_reduce(s8[:], t6[:].rearrange("p (h n) -> p h n", h=8), axis=AX.X, op=ALU.add), R=[t6], W=[s8])
            k.op("dve", lambda e: e.tensor_scalar(s8[:], s8[:], 1e-24, None, op0=ALU.max), R=[s8], W=[s8])
            k.op("act", lambda e: e.sqrt(s8[:], s8[:]), R=[s8], W=[s8])
            k.op("dve", lambda e: e.reciprocal(s8[:], s8[:]), R=[s8], W=[s8])
            k.op("dve", lambda e: e.tensor_tensor(rw[:, 1, :].rearrange("p (h n) -> p h n", h=8), rw[:, 1, :].rearrange("p (h n) -> p h n", h=8),
                                                  s8[:].unsqueeze(2).to_broadcast([128, 8, 64]), op=ALU.mult), R=[rw, s8], W=[rw])
            k.op("pool", lambda e: e.tensor_tensor(rw[:, 2, :], rw[:, 1, :], at[:], op=ALU.mult), R=[rw, at], W=[rw])
            k.op("dve", lambda e: e.scalar_tensor_tensor(t5[:], at[:], -1.0, ka_b[:], op0=ALU.add, op1=ALU.mult), R=[at, ka_b], W=[t5])
            k.op("dve", lambda e: e.scalar_tensor_tensor(rw[:, 3, :], t5[:], 1.0, kx, op0=ALU.add, op1=ALU.mult), R=[t5, us], W=[rw])
            k.op("act", lambda e: e.copy(rw[:, 4, :], r_), R=[us], W=[rw])
            k.op("pool", lambda e: e.tensor_tensor(t6[:], rw[:, 3, :], r_, op=ALU.mult), R=[rw, us], W=[t6])
            k.op("pool", lambda e: e.tensor_tensor(t6[:], t6[:], rk_b[:], op=ALU.mult), R=[t6, rk_b], W=[t6])
            k.op("dve", lambda e: e.tensor_reduce(b8[:], t6[:].rearrange("p (h n) -> p h n", h=8), axis=AX.X, op=ALU.add), R=[t6], W=[b8])
            k.op("dve", lambda e: e.tensor_tensor(bo[:].rearrange("p (h n) -> p h n", h=8), v_.rearrange("p (h n) -> p h n", h=8),
                                                  b8[:].unsqueeze(2).to_broadcast([128, 8, 64]), op=ALU.mult), R=[us, b8], W=[bo])
            for h in range(8):
                k.op("pe", lambda e: e.transpose(pv[:, h, :], us[:, 1024 + h * 64:1024 + (h + 1) * 64], ident_f[:]), R=[us, ident_f], W=[pv])
            k.op("act", lambda e: e.copy(vt_[:], pv[:]), R=[pv], W=[vt_])
            rwb = rowsb[i % 2]
            k.op("act", lambda e: e.copy(rwb[:], rw[:, 1:5, :]), R=[rw], W=[rwb])
            if ROWS_d is not None:
                k.dma("sp", ROWS_d[b, it * 128:(it + 1) * 128, :, :], rw[:], R=[rw], W=[ROWS_d])
            k.dma("sp", ROWSW_d[b, it * 128:(it + 1) * 128, :], rw[:, 0, :], R=[rw], W=[ROWSW_d])
            k.dma("sp", ROWSB_d[b, it * 128:(it + 1) * 128, :, :], rwb[:], R=[rwb], W=[ROWSB_d])
            k.dma("sp", VT_d[b * 64:(b + 1) * 64, it, :, :], vt_[:], R=[vt_], W=[VT_d])
            k.dma("sp", BON_d[i * 128:(i + 1) * 128, :], bo[:], R=[bo], W=[BON_d])
            k.dma("sp", G_d[i * 128:(i + 1) * 128, :], g_[:], R=[g_], W=[G_d])
    k.es = es
    k.regen()
    if stop_after <= 2:
        k.finish(list(outs.values()))
        return nc, es


    TBS = 8
    YT_d = outp("YT", [128, T, 8]) if "YT" in dbg else k.dram("YT", [128, T, 8])
    with ExitStack() as es3:
        k.es = es3
        S = k.sb("S", [128, 512])
        t1 = k.sb("t1", [128, 512]); t2 = k.sb("t2", [128, 512])
        t3 = [k.sb("t3_%d" % i, [128, 512]) for i in range(2)]
        t4 = [k.sb("t4_%d" % i, [128, 512]) for i in range(2)]
        sa = k.sb("sa", [128, 8])
        RWt = [es3.enter_context(nc.sbuf_tensor("RW%d" % i, [128, TBS, 512], F32)) for i in range(2)]
        RBt = [es3.enter_context(nc.sbuf_tensor("RB%d" % i, [128, TBS, 4, 512], BF16)) for i in range(2)]
        RB0 = [Buf(t_, "rb0") for t_ in RBt]; RB1 = [Buf(t_, "rb1") for t_ in RBt]
        RW0 = [Buf(t_, "rw0") for t_ in RWt]; RW1 = [Buf(t_, "rw1") for t_ in RWt]
        VTc = [k.sb("VTc%d" % i, [128, 8, 128]) for i in range(2)]
        Yc = [k.sb("Yc%d" % i, [128, 128, 8]) for i in range(2)]
        SPS = [k.ps("SPS%d" % i, [128, 512]) for i in range(2)]
        k.op("dve", lambda e: e.memset(SPS[0][:], 0.0), W=[SPS[0]])
        k.op("dve", lambda e: e.memset(SPS[1][:], 0.0), W=[SPS[1]])
        v3 = lambda ap: ap.rearrange("p (h n) -> p h n", h=8)
        nblk = 0; nst = 0; pend_y = None
        SW = [k.sb("SW%d" % i, [128, 512]) for i in range(2)]
        for it in range(NT):
            vt_ = VTc[it % 2]; y_ = Yc[it % 2]
            k.dma("sp", vt_[:], VT_d[:, it, :, :], R=[VT_d], W=[vt_])
            for blk in range(128 // TBS):
                t0 = it * 128 + blk * TBS
                rbt = RBt[nblk % 2]; rb0 = RB0[nblk % 2]; rb1 = RB1[nblk % 2]
                rwt = RWt[nblk % 2]; rw0 = RW0[nblk % 2]; rw1 = RW1[nblk % 2]; nblk += 1
                k.dma("sp", rbt[0:64].rearrange("p t f n -> p (t f n)"),
                      ROWSB_d[0:1, t0:t0 + TBS].rearrange("o t f n -> o (t f n)").partition_broadcast(64), R=[ROWSB_d], W=[rb0])
                k.dma("act", rbt[64:128].rearrange("p t f n -> p (t f n)"),
                      ROWSB_d[1:2, t0:t0 + TBS].rearrange("o t f n -> o (t f n)").partition_broadcast(64), R=[ROWSB_d], W=[rb1])
                k.dma("sp", rwt[0:64].rearrange("p t n -> p (t n)"),
                      ROWSW_d[0:1, t0:t0 + TBS].rearrange("o t n -> o (t n)").partition_broadcast(64), R=[ROWSW_d], W=[rw0])
                k.dma("act", rwt[64:128].rearrange("p t n -> p (t n)"),
                      ROWSW_d[1:2, t0:t0 + TBS].rearrange("o t n -> o (t n)").partition_broadcast(64), R=[ROWSW_d], W=[rw1])
                RB = [rb0, rb1]; RWB = [rw0, rw1]
                for s_ in range(TBS):
                    tl = blk * TBS + s_
                    Wb = rwt[:, s_, :]; KKb = rbt[:, s_, 0, :]; KKAb = rbt[:, s_, 1, :]; Kb = rbt[:, s_, 2, :]; Rb = rbt[:, s_, 3, :]
                    t3_ = t3[nst % 2]; t4_ = t4[nst % 2]; sw_ = SW[nst % 2]; cur = SPS[nst % 2]; nxt = SPS[(nst + 1) % 2]; nst += 1
                    def emit_t3(buf, sidx):
                        Kb_ = rbt[:, sidx, 2, :]
                        k.op("pool", lambda e: e.tensor_tensor(v3(buf[:]), v3(Kb_), vt_[:, :, blk * TBS + sidx].unsqueeze(2).to_broadcast([128, 8, 64]), op=ALU.mult),
                             R=RB + [vt_], W=[buf])
                    if s_ == 0:
                        emit_t3(t3_, s_)
                    k.op("pe", lambda e: e.matmul(nxt[:], ident_f[:], t3_[:], start=True, stop=False), R=[ident_f, t3_], W=[nxt])
                    k.op("dve", lambda e: e.tensor_tensor(t1[:], cur[:], KKb, op=ALU.mult), R=[cur] + RB, W=[t1])
                    k.op("dve", lambda e: e.tensor_reduce(sa[:], v3(t1[:]), axis=AX.X, op=ALU.add, negate=True), R=[t1], W=[sa])
                    k.op("pool", lambda e: e.tensor_tensor(v3(t2[:]), v3(KKAb), sa[:].unsqueeze(2).to_broadcast([128, 8, 64]), op=ALU.mult),
                         R=RB + [sa], W=[t2])
                    if s_ + 1 < TBS:
                        emit_t3(t3[nst % 2], s_ + 1)
                    k.op("dve", lambda e: e.tensor_tensor(sw_[:], cur[:], Wb, op=ALU.mult), R=[cur] + RWB, W=[sw_])
                    k.op("pe", lambda e: e.matmul(nxt[:], ident_f[:], sw_[:], start=False, stop=False), R=[ident_f, sw_], W=[nxt])
                    k.op("pe", lambda e: e.matmul(nxt[:], ident_f[:], t2[:], start=False, stop=True), R=[ident_f, t2], W=[nxt])
                    if pend_y is not None:
                        pt4, pRb, pRB, pyd, ptl = pend_y
                        k.op("dve", lambda e: e.tensor_tensor(pt4[:], cur[:], pRb, op=ALU.mult), R=[cur] + pRB, W=[pt4])
                        k.op("dve", lambda e: e.tensor_reduce(pyd[:, ptl, :], v3(pt4[:]), axis=AX.X, op=ALU.add), R=[pt4], W=[pyd])
                    pend_y = (t4_, Rb, RB, y_, tl)
            pt4, pRb, pRB, pyd, ptl = pend_y
            cur = SPS[nst % 2]
            k.op("dve", lambda e: e.tensor_tensor(pt4[:], cur[:], pRb, op=ALU.mult), R=[cur] + pRB, W=[pt4])
            k.op("dve", lambda e: e.tensor_reduce(pyd[:, ptl, :], v3(pt4[:]), axis=AX.X, op=ALU.add), R=[pt4], W=[pyd])
            pend_y = None
            k.dma("sp", YT_d[:, it * 128:(it + 1) * 128, :], y_[:], R=[y_], W=[YT_d])
            if it % 12 == 11 and it != NT - 1:
                k.regen()
    k.es = es
    k.regen()
    if stop_after <= 3:
        k.finish(list(outs.values()))
        return nc, es

    ln_w_d = inp("ln_x_w", [1, 512]); ln_b_d = inp("ln_x_b", [1, 512])
    YCAT_d = outp("YCAT", [NTOK, 1024]) if "YCAT" in dbg else k.dram("YCAT", [NTOK, 1024])
    with ExitStack() as es4:
        k.es = es4
        lnw_b = k.sb("lnw_b", [128, 512]); lnb_b = k.sb("lnb_b", [128, 512])
        k.dma("sp", lnw_b[:], ln_w_d[0:1, :].partition_broadcast(128), W=[lnw_b])
        k.dma("sp", lnb_b[:], ln_b_d[0:1, :].partition_broadcast(128), W=[lnb_b])
        yc = [k.sb("ycp%d" % i, [128, 128, 8]) for i in range(2)]
        py = k.ps("py", [128, 8, 128])
        ytm = k.sb("ytm", [128, 8, 128])
        yb = k.sb("yb", [128, 8, 64]); yq = k.sb("yq", [128, 8, 64])
        m8 = k.sb("m8", [128, 8]); v8 = k.sb("v8", [128, 8])
        bo = [k.sb("pbo%d" % i, [128, 512]) for i in range(2)]; g_ = [k.sb("pg%d" % i, [128, 512]) for i in range(2)]
        yo = [k.sb("yo%d" % i, [128, 512]) for i in range(2)]
        n = 0
        for it in range(NT):
            y_ = yc[it % 2]
            k.dma("sp", y_[:], YT_d[:, it * 128:(it + 1) * 128, :], R=[YT_d], W=[y_])
            for h in range(8):
                k.op("pe", lambda e: e.transpose(py[:, h, :], y_[:, :, h], ident_f[:]), R=[y_, ident_f], W=[py])
            k.op("act", lambda e: e.copy(ytm[:], py[:]), R=[py], W=[ytm])
            for b in range(NB):
                i = b * NT + it
                bo_ = bo[n % 2]; gg = g_[n % 2]; yo_ = yo[n % 2]; n += 1
                k.dma("sp", bo_[:], BON_d[i * 128:(i + 1) * 128, :], R=[BON_d], W=[bo_])
                k.dma("sp", gg[:], G_d[i * 128:(i + 1) * 128, :], R=[G_d], W=[gg])
                ysl = ytm[:, :, b * 64:(b + 1) * 64]
                k.op("dve", lambda e: e.tensor_reduce(m8[:], ysl, axis=AX.X, op=ALU.add), R=[ytm], W=[m8])
                k.op("dve", lambda e: e.tensor_scalar(m8[:], m8[:], 1.0 / 64, None, op0=ALU.mult), R=[m8], W=[m8])
                k.op("dve", lambda e: e.tensor_tensor(yb[:], ysl, m8[:].unsqueeze(2).to_broadcast([128, 8, 64]), op=ALU.subtract), R=[ytm, m8], W=[yb])
                k.op("pool", lambda e: e.tensor_tensor(yq[:], yb[:], yb[:], op=ALU.mult), R=[yb], W=[yq])
                k.op("dve", lambda e: e.tensor_reduce(v8[:], yq[:], axis=AX.X, op=ALU.add), R=[yq], W=[v8])
                k.op("dve", lambda e: e.tensor_scalar(v8[:], v8[:], 1.0 / 64, 64e-5, op0=ALU.mult, op1=ALU.add), R=[v8], W=[v8])
                k.op("act", lambda e: e.sqrt(v8[:], v8[:]), R=[v8], W=[v8])
                k.op("dve", lambda e: e.reciprocal(v8[:], v8[:]), R=[v8], W=[v8])
                k.op("dve", lambda e: e.tensor_tensor(yb[:], yb[:], v8[:].unsqueeze(2).to_broadcast([128, 8, 64]), op=ALU.mult), R=[yb, v8], W=[yb])
                ybf = yb[:].rearrange("p h n -> p (h n)")
                k.op("dve", lambda e: e.tensor_tensor(yo_[:], ybf, lnw_b[:], op=ALU.mult), R=[yb, lnw_b], W=[yo_])
                k.op("pool", lambda e: e.tensor_tensor(yo_[:], yo_[:], lnb_b[:], op=ALU.add), R=[yo_, lnb_b], W=[yo_])
                k.op("pool", lambda e: e.tensor_tensor(yo_[:], yo_[:], bo_[:], op=ALU.add), R=[yo_, bo_], W=[yo_])
                k.op("dve", lambda e: e.tensor_tensor(yo_[:], yo_[:], gg[:], op=ALU.mult), R=[yo_, gg], W=[yo_])
                k.dma("sp", YCAT_d[i * 128:(i + 1) * 128, 0:512], yo_[:], R=[yo_], W=[YCAT_d])
    k.es = es
    k.regen()
    if stop_after <= 4:
        k.finish(list(outs.values()))
        return nc, es


    pos_d = inp("positions", [NTOK, 1], I32)
    qan_d = inp("q_a_norm", [1, 256]); wqb_d = inp("w_q_b", [256, 768]); kvan_d = inp("kv_a_norm", [1, 128])
    wkvb_d = inp("w_kv_b", [128, 1024]); qn_d = inp("q_norm", [1, 96]); kn_d = inp("k_norm", [1, 96])
    QT_d = outp("QT", [NB, 96, 8, T], BF16) if "QT" in dbg else k.dram("QT", [NB, 96, 8, T], BF16)
    KT_d = outp("KT", [NB, 96, 8, T], BF16) if "KT" in dbg else k.dram("KT", [NB, 96, 8, T], BF16)
    V_d = outp("V", [NB, T, 8, 66], BF16) if "V" in dbg else k.dram("V", [NB, T, 8, 66], BF16)
    TWO_PI = float(2 * np.pi)
    with ExitStack() as es5:
        k.es = es5
        def bc(name, src, n):
            t_ = k.sb(name, [128, n])
            k.dma("sp", t_[:], src[0:1, :].partition_broadcast(128), W=[t_])
            return t_
        qan_b = bc("qan_b", qan_d, 256); kvan_b = bc("kvan_b", kvan_d, 128); qn96 = bc("qn96", qn_d, 96); kn96 = bc("kn96", kn_d, 96)
        qn_b = k.sb("qn_b", [128, 8, 96]); kn_b = k.sb("kn_b", [128, 8, 96])
        k.op("dve", lambda e: e.tensor_copy(qn_b[:], qn96[:].unsqueeze(1).to_broadcast([128, 8, 96])), R=[qn96], W=[qn_b])
        k.op("dve", lambda e: e.tensor_copy(kn_b[:], kn96[:].unsqueeze(1).to_broadcast([128, 8, 96])), R=[kn96], W=[kn_b])
        wq_st = k.sb("wq_st", [128, 2, 768]); wq = k.sb("wq", [128, 2, 768], BF16)
        k.dma("sp", wq_st[:], wqb_d[:].rearrange("(c p) n -> p c n", p=128), W=[wq_st])
        k.op("dve", lambda e: e.tensor_copy(wq[:], wq_st[:]), R=[wq_st], W=[wq])
        wkv_st = k.sb("wkv_st", [128, 1024]); wkv = k.sb("wkv", [128, 1024], BF16)
        k.dma("sp", wkv_st[:], wkvb_d[:], W=[wkv_st])
        k.op("dve", lambda e: e.tensor_copy(wkv[:], wkv_st[:]), R=[wkv_st], W=[wkv])
        ji = k.sb("ji", [128, 16], I32); invf = k.sb("invf", [128, 16])
        k.op("pool", lambda e: e.iota(ji[:], pattern=[[1, 16]], base=0, channel_multiplier=0), W=[ji])
        k.op("dve", lambda e: e.tensor_copy(invf[:], ji[:]), R=[ji], W=[invf])
        k.op("act", lambda e: e.activation(invf[:], invf[:], ACT.Exp, scale=-float(np.log(10000.0) / 16)), R=[invf], W=[invf])
        um = [k.sb("um%d" % i, [128, 416]) for i in range(2)]
        posi = k.sb("posi", [128, 1], I32); posf = k.sb("posf", [128, 1])
        ang = k.sb("ang", [128, 32]); angi = k.sb("angi", [128, 32], I32); angf = k.sb("angf", [128, 32]); msk = k.sb("msk", [128, 32])
        cs = k.sb("cs", [128, 32])
        junkm = k.sb("junkm", [128, 256]); s1 = k.sb("s1", [128, 1]); s2 = k.sb("s2", [128, 1])
        qlb = k.sb("qlb", [128, 256], BF16); kvb = k.sb("kvb", [128, 128], BF16)
        qlT = k.sb("qlT", [128, 2, 128], BF16); kvT = k.sb("kvT", [128, 128], BF16)
        pq = k.ps("pq", [128, 2, 512]); pkv = k.ps("pkv", [128, 2, 512])
        ptr = k.ps("ptr", [128, 3, 128], BF16)
        ptq = k.ps("ptq", [96, 8, 128], BF16)
        qf = k.sb("qf", [128, 8, 96]); kf = k.sb("kf", [128, 8, 96]); sqt = k.sb("sqt", [128, 8, 96])
        r8 = k.sb("r8", [128, 8])
        ra = k.sb("ra", [128, 8, 16]); rb_ = k.sb("rb_", [128, 8, 16]); rc = k.sb("rc", [128, 8, 16]); rd_ = k.sb("rd_", [128, 8, 16])
        qfb = k.sb("qfb", [128, 8, 96], BF16)
        qTs = [k.sb("qTs%d" % i, [96, 8, 128], BF16) for i in range(2)]
        kTs = [k.sb("kTs%d" % i, [96, 8, 128], BF16) for i in range(2)]
        vs = [k.sb("vs%d" % i, [128, 8, 66], BF16) for i in range(2)]
        for vv_ in vs:
            k.op("pool", lambda e: e.memset(vv_[:, :, 64:66], 1.0), W=[vv_])

        def headnorm_rope(xf, gain_b, outT, dstT, b, it):
            k.op("pool", lambda e: e.tensor_tensor(sqt[:], xf[:], xf[:], op=ALU.mult), R=[xf], W=[sqt])
            k.op("dve", lambda e: e.tensor_reduce(r8[:], sqt[:], axis=AX.X, op=ALU.add), R=[sqt], W=[r8])
            k.op("dve", lambda e: e.tensor_scalar(r8[:], r8[:], 1.0 / 96, EPS, op0=ALU.mult, op1=ALU.add), R=[r8], W=[r8])
            k.op("act", lambda e: e.sqrt(r8[:], r8[:]), R=[r8], W=[r8])
            k.op("dve", lambda e: e.reciprocal(r8[:], r8[:]), R=[r8], W=[r8])
            k.op("dve", lambda e: e.tensor_tensor(xf[:], xf[:], r8[:].unsqueeze(2).to_broadcast([128, 8, 96]), op=ALU.mult), R=[xf, r8], W=[xf])
            k.op("dve", lambda e: e.tensor_tensor(xf[:], xf[:], gain_b[:], op=ALU.mult), R=[xf, gain_b], W=[xf])
            sinb = cs[:, 0:16].unsqueeze(1).to_broadcast([128, 8, 16]); cosb = cs[:, 16:32].unsqueeze(1).to_broadcast([128, 8, 16])
            x1 = xf[:, :, 64:80]; x2 = xf[:, :, 80:96]
            k.op("dve", lambda e: e.tensor_tensor(ra[:], x1, cosb, op=ALU.mult), R=[xf, cs], W=[ra])
            k.op("dve", lambda e: e.tensor_tensor(rb_[:], x2, sinb, op=ALU.mult), R=[xf, cs], W=[rb_])
            k.op("dve", lambda e: e.tensor_tensor(rc[:], x2, cosb, op=ALU.mult), R=[xf, cs], W=[rc])
            k.op("dve", lambda e: e.tensor_tensor(rd_[:], x1, sinb, op=ALU.mult), R=[xf, cs], W=[rd_])
            k.op("dve", lambda e: e.tensor_tensor(x1, ra[:], rb_[:], op=ALU.subtract), R=[ra, rb_], W=[xf])
            k.op("dve", lambda e: e.tensor_tensor(x2, rc[:], rd_[:], op=ALU.add), R=[rc, rd_], W=[xf])
            k.op("act", lambda e: e.copy(qfb[:], xf[:]), R=[xf], W=[qfb])
            for h in range(8):
                k.op("pe", lambda e: e.transpose(ptq[:, h, :], qfb[:, h, :], ident_b[:]), R=[qfb, ident_b], W=[ptq])
            k.op("act", lambda e: e.copy(outT[:], ptq[:]), R=[ptq], W=[outT])
            if "noQT" not in SKIP:
                k.dma("sp", dstT[b, :, :, it * 128:(it + 1) * 128], outT[:], R=[outT], W=[dstT])

        for i in range(NB * NT):
            b = i // NT; it = i % NT
            u_ = um[i % 2]
            k.dma("sp", u_[:], U_d[i * 128:(i + 1) * 128, 1696:2112], R=[U_d], W=[u_])
            k.dma("sp", posi[:], pos_d[i * 128:(i + 1) * 128, :], W=[posi])
            if "noROPE" not in SKIP:
                k.op("dve", lambda e: e.tensor_copy(posf[:], posi[:]), R=[posi], W=[posf])
                k.op("dve", lambda e: e.tensor_scalar(ang[:, 0:16], invf[:], posf[:], None, op0=ALU.mult), R=[invf, posf], W=[ang])
                k.op("dve", lambda e: e.tensor_scalar(ang[:, 16:32], ang[:, 0:16], float(np.pi / 2), None, op0=ALU.add), R=[ang], W=[ang])
                k.op("dve", lambda e: e.tensor_scalar(angf[:], ang[:], 1.0 / TWO_PI, None, op0=ALU.mult), R=[ang], W=[angf])
                k.op("dve", lambda e: e.tensor_copy(angi[:], angf[:]), R=[angf], W=[angi])
                k.op("dve", lambda e: e.tensor_copy(angf[:], angi[:]), R=[angi], W=[angf])
                k.op("dve", lambda e: e.scalar_tensor_tensor(ang[:], angf[:], -TWO_PI, ang[:], op0=ALU.mult, op1=ALU.add), R=[angf, ang], W=[ang])
                k.op("dve", lambda e: e.tensor_scalar(msk[:], ang[:], float(np.pi), -TWO_PI, op0=ALU.is_gt, op1=ALU.mult), R=[ang], W=[msk])
                k.op("dve", lambda e: e.tensor_tensor(ang[:], ang[:], msk[:], op=ALU.add), R=[ang, msk], W=[ang])
                k.op("dve", lambda e: e.tensor_scalar(msk[:], ang[:], -float(np.pi), TWO_PI, op0=ALU.is_lt, op1=ALU.mult), R=[ang], W=[msk])
                k.op("dve", lambda e: e.tensor_tensor(ang[:], ang[:], msk[:], op=ALU.add), R=[ang, msk], W=[ang])
                k.op("act", lambda e: e.activation(cs[:], ang[:], ACT.Sin), R=[ang], W=[cs])
            if "noLAT" not in SKIP:
                k.op("act", lambda e: e.activation(junkm[:, 0:256], u_[:, 0:256], ACT.Square, accum_out=s1[:]), R=[u_], W=[junkm, s1])
                k.op("dve", lambda e: e.tensor_scalar(s1[:], s1[:], 1.0 / 256, EPS, op0=ALU.mult, op1=ALU.add), R=[s1], W=[s1])
                k.op("act", lambda e: e.sqrt(s1[:], s1[:]), R=[s1], W=[s1])
                k.op("dve", lambda e: e.reciprocal(s1[:], s1[:]), R=[s1], W=[s1])
                k.op("dve", lambda e: e.scalar_tensor_tensor(qlb[:], u_[:, 0:256], s1[:], qan_b[:], op0=ALU.mult, op1=ALU.mult), R=[u_, s1, qan_b], W=[qlb])
                k.op("act", lambda e: e.activation(junkm[:, 0:128], u_[:, 256:384], ACT.Square, accum_out=s2[:]), R=[u_], W=[junkm, s2])
                k.op("dve", lambda e: e.tensor_scalar(s2[:], s2[:], 1.0 / 128, EPS, op0=ALU.mult, op1=ALU.add), R=[s2], W=[s2])
                k.op("act", lambda e: e.sqrt(s2[:], s2[:]), R=[s2], W=[s2])
                k.op("dve", lambda e: e.reciprocal(s2[:], s2[:]), R=[s2], W=[s2])
                k.op("dve", lambda e: e.scalar_tensor_tensor(kvb[:], u_[:, 256:384], s2[:], kvan_b[:], op0=ALU.mult, op1=ALU.mult), R=[u_, s2, kvan_b], W=[kvb])
                k.op("pe", lambda e: e.transpose(ptr[:, 0, :], qlb[:, 0:128], ident_b[:]), R=[qlb, ident_b], W=[ptr])
                k.op("pe", lambda e: e.transpose(ptr[:, 1, :], qlb[:, 128:256], ident_b[:]), R=[qlb, ident_b], W=[ptr])
                k.op("pe", lambda e: e.transpose(ptr[:, 2, :], kvb[:], ident_b[:]), R=[kvb, ident_b], W=[ptr])
                k.op("act", lambda e: e.copy(qlT[:], ptr[:, 0:2, :]), R=[ptr], W=[qlT])
                k.op("act", lambda e: e.copy(kvT[:], ptr[:, 2, :]), R=[ptr], W=[kvT])
                if "noA" not in SKIP:
                    for c in range(2):
                        k.op("pe", lambda e: e.matmul(pq[:, 0, :], qlT[:, c, :], wq[:, c, 0:512], start=(c == 0), stop=(c == 1)), R=[qlT, wq], W=[pq])
                    for c in range(2):
                        k.op("pe", lambda e: e.matmul(pq[:, 1, 0:256], qlT[:, c, :], wq[:, c, 512:768], start=(c == 0), stop=(c == 1)), R=[qlT, wq], W=[pq])
                    k.op("pe", lambda e: e.matmul(pkv[:, 0, :], kvT[:], wkv[:, 0:512], start=True, stop=True), R=[kvT, wkv], W=[pkv])
                    k.op("pe", lambda e: e.matmul(pkv[:, 1, :], kvT[:], wkv[:, 512:1024], start=True, stop=True), R=[kvT, wkv], W=[pkv])
                    qflat = qf[:].rearrange("p h d -> p (h d)")
                    k.op("act", lambda e: e.copy(qflat[:, 0:512], pq[:, 0, :]), R=[pq], W=[qf])
                    k.op("act", lambda e: e.copy(qflat[:, 512:768], pq[:, 1, 0:256]), R=[pq], W=[qf])
                    kv3 = pkv[:].rearrange("p c (h e) -> p (c h) e", e=128)
                    k.op("dve", lambda e: e.tensor_copy(kf[:, :, 0:64], kv3[:, :, 0:64]), R=[pkv], W=[kf])
                    k.op("dve", lambda e: e.tensor_copy(kf[:, :, 64:96], u_[:, 384:416].unsqueeze(1).to_broadcast([128, 8, 32])), R=[u_], W=[kf])
            v_ = vs[i % 2]
            if "noLAT" not in SKIP and "noA" not in SKIP and "noVC" not in SKIP:
                k.op("dve", lambda e: e.tensor_copy(v_[:, :, 0:64], kv3[:, :, 64:128]), R=[pkv], W=[v_])
            if "noV" not in SKIP:
                k.dma("sp", V_d[b, it * 128:(it + 1) * 128, :, :], v_[:], R=[v_], W=[V_d])
            if "noHN" not in SKIP:
                headnorm_rope(qf, qn_b, qTs[i % 2], QT_d, b, it)
                headnorm_rope(kf, kn_b, kTs[i % 2], KT_d, b, it)
    k.es = es
    k.regen()
    if stop_after <= 5:
        k.finish(list(outs.values()))
        return nc, es


    QS = min(4, NT)
    NJ = NT // QS
    QW = QS * 128
    with ExitStack() as es6:
        k.es = es6
        MASK = []
        mi = k.sb("mi", [128, QW], I32)
        for r_ in range(QS):
            m_ = k.sb("mask%d" % r_, [128, QW], BF16)
            k.op("pool", lambda e: e.iota(mi[:], pattern=[[1, QW]], base=-r_ * 128, channel_multiplier=-1), R=[], W=[mi])
            k.op("dve", lambda e: e.tensor_scalar(m_[:], mi[:], 0.0, None, op0=ALU.is_ge), R=[mi], W=[m_])
            MASK.append(m_)
        Vall = k.sb("Vall", [128, NT, 8, 66], BF16)
        QTs = [k.sb("QTs%d" % i, [96, T], BF16) for i in range(2)]
        KTs = [k.sb("KTs%d" % i, [96, T], BF16) for i in range(2)]
        pTs = [k.sb("pTs%d" % i, [128, QW], BF16) for i in range(3)]
        ps_ = [k.ps("ps%d" % i, [128, 512]) for i in range(2)]
        po = [k.ps("po%d" % i, [128, 512]) for i in range(QS)]
        rec = k.sb("rec", [128, 1])
        ym = [k.sb("ym%d" % i, [128, 64]) for i in range(4)]
        SC = float(96 ** -0.5)
        nbh = 0; npt = 0; nym = 0
        for b in range(NB):
            for kb in range(NT):
                k.dma("sp", Vall[:, kb, :, :], V_d[b, kb * 128:(kb + 1) * 128, :, :], R=[V_d], W=[Vall])
            for h in range(8):
                Q_ = QTs[nbh % 2]; K_ = KTs[nbh % 2]; nbh += 1
                k.dma("sp", Q_[:], QT_d[b, :, h, :], R=[QT_d], W=[Q_])
                k.dma("sp", K_[:], KT_d[b, :, h, :], R=[KT_d], W=[K_])
                its = [(J, kb) for J in range(NJ) for kb in range(QS * J + QS)]
                def emit_qk(n):
                    J, kb = its[n]
                    p_ = ps_[(npt0 + n) % 2]
                    k.op("pe", lambda e: e.matmul(p_[:, 0:QW], K_[:, kb * 128:(kb + 1) * 128], Q_[:, J * QW:(J + 1) * QW], start=True, stop=True),
                         R=[K_, Q_], W=[p_])
                npt0 = npt
                emit_qk(0)
                for n, (J, kb) in enumerate(its):
                    if n + 1 < len(its):
                        emit_qk(n + 1)
                    p_ = ps_[(npt0 + n) % 2]; pT = pTs[(npt0 + n) % 3]
                    k.op("act", lambda e: e.activation(pT[:], p_[:, 0:QW], ACT.Exp, scale=SC), R=[p_], W=[pT])
                    r_ = kb - QS * J
                    if r_ >= 0:
                        k.op("dve", lambda e: e.tensor_tensor(pT[:], pT[:], MASK[r_][:], op=ALU.mult), R=[pT, MASK[r_]], W=[pT])
                    for qb in range(QS):
                        if QS * J + qb >= kb:
                            k.op("pe", lambda e: e.matmul(po[qb][:, 0:65], pT[:, qb * 128:(qb + 1) * 128], Vall[:, kb, h, 0:65],
                                                          start=(kb == 0), stop=(kb == QS * J + qb)), R=[pT, Vall], W=[po[qb]])
                    if kb == QS * J + QS - 1:
                        for qb in range(QS):
                            y_ = ym[nym % 4]; nym += 1
                            k.op("dve", lambda e: e.reciprocal(rec[:], po[qb][:, 64:65]), R=[po[qb]], W=[rec])
                            k.op("dve", lambda e: e.tensor_scalar(y_[:], po[qb][:, 0:64], rec[:], None, op0=ALU.mult), R=[po[qb], rec], W=[y_])
                            i = b * NT + QS * J + qb
                            k.dma("sp", YCAT_d[i * 128:(i + 1) * 128, 512 + h * 64:512 + (h + 1) * 64], y_[:], R=[y_], W=[YCAT_d])
                npt += len(its)
    k.es = es
    k.regen()
    if stop_after <= 6:
        k.finish(list(outs.values()))
        return nc, es


    NE = 256
    BLK = 256
    LOGB = 8
    NBLK = (NTOK * 8 + NE * (BLK - 1)) // BLK
    NROWS = NBLK * BLK
    w_out_d = inp("w_out", [D, D]); norm_ffn_d = inp("norm_ffn", [1, D]); w_router_d = inp("w_router", [D, NE]); rbias_d = inp("router_bias", [1, NE])
    X1_d = outp("X1", [NTOK, D]) if "X1" in dbg else k.dram("X1", [NTOK, D])
    H2T_d = outp("H2T", [128, 8, NTOK], BF16) if "H2T" in dbg else k.dram("H2T", [128, 8, NTOK], BF16)
    XBUF_d = k.dram("XBUF", [NROWS, D], BF16)
    YBUF_d = k.dram("YBUF", [NROWS, D], BF16)
    H2B_d = k.dram("H2B", [NTOK, D], BF16)
    dbgH2 = outp("H2", [NTOK, D]) if "H2" in dbg else None
    dbgG = outp("GATE", [NTOK, NE]) if "GATE" in dbg else None
    dbgDW = outp("DW", [NTOK, 16]) if "DW" in dbg else None
    NTT = NB * NT
    BREG = nc.gpsimd.to_reg(NROWS - 1)
    DESTI = k.sb("DESTI", [128, NTT, 8], I32)
    WK = k.sb("WK", [128, NTT, 8])
    IDXG = k.sb("IDXG", [128, NBLK], I32)
    with ExitStack() as es7:
        k.es = es7
        nfb = k.sb("nfb", [128, D])
        k.dma("sp", nfb[:], norm_ffn_d[0:1, :].partition_broadcast(128), W=[nfb])
        for b in range(NB):
            m = MOD[b][4]
            k.op("dve", lambda e: e.scalar_tensor_tensor(m[:], m[:], 1.0, nfb[:], op0=ALU.add, op1=ALU.mult), R=[m, nfb], W=[m])
        rb_b = k.sb("rb_b", [128, NE])
        k.dma("sp", rb_b[:], rbias_d[0:1, :].partition_broadcast(128), W=[rb_b])
        wo = k.sb("wo", [128, 8, D], BF16); wr = k.sb("wr", [128, 8, NE])
        wst = [k.sb("wost%d" % i, [128, D]) for i in range(2)]
        for j in range(8):
            s_ = wst[j % 2]
            k.dma("sp", s_[:], w_out_d[j * 128:(j + 1) * 128, :], W=[s_])
            k.op("pool" if j % 2 else "dve", lambda e: e.tensor_copy(wo[:, j, :], s_[:]), R=[s_], W=[wo])
        k.dma("sp", wr[:], w_router_d[:].rearrange("(j p) n -> p j n", p=128), W=[wr])
        UT = k.sb("UT", [128, 128], BF16); ONESB = k.sb("ONESB", [128, 128], BF16); ones256 = k.sb("ones256", [128, NE])
        uti = k.sb("uti", [128, 128], I32)
        k.op("pool", lambda e: e.iota(uti[:], pattern=[[1, 128]], base=0, channel_multiplier=-1), W=[uti])
        k.op("dve", lambda e: e.tensor_scalar(UT[:], uti[:], 0.0, None, op0=ALU.is_gt), R=[uti], W=[UT])
        k.op("dve", lambda e: e.memset(ONESB[:], 1.0), W=[ONESB])
        k.op("dve", lambda e: e.memset(ones256[:], 1.0), W=[ones256])
        ecap_i = k.sb("ecap_i", [128, NE], I32); eidx = k.sb("eidx", [128, NE])
        k.op("pool", lambda e: e.iota(ecap_i[:], pattern=[[1, NE]], base=0, channel_multiplier=0), W=[ecap_i])
        k.op("dve", lambda e: e.tensor_copy(eidx[:], ecap_i[:]), R=[ecap_i], W=[eidx])
        EK = k.sb("EK", [128, NTT, 8]); RK = k.sb("RK", [128, NTT, 8])
        carry = k.sb("carry", [128, NE])
        k.op("dve", lambda e: e.memset(carry[:], 0.0), W=[carry])
        yc_ = [k.sb("ycat%d" % i, [128, D]) for i in range(2)]
        ycb = k.sb("ycb", [128, D], BF16); ycT = k.sb("ycT", [128, 8, 128], BF16)
        xt = [k.sb("x3_%d" % i, [128, D]) for i in range(2)]
        x1 = [k.sb("x1_%d" % i, [128, D]) for i in range(2)]
        junk = k.sb("junk3", [128, D]); ssq = k.sb("ssq3", [128, 1])
        h2 = k.sb("h2", [128, D]); h2b = [k.sb("h2b%d" % i, [128, D], BF16) for i in range(2)]
        h2T = k.sb("h2T", [128, 8, 128]); h2Tb = [k.sb("h2Tb%d" % i, [128, 8, 128], BF16) for i in range(2)]
        pT3 = k.ps("pT3", [128, 8, 128], BF16); pTf = k.ps("pTf", [128, 8, 128])
        pp = k.ps("pp", [128, 2, 512]); plg = k.ps("plg", [128, NE]); prk = k.ps("prk", [128, 2, NE])
        sc = k.sb("sc", [128, NE]); sel = k.sb("sel", [128, NE]); selm = k.sb("selm", [128, NE])
        m8g = k.sb("m8g", [128, 8, 8]); gs = k.sb("gs", [128, 8]); gm8 = k.sb("gm8", [128, 8]); gmask = k.sb("gmask", [128, 8]); em8 = k.sb("em8", [128, 8])
        emask = k.sb("emask", [128, NE]); emb = k.sb("emb", [128, NE], BF16); Gm = k.sb("Gm", [128, NE]); wsum = k.sb("wsum", [128, 1])
        pos = k.sb("pos", [128, NE]); dfull = k.sb("dfull", [128, NE]); ovf = k.sb("ovf", [128, NE]); slot = k.sb("slot", [128, NE])
        junk2 = k.sb("junk2", [128, NE]); destf = k.sb("destf", [128, 8])
        for i in range(NTT):
            b = i // NT
            y_ = yc_[i % 2]; x_ = xt[i % 2]; x1_ = x1[i % 2]; hb_ = h2b[i % 2]; hTb_ = h2Tb[i % 2]
            k.dma("sp", y_[:], YCAT_d[i * 128:(i + 1) * 128, :], R=[YCAT_d], W=[y_])
            k.dma("sp", x_[:], x_d[i * 128:(i + 1) * 128, :], W=[x_])
            k.op("act", lambda e: e.copy(ycb[:], y_[:]), R=[y_], W=[ycb])
            for j in range(8):
                k.op("pe", lambda e: e.transpose(pT3[:, j, :], ycb[:, j * 128:(j + 1) * 128], ident_b[:]), R=[ycb, ident_b], W=[pT3])
            k.op("act", lambda e: e.copy(ycT[:], pT3[:]), R=[pT3], W=[ycT])
            for hh in range(2):
                for j in range(8):
                    k.op("pe", lambda e: e.matmul(pp[:, hh, :], ycT[:, j, :], wo[:, j, hh * 512:(hh + 1) * 512], start=(j == 0), stop=(j == 7)), R=[ycT, wo], W=[pp])
            ppf = pp[:].rearrange("p a n -> p (a n)")
            k.op("dve", lambda e: e.tensor_tensor(x1_[:], ppf, MOD[b][2][:], op=ALU.mult), R=[pp, MOD[b][2]], W=[x1_])
            k.op("pool", lambda e: e.tensor_tensor(x1_[:], x1_[:], x_[:], op=ALU.add), R=[x1_, x_], W=[x1_])
            k.dma("sp", X1_d[i * 128:(i + 1) * 128, :], x1_[:], R=[x1_], W=[X1_d])
            k.op("act", lambda e: e.activation(junk[:], x1_[:], ACT.Square, accum_out=ssq[:]), R=[x1_], W=[junk, ssq])
            k.op("dve", lambda e: e.tensor_scalar(ssq[:], ssq[:], 1.0 / D, EPS, op0=ALU.mult, op1=ALU.add), R=[ssq], W=[ssq])
            k.op("act", lambda e: e.sqrt(ssq[:], ssq[:]), R=[ssq], W=[ssq])
            k.op("dve", lambda e: e.reciprocal(ssq[:], ssq[:]), R=[ssq], W=[ssq])
            k.op("dve", lambda e: e.scalar_tensor_tensor(h2[:], x1_[:], ssq[:], MOD[b][4][:], op0=ALU.mult, op1=ALU.mult), R=[x1_, ssq, MOD[b][4]], W=[h2])
            k.op("pool", lambda e: e.tensor_tensor(h2[:], h2[:], MOD[b][3][:], op=ALU.add), R=[h2, MOD[b][3]], W=[h2])
            k.op("act", lambda e: e.copy(hb_[:], h2[:]), R=[h2], W=[hb_])
            if dbgH2 is not None:
                k.dma("sp", dbgH2[i * 128:(i + 1) * 128, :], h2[:], R=[h2], W=[dbgH2])
            for j in range(8):
                k.op("pe", lambda e: e.transpose(pTf[:, j, :], h2[:, j * 128:(j + 1) * 128], ident_f[:]), R=[h2, ident_f], W=[pTf])
            k.op("dve", lambda e: e.tensor_copy(h2T[:], pTf[:]), R=[pTf], W=[h2T])
            k.op("pool", lambda e: e.tensor_copy(hTb_[:], h2T[:]), R=[h2T], W=[hTb_])
            if "noH2T" not in SKIP:
                k.dma("sp", H2T_d[:, :, i * 128:(i + 1) * 128], hTb_[:], R=[hTb_], W=[H2T_d])
            if "noRT" not in SKIP:
                for j in range(8):
                    k.op("pe", lambda e: e.matmul(plg[:], h2T[:, j, :], wr[:, j, :], start=(j == 0), stop=(j == 7)), R=[h2T, wr], W=[plg])
                k.op("act", lambda e: e.activation(sc[:], plg[:], ACT.Sigmoid), R=[plg], W=[sc])
                k.op("dve", lambda e: e.tensor_tensor(sel[:], sc[:], rb_b[:], op=ALU.add), R=[sc, rb_b], W=[sel])
                for g in range(8):
                    k.op("dve", lambda e: e.max(m8g[:, g, :], sel[:, g * 32:(g + 1) * 32]), R=[sel], W=[m8g])
                k.op("dve", lambda e: e.tensor_tensor(gs[:], m8g[:, :, 0], m8g[:, :, 1], op=ALU.add), R=[m8g], W=[gs])
                k.op("dve", lambda e: e.max(gm8[:], gs[:]), R=[gs], W=[gm8])
                k.op("dve", lambda e: e.tensor_scalar(gmask[:], gs[:], gm8[:, 3:4], None, op0=ALU.is_ge), R=[gs, gm8], W=[gmask])
                k.op("dve", lambda e: e.scalar_tensor_tensor(selm[:].rearrange("p (g n) -> p g n", g=8), sel[:].rearrange("p (g n) -> p g n", g=8), 2.0,
                                                             gmask[:].unsqueeze(2).to_broadcast([128, 8, 32]), op0=ALU.add, op1=ALU.mult), R=[sel, gmask], W=[selm])
                k.op("dve", lambda e: e.max(em8[:], selm[:]), R=[selm], W=[em8])
                k.op("dve", lambda e: e.tensor_scalar(emask[:], selm[:], em8[:, 7:8], None, op0=ALU.is_ge), R=[selm, em8], W=[emask])
                k.op("act", lambda e: e.copy(emb[:], emask[:]), R=[emask], W=[emb])
                k.op("dve", lambda e: e.scalar_tensor_tensor(Gm[:], sc[:], 1.0, emask[:], op0=ALU.mult, op1=ALU.mult, accum_out=wsum[:]), R=[sc, emask], W=[Gm, wsum])
                k.op("dve", lambda e: e.reciprocal(wsum[:], wsum[:]), R=[wsum], W=[wsum])
                k.op("dve", lambda e: e.tensor_scalar(Gm[:], Gm[:], wsum[:], 2.5, op0=ALU.mult, op1=ALU.mult), R=[Gm, wsum], W=[Gm])
                if dbgG is not None:
                    k.dma("sp", dbgG[i * 128:(i + 1) * 128, :], Gm[:], R=[Gm], W=[dbgG])
            if "noRT" not in SKIP and "noRK" not in SKIP:
                k.op("pe", lambda e: e.matmul(prk[:, 0, :], UT[:], emb[:], start=True, stop=True), R=[UT, emb], W=[prk])
                k.op("pe", lambda e: e.matmul(prk[:, 1, :], ONESB[:], emb[:], start=True, stop=True), R=[ONESB, emb], W=[prk])
                k.op("dve", lambda e: e.tensor_tensor(pos[:], prk[:, 0, :], carry[:], op=ALU.add), R=[prk, carry], W=[pos])
                k.op("dve", lambda e: e.tensor_tensor(carry[:], prk[:, 1, :], carry[:], op=ALU.add), R=[prk, carry], W=[carry])
                k.op("dve", lambda e: e.tensor_tensor_scan(slot[:], ones256[:], emask[:], 0.0, op0=ALU.mult, op1=ALU.add), R=[ones256, emask], W=[slot])
                k.op("dve", lambda e: e.tensor_tensor(slot[:], slot[:], emask[:], op=ALU.mult), R=[slot, emask], W=[slot])
                for ks in range(8):
                    k.op("dve", lambda e: e.scalar_tensor_tensor(junk2[:], slot[:], float(ks + 1), eidx[:], op0=ALU.is_equal, op1=ALU.mult, accum_out=EK[:, i, ks:ks + 1]),
                         R=[slot, eidx], W=[junk2, EK])
                    k.op("dve", lambda e: e.scalar_tensor_tensor(junk2[:], slot[:], float(ks + 1), pos[:], op0=ALU.is_equal, op1=ALU.mult, accum_out=RK[:, i, ks:ks + 1]),
                         R=[slot, pos], W=[junk2, RK])
                    k.op("dve", lambda e: e.scalar_tensor_tensor(junk2[:], slot[:], float(ks + 1), Gm[:], op0=ALU.is_equal, op1=ALU.mult, accum_out=WK[:, i, ks:ks + 1]),
                         R=[slot, Gm], W=[junk2, WK])
            k.dma("sp", H2B_d[i * 128:(i + 1) * 128, :], hb_[:], R=[hb_], W=[H2B_d])
        cnt_i = k.sb("cnt_i", [128, NE], I32); padc = k.sb("padc", [128, NE]); pend = k.sb("pend", [128, NE]); pstart = k.sb("pstart", [128, NE])
        k.op("dve", lambda e: e.tensor_scalar(padc[:], carry[:], float(BLK - 1), None, op0=ALU.add), R=[carry], W=[padc])
        k.op("dve", lambda e: e.tensor_copy(cnt_i[:], padc[:]), R=[padc], W=[cnt_i])
        k.op("dve", lambda e: e.tensor_scalar(cnt_i[:], cnt_i[:], LOGB, LOGB, op0=ALU.arith_shift_right, op1=ALU.logical_shift_left), R=[cnt_i], W=[cnt_i])
        k.op("dve", lambda e: e.tensor_copy(padc[:], cnt_i[:]), R=[cnt_i], W=[padc])
        k.op("dve", lambda e: e.tensor_tensor_scan(pend[:], ones256[:], padc[:], 0.0, op0=ALU.mult, op1=ALU.add), R=[ones256, padc], W=[pend])
        k.op("dve", lambda e: e.tensor_tensor(pstart[:], pend[:], padc[:], op=ALU.subtract), R=[pend, padc], W=[pstart])
        bexp = k.sb("bexp", [128, NBLK])
        for j in range(NBLK):
            k.op("dve", lambda e: e.tensor_scalar(junk2[:], pend[:], float(BLK * j), 0.0, op0=ALU.is_le, op1=ALU.add, accum_out=bexp[:, j:j + 1]), R=[pend], W=[junk2, bexp])
        k.op("dve", lambda e: e.tensor_scalar(bexp[:], bexp[:], float(NE - 1), None, op0=ALU.min), R=[bexp], W=[bexp])
        bgi = k.sb("bgi", [128, 1], I32); bgf = k.sb("bgf", [128, 1])
        k.op("pool", lambda e: e.iota(bgi[:], pattern=[[1, 1]], base=0, channel_multiplier=1), W=[bgi])
        k.op("dve", lambda e: e.tensor_copy(bgf[:], bgi[:]), R=[bgi], W=[bgf])
        idxf = k.sb("idxf", [128, NBLK])
        k.op("dve", lambda e: e.tensor_scalar(idxf[:], bexp[:], 128.0, bgf[:, 0:1], op0=ALU.mult, op1=ALU.add), R=[bexp, bgf], W=[idxf])
        k.op("dve", lambda e: e.tensor_copy(IDXG[:], idxf[:]), R=[idxf], W=[IDXG])
        if "BEXP" in dbg:
            o = outp("BEXP", [1, NBLK]); k.dma("sp", o[0:1, :], bexp[0:1, :], R=[bexp], W=[o])
            o2 = outp("PSTART", [1, NE]); k.dma("sp", o2[0:1, :], pstart[0:1, :], R=[pstart], W=[o2])
        hbb = [k.sb("hbb%d" % i, [128, D], BF16) for i in range(2)]
        for i in range(NTT):
            hb_ = hbb[i % 2]
            k.dma("sp", hb_[:], H2B_d[i * 128:(i + 1) * 128, :], R=[H2B_d], W=[hb_])
            for ks in range(8):
                k.op("dve", lambda e: e.scalar_tensor_tensor(junk2[:], eidx[:], EK[:, i, ks:ks + 1], pstart[:], op0=ALU.is_equal, op1=ALU.mult, accum_out=destf[:, ks:ks + 1]),
                     R=[eidx, EK, pstart], W=[junk2, destf])
            k.op("dve", lambda e: e.tensor_tensor(destf[:], destf[:], RK[:, i, :], op=ALU.add), R=[destf, RK], W=[destf])
            k.op("dve", lambda e: e.tensor_copy(DESTI[:, i, :], destf[:]), R=[destf], W=[DESTI])
            if dbgDW is not None:
                k.dma("sp", dbgDW[i * 128:(i + 1) * 128, 0:8], destf[:], R=[destf], W=[dbgDW])
                k.dma("sp", dbgDW[i * 128:(i + 1) * 128, 8:16], WK[:, i, :], R=[WK], W=[dbgDW])
            for ks in range(8):
                k.idma(out=XBUF_d[:, :], out_offset=bass.IndirectOffsetOnAxis(ap=DESTI[:, i, ks:ks + 1], axis=0), in_=hb_[:, :], in_offset=None,
                       bounds_check=BREG, oob_is_err=False, R=[hb_, DESTI], W=[XBUF_d])
        if True:
            o = outp("CNT", [1, NE])
            k.dma("sp", o[0:1, :], carry[0:1, :], R=[carry], W=[o])
    k.es = es
    k.regen()
    if stop_after <= 7:
        k.finish(list(outs.values()))
        return nc, es


    wegu_d = inp("w_e_gate_up", [NE * 128, 8 * 512]); wed_d = inp("w_e_down", [NE * 128, 2 * D])
    NBX = int(os.environ.get("NBX", str(NBLK)))
    CT = BLK // 128
    with ExitStack() as es8:
        k.es = es8
        k.imax = 12
        gst = [k.sb("gst%d" % i, [128, 8, 512]) for i in range(2)]
        dst_ = [k.sb("dst%d" % i, [128, 2, D]) for i in range(2)]
        wgu = [k.sb("wgu%d" % i, [128, 8, 512], BF16) for i in range(2)]
        wd = [k.sb("wd%d" % i, [128, 2, D], BF16) for i in range(2)]
        xin = [k.sb("xin%d" % i, [128, D], BF16) for i in range(4)]
        xT = [k.sb("xT%d" % i, [128, 8, BLK], BF16) for i in range(2)]
        sg = [k.sb("sg%d" % i, [128, BLK]) for i in range(2)]
        actT = [k.sb("actT%d" % i, [128, 2, BLK], BF16) for i in range(2)]
        yo = [k.sb("yo4_%d" % i, [128, D], BF16) for i in range(4)]
        pTx = k.ps("pTx", [128, 8, 128], BF16)
        pu = [k.ps("pu4_%d" % i, [128, 512]) for i in range(4)]
        pd = k.ps("pd", [128, 2, 512])
        nx = 0; ny = 0
        for jb in range(NBX):
            g_ = gst[jb % 2]; d_ = dst_[jb % 2]; wg = wgu[jb % 2]; wd_ = wd[jb % 2]; xT_ = xT[jb % 2]; aT = actT[jb % 2]
            k.idma(out=g_[:].rearrange("p j n -> p (j n)"), out_offset=None, in_=wegu_d[:, :], in_offset=bass.IndirectOffsetOnAxis(ap=IDXG[:, jb:jb + 1], axis=0),
                   R=[wegu_d, IDXG], W=[g_])
            k.idma(out=d_[:].rearrange("p c n -> p (c n)"), out_offset=None, in_=wed_d[:, :], in_offset=bass.IndirectOffsetOnAxis(ap=IDXG[:, jb:jb + 1], axis=0),
                   R=[wed_d, IDXG], W=[d_])
            k.op("dve", lambda e: e.tensor_copy(wg[:, 0:5, :], g_[:, 0:5, :]), R=[g_], W=[wg])
            k.op("act", lambda e: e.copy(wg[:, 5:8, :], g_[:, 5:8, :]), R=[g_], W=[wg])
            k.op("act", lambda e: e.copy(wd_[:], d_[:]), R=[d_], W=[wd_])
            if jb == 0:
                for tt in range(CT):
                    k.dma("sp", xin[tt][:], XBUF_d[tt * 128:(tt + 1) * 128, :], R=[XBUF_d], W=[xin[tt]])
            if jb + 1 < NBX:
                for tt in range(CT):
                    xn = xin[((jb + 1) * CT + tt) % 4]
                    k.dma("sp", xn[:], XBUF_d[(jb + 1) * BLK + tt * 128:(jb + 1) * BLK + (tt + 1) * 128, :], R=[XBUF_d], W=[xn])
            for tt in range(CT):
                xi = xin[(jb * CT + tt) % 4]
                for j in range(8):
                    k.op("pe", lambda e: e.transpose(pTx[:, j, :], xi[:, j * 128:(j + 1) * 128], ident_b[:]), R=[xi, ident_b], W=[pTx])
                k.op("act" if tt % 2 else "dve", (lambda e: e.copy(xT_[:, :, tt * 128:(tt + 1) * 128], pTx[:])) if tt % 2 else
                     (lambda e: e.tensor_copy(xT_[:, :, tt * 128:(tt + 1) * 128], pTx[:])), R=[pTx], W=[xT_])
            for c in range(4):
                for j in range(8):
                    k.op("pe", lambda e: e.matmul(pu[c][:, 0:BLK], wg[:, j, c * 128:(c + 1) * 128], xT_[:, j, :], start=(j == 0), stop=(j == 7)), R=[wg, xT_], W=[pu[c]])
            for c in range(2):
                s_ = sg[c]
                k.op("act", lambda e: e.activation(s_[:], pu[c][:, 0:BLK], ACT.Silu), R=[pu[c]], W=[s_])
                k.op("dve", lambda e: e.tensor_tensor(aT[:, c, :], s_[:], pu[2 + c][:, 0:BLK], op=ALU.mult), R=[s_, pu[2 + c]], W=[aT])
            for tt in range(CT):
                for hh in range(2):
                    for c in range(2):
                        k.op("pe", lambda e: e.matmul(pd[:, hh, :], aT[:, c, tt * 128:(tt + 1) * 128], wd_[:, c, hh * 512:(hh + 1) * 512], start=(c == 0), stop=(c == 1)), R=[aT, wd_], W=[pd])
                y_ = yo[ny % 4]; ny += 1
                k.op("dve", lambda e: e.tensor_copy(y_[:], pd[:].rearrange("p a n -> p (a n)")), R=[pd], W=[y_])
                k.dma("sp", YBUF_d[jb * BLK + tt * 128:jb * BLK + (tt + 1) * 128, :], y_[:], R=[y_], W=[YBUF_d])
        k.imax = 6
    k.es = es
    k.regen()
    if stop_after <= 8:
        k.finish(list(outs.values()))
        return nc, es

    wsgu_d = inp("w_sh_gate_up", [D, 512]); wsd_d = inp("w_sh_down", [256, D])
    OUT_d = outp("out", [NTOK, D])
    TB5 = min(4, NTT)
    with ExitStack() as es9:
        k.es = es9
        gst = k.sb("sgst", [128, 8, 512]); dst_ = k.sb("sdst", [128, 2, D])
        wg = k.sb("swgu", [128, 8, 512], BF16); wd_ = k.sb("swd", [128, 2, D], BF16)
        k.dma("sp", gst[:], wsgu_d[:].rearrange("(j p) n -> p j n", p=128), W=[gst])
        k.dma("sp", dst_[:], wsd_d[:].rearrange("(c p) n -> p c n", p=128), W=[dst_])
        k.op("dve", lambda e: e.tensor_copy(wg[:], gst[:]), R=[gst], W=[wg])
        k.op("pool", lambda e: e.tensor_copy(wd_[:], dst_[:]), R=[dst_], W=[wd_])
        xTs = [k.sb("xTs%d" % i, [128, 8, TB5 * 128], BF16) for i in range(2)]
        sg = [k.sb("sg5_%d" % i, [128, TB5 * 128]) for i in range(2)]
        aT5 = [k.sb("aT5_%d" % i, [128, 2, TB5 * 128], BF16) for i in range(2)]
        yg = [k.sb("yg%d" % i, [128, D], BF16) for i in range(4)]
        for y_ in yg:
            k.op("pool", lambda e: e.memset(y_[:], 0.0), W=[y_])
        acc = [k.sb("acc%d" % i, [128, D]) for i in range(2)]
        x1t = [k.sb("x1t%d" % i, [128, D]) for i in range(2)]
        pu = [k.ps("pu5_%d" % i, [128, 512]) for i in range(4)]
        pd = [k.ps("pd5_%d" % i, [128, 2, 512]) for i in range(2)]
        nyg = 0
        for blk in range(NTT // TB5):
            xT_ = xTs[blk % 2]; aT = aT5[blk % 2]
            W5 = TB5 * 128
            k.dma("sp", xT_[:], H2T_d[:, :, blk * W5:(blk + 1) * W5], R=[H2T_d], W=[xT_])
            for c in range(4):
                for j in range(8):
                    k.op("pe", lambda e: e.matmul(pu[c][:, 0:W5], wg[:, j, c * 128:(c + 1) * 128], xT_[:, j, :], start=(j == 0), stop=(j == 7)), R=[wg, xT_], W=[pu[c]])
            for c in range(2):
                s_ = sg[c]
                k.op("act", lambda e: e.activation(s_[:], pu[c][:, 0:W5], ACT.Silu), R=[pu[c]], W=[s_])
                k.op("dve", lambda e: e.tensor_tensor(aT[:, c, :], s_[:], pu[2 + c][:, 0:W5], op=ALU.mult), R=[s_, pu[2 + c]], W=[aT])
            for tt in range(TB5):
                i = blk * TB5 + tt; b = i // NT
                pd_ = pd[i % 2]; a_ = acc[i % 2]; x1_ = x1t[i % 2]
                for hh in range(2):
                    for c in range(2):
                        k.op("pe", lambda e: e.matmul(pd_[:, hh, :], aT[:, c, tt * 128:(tt + 1) * 128], wd_[:, c, hh * 512:(hh + 1) * 512], start=(c == 0), stop=(c == 1)), R=[aT, wd_], W=[pd_])
                k.dma("sp", x1_[:], X1_d[i * 128:(i + 1) * 128, :], R=[X1_d], W=[x1_])
                k.op("act", lambda e: e.copy(a_[:], pd_[:].rearrange("p a n -> p (a n)")), R=[pd_], W=[a_])
                for ks in range(8):
                    y_ = yg[nyg % 4]; nyg += 1
                    k.idma(out=y_[:, :], out_offset=None, in_=YBUF_d[:, :], in_offset=bass.IndirectOffsetOnAxis(ap=DESTI[:, i, ks:ks + 1], axis=0),
                           bounds_check=BREG, oob_is_err=False, R=[YBUF_d, DESTI], W=[y_])
                    k.op("dve", lambda e: e.scalar_tensor_tensor(a_[:], y_[:], WK[:, i, ks:ks + 1], a_[:], op0=ALU.mult, op1=ALU.add), R=[y_, WK, a_], W=[a_])
                k.op("dve", lambda e: e.tensor_tensor(a_[:], a_[:], MOD[b][5][:], op=ALU.mult), R=[a_, MOD[b][5]], W=[a_])
                k.op("pool", lambda e: e.tensor_tensor(a_[:], a_[:], x1_[:], op=ALU.add), R=[a_, x1_], W=[a_])
                k.dma("sp", OUT_d[i * 128:(i + 1) * 128, :], a_[:], R=[a_], W=[OUT_d])
    k.es = es
    k.regen()
    k.finish(list(outs.values()))
    return nc, es


_CACHE = {}


def kernel(**inputs):
    T = 4096
    ncores = 8
    if "nc" not in _CACHE:
        _CACHE["nc"] = build(T)
    nc, es = _CACHE["nc"]
    names = [a.memorylocations[0].name for a in nc.allocations
             if hasattr(a, "kind") and a.kind == "ExternalInput" and a.memorylocations[0].name != "partition_id"]
    shared = {}
    for n in names:
        if n in ("x", "c", "positions"):
            continue
        a = np.asarray(inputs[n])[0]
        if n == "rwkv_r_k":
            a = a.reshape(1, 512)
        elif n == "w_e_gate_up":
            a = a.reshape(256, 8, 128, 512).transpose(0, 2, 1, 3).reshape(256 * 128, 8 * 512)
        elif n == "w_e_down":
            a = a.reshape(256, 2, 128, 1024).transpose(0, 2, 1, 3).reshape(256 * 128, 2 * 1024)
        elif a.ndim == 1:
            a = a.reshape(1, -1)
        shared[n] = np.ascontiguousarray(a)
    x = np.asarray(inputs["x"]); c = np.asarray(inputs["c"]); pos = np.asarray(inputs["positions"])
    in_maps = []
    for cid in range(ncores):
        m = dict(shared)
        m["x"] = np.ascontiguousarray(x[2 * cid:2 * cid + 2].reshape(2 * T, 1024))
        m["c"] = np.ascontiguousarray(c[2 * cid:2 * cid + 2])
        m["positions"] = np.ascontiguousarray(pos[2 * cid:2 * cid + 2].reshape(2 * T, 1).astype(np.int32))
        in_maps.append(m)
    res = run_bass_kernel_spmd(nc, in_maps, core_ids=list(range(ncores)))
    try:
        print("max expert count per core:", [int(r["CNT"].max()) for r in res.results], flush=True)
    except Exception:
        pass
    out = np.stack([r["out"].reshape(2, T, 1024) for r in res.results], 0).reshape(16, T, 1024)
    return out.astype(np.float32)
```
